# Optimizing a Trainium2 kernel written in Bass

```python
import math
import jax, jax.numpy as jnp
from jax import lax
import numpy as np

D_MODEL = 1024
BATCH = 4
SEQ = 4096
DEPTH = 4

N_MIXERS = 3
N_RET = (DEPTH + 2) // 3
N_MOBA = (DEPTH + 1) // 3
N_DIFF = DEPTH // 3

RET_HEADS = 4
RET_DK = D_MODEL // RET_HEADS
RET_DV = 2 * RET_DK
RET_CHUNK = 128
ROPE_BASE = 10000.0

ATTN_HEADS = 16
HEAD_DIM = D_MODEL // ATTN_HEADS
MOBA_BLOCK = 256
MOBA_TOPK = 3
MOBA_Q_CHUNK = 16
DIFF_HEADS = ATTN_HEADS // 2
DIFF_V_DIM = 2 * HEAD_DIM
ATTN_Q_BLOCK = 128

REL_BUCKETS = 32
REL_MAX_EXACT = REL_BUCKETS // 2
REL_MAX_DISTANCE = 1024

D_FF = 4 * D_MODEL
EPS = 1e-6
NEG = -1e30

kernel_name = "hybrid_retention_moba_diffattn_trunk"


def _rmsnorm(x, w):
    xf = x.astype(jnp.float32)
    y = xf * lax.rsqrt(jnp.mean(xf * xf, axis=-1, keepdims=True) + EPS)
    return y.astype(x.dtype) * w


def _rel_bucket(dist):
    n = jnp.maximum(dist, 0)
    nf = jnp.maximum(n, 1).astype(jnp.float32)
    large = REL_MAX_EXACT + (jnp.log(nf / REL_MAX_EXACT) / math.log(REL_MAX_DISTANCE / REL_MAX_EXACT)
                             * (REL_BUCKETS - REL_MAX_EXACT)).astype(jnp.int32)
    large = jnp.minimum(large, REL_BUCKETS - 1)
    return jnp.where(n < REL_MAX_EXACT, n, large)


def _rotary(x):
    S, d = x.shape[2], x.shape[3]
    inv_freq = ROPE_BASE ** (-jnp.arange(0, d, 2, dtype=jnp.float32) / d)
    ang = jnp.arange(S, dtype=jnp.float32)[:, None] * inv_freq[None, :]
    cos, sin = jnp.cos(ang), jnp.sin(ang)
    x1, x2 = x[..., : d // 2], x[..., d // 2:]
    return jnp.concatenate([x1 * cos - x2 * sin, x1 * sin + x2 * cos], axis=-1)


def _squared_relu_mlp(h, w_up, w_down):
    u = jax.nn.relu(h @ w_up)
    return (u * u) @ w_down


def _retention_mixer(h, w_in, w_out):
    B, S, _ = h.shape
    H, dk, dv, C = RET_HEADS, RET_DK, RET_DV, RET_CHUNK
    N = S // C
    proj = (h @ w_in).astype(jnp.float32)
    q, k, v, g = jnp.split(proj, [H * dk, 2 * H * dk, 2 * H * dk + H * dv], axis=-1)
    q = _rotary(q.reshape(B, S, H, dk).transpose(0, 2, 1, 3))
    k = _rotary(k.reshape(B, S, H, dk).transpose(0, 2, 1, 3)) * (dk ** -0.5)
    v = v.reshape(B, S, H, dv).transpose(0, 2, 1, 3)

    log_gamma = jnp.log(1.0 - 2.0 ** (-5.0 - jnp.arange(H, dtype=jnp.float32)))
    pos = jnp.arange(C, dtype=jnp.float32)
    rel = pos[:, None] - pos[None, :]
    inner_decay = jnp.where(rel >= 0, jnp.exp(jnp.maximum(rel, 0.0)[None] * log_gamma[:, None, None]), 0.0)

    qc = q.reshape(B, H, N, C, dk)
    kc = k.reshape(B, H, N, C, dk)
    vc = v.reshape(B, H, N, C, dv)
    s_in = jnp.einsum('bhnid,bhnjd->bhnij', qc, kc) * inner_decay[:, None]
    inner = jnp.einsum('bhnij,bhnjv->bhniv', s_in, vc)
    q_dec = qc * jnp.exp((pos + 1.0)[None, :] * log_gamma[:, None])[:, None, :, None]
    k_dec = kc * jnp.exp((C - 1.0 - pos)[None, :] * log_gamma[:, None])[:, None, :, None]
    chunk_decay = jnp.exp(C * log_gamma)[:, None, None]

    def step(state, xs):
        qn, kn, vn = xs
        out = jnp.einsum('bhid,bhdv->bhiv', qn, state)
        state = state * chunk_decay + jnp.einsum('bhjd,bhjv->bhdv', kn, vn)
        return state, out

    xs = (jnp.moveaxis(q_dec, 2, 0), jnp.moveaxis(k_dec, 2, 0), jnp.moveaxis(vc, 2, 0))
    _, cross = lax.scan(step, jnp.zeros((B, H, dk, dv), jnp.float32), xs)
    o = (inner + jnp.moveaxis(cross, 0, 2)).reshape(B, H, S, dv)
    o = o * lax.rsqrt(jnp.mean(o * o, axis=-1, keepdims=True) + EPS)
    o = o.transpose(0, 2, 1, 3).reshape(B, S, H * dv)
    return ((jax.nn.silu(g) * o) @ w_out).astype(h.dtype)


def _moba_mixer(h, w_in, q_norm_w, k_norm_w, w_out, rel_bias):
    B, S, _ = h.shape
    H, d, BS, QC = ATTN_HEADS, HEAD_DIM, MOBA_BLOCK, MOBA_Q_CHUNK
    proj = h @ w_in
    q, k, v = jnp.split(proj, 3, axis=-1)
    q = _rmsnorm(q.reshape(B, S, H, d), q_norm_w).transpose(0, 2, 1, 3).astype(jnp.float32) * (d ** -0.5)
    k = _rmsnorm(k.reshape(B, S, H, d), k_norm_w).transpose(0, 2, 1, 3).astype(jnp.float32)
    v = v.reshape(B, S, H, d).transpose(0, 2, 1, 3)
    NB = -(-S // BS)
    pad = NB * BS - S
    k_pad = jnp.pad(k, ((0, 0), (0, 0), (0, pad), (0, 0)))
    v_pad = jnp.pad(v, ((0, 0), (0, 0), (0, pad), (0, 0)))
    kb = k_pad.reshape(B, H, NB, BS, d)
    vb = v_pad.reshape(B, H, NB, BS, d)
    k_mean = kb.mean(axis=3)
    topk = min(MOBA_TOPK, NB)
    bias_t = rel_bias.T.astype(jnp.float32)
    head_idx = jnp.arange(H)[None, :, None, None, None]
    in_block = jnp.arange(BS)
    gather = jax.vmap(jax.vmap(lambda blocks, ix: blocks[ix]))

    def chunk(c):
        t0 = c * QC
        b = t0 // BS
        t = t0 + jnp.arange(QC)
        qc = lax.dynamic_slice_in_dim(q, t0, QC, axis=2)
        gate = jnp.einsum('bhqd,bhnd->bhqn', qc, k_mean)
        gate = jnp.where(jnp.arange(NB) < b, gate, NEG)
        _, idx = lax.top_k(gate, topk)
        valid = idx < b
        k_sel = gather(kb, idx)
        v_sel = gather(vb, idx)
        s_sel = jnp.einsum('bhqd,bhqkjd->bhqkj', qc, k_sel)
        kpos = idx[..., None] * BS + in_block
        s_sel = s_sel + bias_t[head_idx, _rel_bucket(t[:, None, None] - kpos)]
        s_sel = jnp.where(valid[..., None], s_sel, NEG)
        k_own = lax.dynamic_slice_in_dim(k_pad, b * BS, BS, axis=2)
        v_own = lax.dynamic_slice_in_dim(v_pad, b * BS, BS, axis=2)
        dist = t[:, None] - (b * BS + in_block)[None, :]
        s_own = jnp.einsum('bhqd,bhjd->bhqj', qc, k_own) + bias_t[:, _rel_bucket(dist)]
        s_own = jnp.where(dist >= 0, s_own, NEG)
        scores = jnp.concatenate([s_sel.reshape(B, H, QC, topk * BS), s_own], axis=-1)
        p = jax.nn.softmax(scores, axis=-1)
        p_sel = p[..., : topk * BS].reshape(B, H, QC, topk, BS)
        p_own = p[..., topk * BS:]
        return (jnp.einsum('bhqkj,bhqkjd->bhqd', p_sel, v_sel)
                + jnp.einsum('bhqj,bhjd->bhqd', p_own, v_own))

    outs = lax.map(chunk, jnp.arange(S // QC))
    o = outs.transpose(1, 2, 0, 3, 4).reshape(B, H, S, d).transpose(0, 2, 1, 3).reshape(B, S, H * d)
    return (o @ w_out).astype(h.dtype)


def _diff_mixer(h, w_in, q_norm_w, k_norm_w, lambdas, subln_w, w_out, rel_bias, lambda_init):
    B, S, _ = h.shape
    H2, d, QB = 2 * DIFF_HEADS, HEAD_DIM, ATTN_Q_BLOCK
    proj = h @ w_in
    q, k, v = jnp.split(proj, 3, axis=-1)
    q = _rmsnorm(q.reshape(B, S, H2, d), q_norm_w).transpose(0, 2, 1, 3).astype(jnp.float32) * (d ** -0.5)
    k = _rmsnorm(k.reshape(B, S, H2, d), k_norm_w).transpose(0, 2, 1, 3).astype(jnp.float32)
    v = v.reshape(B, S, DIFF_HEADS, DIFF_V_DIM).transpose(0, 2, 1, 3)
    lam = lambdas.astype(jnp.float32)
    lambda_full = jnp.exp(jnp.sum(lam[0] * lam[1])) - jnp.exp(jnp.sum(lam[2] * lam[3])) + lambda_init
    bias_t = rel_bias.T.astype(jnp.float32)
    kpos = jnp.arange(S)

    def block(c):
        t0 = c * QB
        t = t0 + jnp.arange(QB)
        qb = lax.dynamic_slice_in_dim(q, t0, QB, axis=2)
        dist = t[:, None] - kpos[None, :]
        s = jnp.einsum('bhqd,bhkd->bhqk', qb, k) + bias_t[:, _rel_bucket(dist)]
        s = jnp.where(dist >= 0, s, NEG)
        p = jax.nn.softmax(s, axis=-1).reshape(B, DIFF_HEADS, 2, QB, S)
        attn = p[:, :, 0] - lambda_full * p[:, :, 1]
        return jnp.einsum('bhqk,bhkv->bhqv', attn, v)

    outs = lax.map(block, jnp.arange(S // QB))
    o = outs.transpose(1, 2, 0, 3, 4).reshape(B, DIFF_HEADS, S, DIFF_V_DIM)
    o = _rmsnorm(o, subln_w) * (1.0 - lambda_init)
    o = o.transpose(0, 2, 1, 3).reshape(B, S, DIFF_HEADS * DIFF_V_DIM)
    return (o @ w_out).astype(h.dtype)


def setup_inputs(seed: int = 0) -> dict:
    key = jax.random.key(seed)
    ks = jax.random.split(key, 18)

    def nrm(k, shape, scale):
        return jax.random.normal(k, shape, jnp.float32) * scale

    D = D_MODEL
    ret_in = 2 * RET_HEADS * RET_DK + 2 * RET_HEADS * RET_DV
    return {
        "x": nrm(ks[0], (BATCH, SEQ, D), 1.0),
        "rel_bias": nrm(ks[1], (REL_BUCKETS, ATTN_HEADS), 0.5),
        "norm1": 1.0 + nrm(ks[2], (DEPTH, D), 0.02),
        "norm2": 1.0 + nrm(ks[3], (DEPTH, D), 0.02),
        "w_up": nrm(ks[4], (DEPTH, D, D_FF), D ** -0.5),
        "w_down": nrm(ks[5], (DEPTH, D_FF, D), D_FF ** -0.5),
        "ret_w_in": nrm(ks[6], (N_RET, D, ret_in), D ** -0.5),
        "ret_w_out": nrm(ks[7], (N_RET, RET_HEADS * RET_DV, D), (RET_HEADS * RET_DV) ** -0.5),
        "moba_w_in": nrm(ks[8], (N_MOBA, D, 3 * ATTN_HEADS * HEAD_DIM), D ** -0.5),
        "moba_q_norm": 1.0 + nrm(ks[9], (N_MOBA, HEAD_DIM), 0.02),
        "moba_k_norm": 1.0 + nrm(ks[10], (N_MOBA, HEAD_DIM), 0.02),
        "moba_w_out": nrm(ks[11], (N_MOBA, ATTN_HEADS * HEAD_DIM, D), (ATTN_HEADS * HEAD_DIM) ** -0.5),
        "diff_w_in": nrm(ks[12], (N_DIFF, D, 3 * ATTN_HEADS * HEAD_DIM), D ** -0.5),
        "diff_q_norm": 1.0 + nrm(ks[13], (N_DIFF, HEAD_DIM), 0.02),
        "diff_k_norm": 1.0 + nrm(ks[14], (N_DIFF, HEAD_DIM), 0.02),
        "diff_lambda": nrm(ks[15], (N_DIFF, 4, HEAD_DIM), 0.1),
        "diff_subln": 1.0 + nrm(ks[16], (N_DIFF, DIFF_V_DIM), 0.02),
        "diff_w_out": nrm(ks[17], (N_DIFF, DIFF_HEADS * DIFF_V_DIM, D), (DIFF_HEADS * DIFF_V_DIM) ** -0.5),
    }


def reference(x, rel_bias, norm1, norm2, w_up, w_down, ret_w_in, ret_w_out,
              moba_w_in, moba_q_norm, moba_k_norm, moba_w_out,
              diff_w_in, diff_q_norm, diff_k_norm, diff_lambda, diff_subln, diff_w_out):
    for i in range(DEPTH):
        kind, j = i % N_MIXERS, i // N_MIXERS
        h = _rmsnorm(x, norm1[i])
        if kind == 0:
            m = _retention_mixer(h, ret_w_in[j], ret_w_out[j])
        elif kind == 1:
            m = _moba_mixer(h, moba_w_in[j], moba_q_norm[j], moba_k_norm[j], moba_w_out[j], rel_bias)
        else:
            lambda_init = 0.8 - 0.6 * math.exp(-0.3 * i)
            m = _diff_mixer(h, diff_w_in[j], diff_q_norm[j], diff_k_norm[j], diff_lambda[j],
                            diff_subln[j], diff_w_out[j], rel_bias, lambda_init)
        x = x + m
        x = x + _squared_relu_mlp(_rmsnorm(x, norm2[i]), w_up[i], w_down[i])
    return x
```

```python
import math
from contextlib import ExitStack
import numpy as np
import concourse.bass as bass
import concourse.mybir as mybir
from concourse.bass_utils import run_bass_kernel_spmd

F32 = mybir.dt.float32
BF16 = mybir.dt.bfloat16
AF = mybir.ActivationFunctionType
ALU = mybir.AluOpType
AX = mybir.AxisListType

D = 1024
KC = 8
S = 4096
NTOK = 2048
DFF = 4096
EPS = 1e-6
ENGS = ("pe", "act", "dve", "pool", "sp")


class T:
    __slots__ = ("name", "w", "r")

    def __init__(self, name=""):
        self.name = name
        self.w = None
        self.r = {}


class Prog:
    def __init__(self, nc, same_engine_sync=True):
        self.nc = nc
        self.ops = {e: [] for e in ENGS}
        self.cnt = {}
        self.seen = {e: {} for e in ENGS}
        self.same = same_engine_sync
        self.dma_sems = {}

    def _deps(self, eng, reads, writes):
        deps = {}

        def add(tok):
            if tok is None:
                return
            k, v = tok
            if deps.get(k, 0) < v:
                deps[k] = v
        for t in reads:
            add(t.w)
        for t in writes:
            add(t.w)
            for r in t.r.items():
                add(r)
        waits = []
        for k, v in deps.items():
            if k == eng and (not self.same or eng == "pe"):
                continue
            if self.seen[eng].get(k, 0) >= v:
                continue
            self.seen[eng][k] = v
            waits.append((k, v))
        return waits

    def _mark(self, tok, reads, writes):
        for t in reads:
            if t.r.get(tok[0], 0) < tok[1]:
                t.r[tok[0]] = tok[1]
        for t in writes:
            t.w = tok
            t.r = {}

    def op(self, eng, fn, reads=(), writes=()):
        waits = self._deps(eng, reads, writes)
        self.cnt[eng] = self.cnt.get(eng, 0) + 1
        tok = (eng, self.cnt[eng])
        self._mark(tok, reads, writes)
        self.ops[eng].append((fn, waits, (eng, 1)))
        return tok

    def dma(self, q, fns, reads=(), writes=(), sem=None):
        if not isinstance(fns, (list, tuple)):
            fns = [fns]
        key = ("dma", sem)
        self.dma_sems[key] = None
        waits = self._deps(q, reads, writes)
        self.cnt[key] = self.cnt.get(key, 0) + 16 * len(fns)
        tok = (key, self.cnt[key])
        self._mark(tok, reads, writes)
        for i, fn in enumerate(fns):
            self.ops[q].append((fn, waits if i == 0 else [], (key, 16)))
        return tok

    def wait_all(self, eng, toks):
        best = {}
        for k, v in toks:
            best[k] = max(best.get(k, 0), v)
        waits = []
        for k, v in best.items():
            if self.seen[eng].get(k, 0) >= v:
                continue
            self.seen[eng][k] = v
            waits.append((k, v))
        self.ops[eng].append((None, waits, None))

    def barrier(self):
        toks = list(self.cnt.items())
        for e in ENGS:
            self.wait_all(e, toks)

    def emit(self, stack):
        nc = self.nc
        sems = {}
        for e in ENGS:
            if self.ops[e]:
                sems[e] = stack.enter_context(nc.semaphore("s_" + e))
        for k in self.dma_sems:
            sems[k] = stack.enter_context(nc.semaphore("d_%s" % (k[1],)))
        block = stack.enter_context(nc.Block())
        handles = {"pe": block.tensor, "act": block.scalar, "dve": block.vector,
                   "pool": block.gpsimd, "sp": block.sync}

        def make(e):
            def body(engine):
                for fn, waits, inc in self.ops[e]:
                    for k, v in waits:
                        engine.wait_ge(sems[k], v)
                    if fn is not None:
                        fn(engine).then_inc(sems[inc[0]], inc[1])
            return body
        for e in ENGS:
            if self.ops[e]:
                handles[e](make(e))


class Ctx:
    def __init__(self, nc, st):
        self.nc, self.st = nc, st
        self.P = Prog(nc)
        self.ps = st.enter_context(nc.psum_tensor("ps", [128, 8, 512], F32))
        self.Tps = [T("ps%d" % i) for i in range(8)]
        self.ones = st.enter_context(nc.sbuf_tensor("ones", [128, 128], BF16))
        self.Tones = T("ones")
        self.eps = st.enter_context(nc.sbuf_tensor("epsc", [128, 1], F32))
        self.P.op("pool", lambda e: e.memset(self.eps[:], EPS), writes=[self.Tones])
        self.P.op("pool", lambda e: e.memset(self.ones[:], 1.0), writes=[self.Tones])
        self.bank_rr = {}

    def sb(self, name, shape, dt):
        self.nalloc = getattr(self, "nalloc", 0) + 1
        stack = self.ph if getattr(self, "ph", None) is not None else self.st
        return stack.enter_context(self.nc.sbuf_tensor("%s_%d" % (name, self.nalloc), shape, dt))

    def bank(self, group, banks):
        i = self.bank_rr.get(group, 0)
        self.bank_rr[group] = i + 1
        return banks[i % len(banks)]


def mm_group(C, bank, mms, reads, n=512, prow=128):
    ps = C.ps

    def fn(e, mms=mms):
        ins = None
        for i, (l, r) in enumerate(mms):
            ins = e.matmul(ps[0:prow, bank, 0:n], lhsT=l, rhs=r, start=(i == 0), stop=(i == len(mms) - 1))
        return ins
    return C.P.op("pe", fn, reads=reads, writes=[C.Tps[bank]])


def rmsnorm_fm(C, x, Tx, w32, Tw, out, Tout, ntok, tmp, nd=KC, banks=(6, 7), tt_list=None, xoff=0):
    P = C.P
    sq, Tsq, rstd, Trstd = tmp
    nfeat = nd * 128
    for tt in (tt_list if tt_list is not None else range(ntok // 512)):
        ts = slice(tt * 512, (tt + 1) * 512)
        xs = slice(xoff + tt * 512, xoff + (tt + 1) * 512)
        b = C.bank("nrm", banks)
        for c in range(nd):
            P.op("act", lambda e, c=c, xs=xs: e.activation(out=sq[:, c, :], in_=x[:, c, xs], func=AF.Square),
                 reads=[Tx[c][tt]], writes=[Tsq[c]])
        mm_group(C, b, [(C.ones[:], sq[:, c, :]) for c in range(nd)], reads=[C.Tones] + Tsq[:nd])
        P.op("act", lambda e, b=b: e.activation(out=rstd[:], in_=C.ps[:, b, :], func=AF.Sqrt, scale=1.0 / nfeat, bias=C.eps[:]),
             reads=[C.Tps[b], C.Tones], writes=[Trstd])
        P.op("dve", lambda e: e.reciprocal(out=rstd[:], in_=rstd[:]), reads=[Trstd], writes=[Trstd])
        for c in range(nd):
            P.op("dve", lambda e, c=c, ts=ts, xs=xs: e.scalar_tensor_tensor(
                out=out[:, c, ts], in0=x[:, c, xs], scalar=w32[:, c:c + 1], in1=rstd[:], op0=ALU.mult, op1=ALU.mult),
                reads=[Tx[c][tt], Tw, Trstd], writes=[Tout[c][tt]])


def emit_phase_a(C, ntok, x_src, x_dst, o_src=None, wout=None, fo=0, wup=None, wdown=None, n2=None, n1=None, h_dst=None, TT=1024):
    P = C.P
    do_mix, do_ffn, do_next = o_src is not None, wup is not None, h_dst is not None
    NS = TT // 512
    out_toks = []
    x = C.sb("x", [128, KC, TT], F32)
    Tx = [[T("x") for _ in range(NS)] for c in range(KC)]
    sq = C.sb("sq", [128, KC, 512], BF16)
    Tsq = [T("sq") for c in range(KC)]
    rstd = C.sb("rstd", [128, 512], F32)
    tmpn = (sq, Tsq, rstd, T("rstd"))
    hb = C.sb("hb", [128, KC, TT], BF16)
    Thb = [[T("hb") for _ in range(NS)] for c in range(KC)]
    allx = [t for c in range(KC) for t in Tx[c]]
    allh = [t for c in range(KC) for t in Thb[c]]

    def load_w(name, src):
        w = C.sb(name, [128, KC], F32)
        Tw = T(name)
        P.dma("sp", lambda e: e.dma_start(out=w[:], in_=src), writes=[Tw], sem=name)
        return w, Tw
    if do_ffn or do_mix:
        u2 = C.sb("u2", [128, 32, TT], BF16)
        Tu2 = [T("u2") for f in range(32)]
    if do_mix:
        FK = fo // 128
        wo = [C.sb("wo", [128, FK, 128], BF16) for i in range(2)]
        Two = [T("wo") for i in range(2)]
    if do_ffn:
        w2, Tw2 = load_w("n2w", n2)
        wu = [C.sb("wu", [128, KC, 512], BF16) for i in range(2)]
        Twu = [T("wu") for i in range(2)]
        wd = [C.sb("wd", [128, 32, 256], BF16) for i in range(2)]
        Twd = [T("wd") for i in range(2)]
        rl = [C.sb("rl", [128, 512], F32) for i in range(2)]
        Trl = [T("rl") for i in range(2)]
    if do_next:
        w1, Tw1 = load_w("n1w", n1)
    cnt = {"wo": 0, "wu": 0, "wd": 0, "rl": 0}

    def tile_body(tt):
        t0 = tt * TT
        P.dma("sp", [lambda e, c=c: e.dma_start(out=x[:, c, :], in_=x_src[c, :, t0:t0 + TT]) for c in range(KC)], writes=allx, sem="xin")
        if do_mix:
            ob = u2
            P.dma("sp", [lambda e, k=k: e.dma_start(out=ob[:, k, :], in_=o_src[k, :, t0:t0 + TT]) for k in range(FK)], writes=Tu2[:FK], sem="oin")
            for n in range(KC):
                sl = cnt["wo"] % 2
                cnt["wo"] += 1
                P.dma("pool", lambda e, n=n, sl=sl: e.dma_start(
                    out=wo[sl][:], in_=wout[:, n * 128:(n + 1) * 128].rearrange("(k p) n -> p k n", p=128)),
                    writes=[Two[sl]], sem="wo%d" % sl)
                for s_ in range(NS):
                    ss = slice(s_ * 512, (s_ + 1) * 512)
                    b = C.bank("dn", (4, 5))
                    mm_group(C, b, [(wo[sl][:, k, :], ob[:, k, ss]) for k in range(FK)], reads=[Two[sl]] + Tu2[:FK])
                    P.op("dve", lambda e, n=n, ss=ss, b=b: e.tensor_tensor(out=x[:, n, ss], in0=x[:, n, ss], in1=C.ps[:, b, :], op=ALU.add),
                         reads=[C.Tps[b], Tx[n][s_]], writes=[Tx[n][s_]])
        if do_ffn:
            rmsnorm_fm(C, x, Tx, w2, Tw2, hb, Thb, TT, tmpn)
            for fg in range(8):
                sl = cnt["wu"] % 2
                cnt["wu"] += 1
                P.dma("pool", lambda e, fg=fg, sl=sl: e.dma_start(
                    out=wu[sl][:], in_=wup[:, fg * 512:(fg + 1) * 512].rearrange("(k p) f -> p k f", p=128)),
                    writes=[Twu[sl]], sem="wu%d" % sl)
                for fi in range(4):
                    f = fg * 4 + fi
                    for s_ in range(NS):
                        ss = slice(s_ * 512, (s_ + 1) * 512)
                        b = C.bank("up", (0, 1, 2, 3))
                        mm_group(C, b, [(wu[sl][:, c, fi * 128:(fi + 1) * 128], hb[:, c, ss]) for c in range(KC)],
                                 reads=[Twu[sl]] + [Thb[c][s_] for c in range(KC)])
                        r = cnt["rl"] % 2
                        cnt["rl"] += 1
                        P.op("act", lambda e, b=b, r=r: e.activation(out=rl[r][:], in_=C.ps[:, b, :], func=AF.Relu),
                             reads=[C.Tps[b]], writes=[Trl[r]])
                        P.op("dve", lambda e, f=f, r=r, ss=ss: e.tensor_tensor(out=u2[:, f, ss], in0=rl[r][:], in1=rl[r][:], op=ALU.mult),
                             reads=[Trl[r]], writes=[Tu2[f]])
            for ng in range(4):
                sl = cnt["wd"] % 2
                cnt["wd"] += 1
                P.dma("pool", lambda e, ng=ng, sl=sl: e.dma_start(
                    out=wd[sl][:], in_=wdown[:, ng * 256:(ng + 1) * 256].rearrange("(k p) n -> p k n", p=128)),
                    writes=[Twd[sl]], sem="wd%d" % sl)
                for ni in range(2):
                    n = ng * 2 + ni
                    for s_ in range(NS):
                        ss = slice(s_ * 512, (s_ + 1) * 512)
                        b = C.bank("dn", (4, 5))
                        mm_group(C, b, [(wd[sl][:, f, ni * 128:(ni + 1) * 128], u2[:, f, ss]) for f in range(32)],
                                 reads=[Twd[sl]] + Tu2)
                        P.op("dve", lambda e, n=n, ss=ss, b=b: e.tensor_tensor(out=x[:, n, ss], in0=x[:, n, ss], in1=C.ps[:, b, :], op=ALU.add),
                             reads=[C.Tps[b], Tx[n][s_]], writes=[Tx[n][s_]])
        if x_dst is not None:
            out_toks.append(P.dma("sp", [lambda e, c=c: e.dma_start(out=x_dst[c, :, t0:t0 + TT], in_=x[:, c, :]) for c in range(KC)],
                                  reads=allx, sem="xout"))
        if do_next:
            rmsnorm_fm(C, x, Tx, w1, Tw1, hb, Thb, TT, tmpn)
            out_toks.append(P.dma("sp", [lambda e, c=c: e.dma_start(out=h_dst[c, :, t0:t0 + TT], in_=hb[:, c, :]) for c in range(KC)],
                                  reads=allh, sem="hout"))
    for tt in range(ntok // TT):
        tile_body(tt)
    return out_toks


RH, RDK, RDV, RC = 4, 256, 512, 128


def ret_consts():
    inv = (10000.0 ** (-np.arange(0, RDK, 2, dtype=np.float32) / np.float32(RDK))).astype(np.float32)
    ang = (np.arange(S, dtype=np.float32)[:, None] * inv[None, :]).astype(np.float32)
    cs = np.ascontiguousarray(np.stack([np.cos(ang).T, np.sin(ang).T]).astype(np.float32))
    lg = np.log(1.0 - 2.0 ** (-5.0 - np.arange(RH, dtype=np.float64)))
    pos = np.arange(RC, dtype=np.float64)
    per_head = []
    for h in range(RH):
        rel = pos[None, :] - pos[:, None]
        dT = np.where(rel >= 0, np.exp(np.maximum(rel, 0) * lg[h]), 0.0) * RDK ** -0.5
        gq = np.broadcast_to(np.exp((pos + 1.0) * lg[h])[None, :], (128, 128))
        kd = np.exp((RC - 1.0 - pos) * lg[h]) * RDK ** -0.5
        cd = np.full(128, np.exp(RC * lg[h]))
        per_head.append((dT.astype(np.float32), gq.astype(np.float32), kd.astype(np.float32), cd.astype(np.float32)))
    return cs, per_head


def emit_ret(C, h_src, win, hsel, cs_d, dmat_d, gq_d, kdcd_d, ident_d, o_dst, c0, ntt=S // 512):
    if True:
        P = C.P
        ps = C.ps
        oT = o_dst
        hT = h_src
        w = [C.sb("w%d" % h, [128, KC, 1536], BF16) for h in range(2)]
        Tw = [T("w%d" % h) for h in range(2)]
        for h in range(2):
            g = hsel[h]
            segs = [(0, g * 256, 256), (256, 1024 + g * 256, 256), (512, 2048 + g * 512, 512), (1024, 4096 + g * 512, 512)]
            P.dma("pool", [lambda e, h=h, d0=d0, s0=s0, n=n: e.dma_start(
                out=w[h][:, :, d0:d0 + n], in_=win[:, s0:s0 + n].rearrange("(k p) n -> p k n", p=128)) for d0, s0, n in segs],
                writes=[Tw[h]], sem="w%d" % h)
        dmat = C.sb("dmat_s", [128, 2, 128], F32)
        gq = C.sb("gq_s", [128, 2, 128], F32)
        kdcd = C.sb("kdcd_s", [128, 4], F32)
        ident = C.sb("ident_s", [128, 128], F32)
        Tc = T("consts")
        P.dma("sp", [lambda e, h=h: e.dma_start(out=dmat[:, h, :], in_=dmat_d[hsel[h]]) for h in range(2)]
              + [lambda e, h=h: e.dma_start(out=gq[:, h, :], in_=gq_d[hsel[h]]) for h in range(2)]
              + [lambda e, h=h: e.dma_start(out=kdcd[:, h:h + 1], in_=kdcd_d[:, hsel[h]:hsel[h] + 1], allow_slow_non_contiguous=True) for h in range(2)]
              + [lambda e, h=h: e.dma_start(out=kdcd[:, 2 + h:3 + h], in_=kdcd_d[:, 4 + hsel[h]:5 + hsel[h]], allow_slow_non_contiguous=True) for h in range(2)]
              + [lambda e: e.dma_start(out=ident[:], in_=ident_d)],
              writes=[Tc], sem="c")

        hb = [C.sb("hb%d" % i, [128, KC, 512], BF16) for i in range(2)]
        Thb = [T("hb%d" % i) for i in range(2)]
        csb = [C.sb("cs%d" % i, [128, 2, 512], F32) for i in range(2)]
        Tcs = [T("cs%d" % i) for i in range(2)]
        raw = C.sb("raw", [128, 2, 512], F32); Traw = T("raw")
        t1 = C.sb("t1", [128, 512], F32); Tt1 = T("t1")
        t2 = C.sb("t2", [128, 512], F32); Tt2 = T("t2")
        rot = C.sb("rot", [128, 2, 512], F32); Trot = T("rot")
        qb = C.sb("qb", [128, 2, 512], BF16); Tqb = T("qb")
        qd = C.sb("qd", [128, 2, 512], BF16); Tqd = T("qd")
        kb = C.sb("kb", [128, 2, 512], BF16); Tkb = T("kb")
        kdt = C.sb("kdt", [128, 4, 256], BF16); Tkdt = T("kdt")
        vt = C.sb("vt", [128, 4, 512], BF16); Tvt = T("vt")
        sg = C.sb("sg", [128, 4, 512], F32); Tsg = T("sg")
        of = C.sb("of", [128, 4, 512], F32); Tof = [[T("of%d_%d" % (v, 0))] for v in range(4)]
        at = C.sb("at", [128, 128], BF16); Tat = T("at")
        state = [C.sb("st%d" % h, [128, 2, 512], F32) for h in range(2)]
        stb = [C.sb("stb%d" % h, [128, 2, 512], BF16) for h in range(2)]
        Tst = [T("st%d" % h) for h in range(2)]
        Tstb = [T("stb%d" % h) for h in range(2)]
        sq = C.sb("sq", [128, 4, 512], BF16); Tsq = [T("sq%d" % c) for c in range(4)]
        rstd = C.sb("rstd", [128, 512], F32); Trstd = T("rstd")
        ob = [C.sb("ob%d" % i, [128, 8, 512], BF16) for i in range(2)]
        Tob = [T("ob%d" % i) for i in range(2)]
        out_toks = []

        for tt in range(ntt):
            ts = slice(tt * 512, (tt + 1) * 512)
            hbuf, Th = hb[tt % 2], Thb[tt % 2]
            cbuf, Tcb = csb[tt % 2], Tcs[tt % 2]
            P.dma("sp", [lambda e, k=k, ts=ts, hbuf=hbuf: e.dma_start(out=hbuf[:, k, :], in_=hT[k, :, ts]) for k in range(KC)],
                  writes=[Th], sem="h%d" % (tt % 2))
            P.dma("sp", [lambda e, i=i, ts=ts, cbuf=cbuf: e.dma_start(out=cbuf[:, i, :], in_=cs_d[i, :, ts]) for i in range(2)],
                  writes=[Tcb], sem="cs%d" % (tt % 2))
            obuf, Tobuf = ob[tt % 2], Tob[tt % 2]
            def head_body(h, tt=tt, ts=ts, hbuf=hbuf, Th=Th, cbuf=cbuf, Tcb=Tcb, obuf=obuf, Tobuf=Tobuf):
                def proj_fm(col0, banks):
                    b = C.bank("pj", banks)
                    mm_group(C, b, [(w[h][:, k, col0:col0 + 128], hbuf[:, k, :]) for k in range(KC)], reads=[Tw[h], Th])
                    return b

                def rotary(col0, dst_bf, Tdst, want_f32):
                    b0 = proj_fm(col0, (0, 1, 2, 3))
                    b1 = proj_fm(col0 + 128, (0, 1, 2, 3))
                    P.op("act", lambda e: e.copy(out=raw[:, 0, :], in_=ps[:, b0, :]), reads=[C.Tps[b0]], writes=[Traw])
                    P.op("act", lambda e: e.copy(out=raw[:, 1, :], in_=ps[:, b1, :]), reads=[C.Tps[b1]], writes=[Traw])
                    cos, sin = cbuf[:, 0, :], cbuf[:, 1, :]
                    P.op("dve", lambda e: e.tensor_tensor(out=t1[:], in0=raw[:, 0, :], in1=cos, op=ALU.mult), reads=[Traw, Tcb], writes=[Tt1])
                    P.op("pool", lambda e: e.tensor_tensor(out=t2[:], in0=raw[:, 1, :], in1=sin, op=ALU.mult), reads=[Traw, Tcb], writes=[Tt2])
                    P.op("dve", lambda e: e.tensor_tensor(out=rot[:, 0, :], in0=t1[:], in1=t2[:], op=ALU.subtract), reads=[Tt1, Tt2], writes=[Trot])
                    P.op("dve", lambda e: e.tensor_tensor(out=t1[:], in0=raw[:, 0, :], in1=sin, op=ALU.mult), reads=[Traw, Tcb], writes=[Tt1])
                    P.op("pool", lambda e: e.tensor_tensor(out=t2[:], in0=raw[:, 1, :], in1=cos, op=ALU.mult), reads=[Traw, Tcb], writes=[Tt2])
                    P.op("dve", lambda e: e.tensor_tensor(out=rot[:, 1, :], in0=t1[:], in1=t2[:], op=ALU.add), reads=[Tt1, Tt2], writes=[Trot])
                    P.op("act", lambda e: e.copy(out=dst_bf[:], in_=rot[:]), reads=[Trot], writes=[Tdst])

                rotary(0, qb, Tqb, False)
                for dc in range(2):
                    P.op("pool", lambda e, dc=dc: e.tensor_tensor(
                        out=qd[:, dc, :].rearrange("p (c i) -> p c i", i=128), in0=rot[:, dc, :].rearrange("p (c i) -> p c i", i=128),
                        in1=gq[:, h:h + 1, :].to_broadcast([128, 4, 128]), op=ALU.mult), reads=[Trot, Tc], writes=[Tqd])
                rotary(256, kb, Tkb, True)
                for ci in range(4):
                    for dc in range(2):
                        def tr(e, ci=ci, dc=dc):
                            return e.transpose(out=ps[:, 4, dc * 128:(dc + 1) * 128], in_=rot[:, dc, ci * 128:(ci + 1) * 128], identity=ident[:])
                        P.op("pe", tr, reads=[Trot, Tc], writes=[C.Tps[4]])
                    P.op("act", lambda e, ci=ci: e.activation(out=kdt[:, ci, :], in_=ps[:, 4, 0:256], func=AF.Copy, scale=kdcd[:, h:h + 1]),
                         reads=[C.Tps[4], Tc], writes=[Tkdt])
                for ci in range(4):
                    b = C.bank("pj", (0, 1, 2, 3))
                    mm_group(C, b, [(hbuf[:, k, ci * 128:(ci + 1) * 128], w[h][:, k, 512:1024]) for k in range(KC)], reads=[Tw[h], Th])
                    P.op("act", lambda e, ci=ci, b=b: e.copy(out=vt[:, ci, :], in_=ps[:, b, :]), reads=[C.Tps[b]], writes=[Tvt])
                for vc in range(4):
                    b = proj_fm(1024 + vc * 128, (0, 1, 2, 3))
                    P.op("act", lambda e, vc=vc, b=b: e.activation(out=sg[:, vc, :], in_=ps[:, b, :], func=AF.Silu), reads=[C.Tps[b]], writes=[Tsg])
                for ci in range(4):
                    cs_ = slice(ci * 128, (ci + 1) * 128)
                    first = (tt == 0 and ci == 0)
                    mm_group(C, 4, [(kb[:, dc, cs_], qb[:, dc, cs_]) for dc in range(2)], reads=[Tkb, Tqb], n=128)
                    P.op("dve", lambda e: e.tensor_tensor(out=at[:], in0=ps[:, 4, 0:128], in1=dmat[:, h, :], op=ALU.mult),
                         reads=[C.Tps[4], Tc], writes=[Tat])

                    def omm(e, ci=ci, cs_=cs_, first=first):
                        ins = None
                        for vc in range(4):
                            vs = slice(vc * 128, (vc + 1) * 128)
                            ins = e.matmul(ps[:, 5, vs], lhsT=vt[:, ci, vs], rhs=at[:], start=True, stop=first)
                            if not first:
                                for dc in range(2):
                                    ins = e.matmul(ps[:, 5, vs], lhsT=stb[h][:, dc, vs], rhs=qd[:, dc, cs_], start=False, stop=(dc == 1))
                        return ins
                    P.op("pe", omm, reads=[Tvt, Tat, Tstb[h], Tqd], writes=[C.Tps[5]])
                    P.op("act", lambda e, cs_=cs_: e.copy(out=of[:, :, cs_], in_=ps[:, 5, :].rearrange("p (v i) -> p v i", i=128)),
                         reads=[C.Tps[5]], writes=[Tof[0][0]])
                    for dc in range(2):
                        b = 6 + dc
                        mm_group(C, b, [(kdt[:, ci, dc * 128:(dc + 1) * 128], vt[:, ci, :])], reads=[Tkdt, Tvt])
                        if first:
                            P.op("dve", lambda e, dc=dc, b=b: e.tensor_copy(out=state[h][:, dc, :], in_=ps[:, b, :]), reads=[C.Tps[b]], writes=[Tst[h]])
                        else:
                            P.op("dve", lambda e, dc=dc, b=b: e.scalar_tensor_tensor(
                                out=state[h][:, dc, :], in0=state[h][:, dc, :], scalar=kdcd[:, 2 + h:3 + h], in1=ps[:, b, :],
                                op0=ALU.mult, op1=ALU.add), reads=[C.Tps[b], Tst[h], Tc], writes=[Tst[h]])
                    P.op("pool", lambda e: e.tensor_copy(out=stb[h][:], in_=state[h][:]), reads=[Tst[h]], writes=[Tstb[h]])
                for vc in range(4):
                    P.op("act", lambda e, vc=vc: e.activation(out=sq[:, vc, :], in_=of[:, vc, :], func=AF.Square), reads=[Tof[0][0]], writes=[Tsq[vc]])
                b = C.bank("pj", (0, 1, 2, 3))
                mm_group(C, b, [(C.ones[:], sq[:, vc, :]) for vc in range(4)], reads=[C.Tones] + Tsq)
                P.op("act", lambda e, b=b: e.activation(out=rstd[:], in_=ps[:, b, :], func=AF.Sqrt, scale=1.0 / RDV, bias=C.eps[:]),
                     reads=[C.Tps[b], C.Tones], writes=[Trstd])
                P.op("dve", lambda e: e.reciprocal(out=rstd[:], in_=rstd[:]), reads=[Trstd], writes=[Trstd])
                for vc in range(4):
                    P.op("dve", lambda e, vc=vc: e.tensor_tensor(out=of[:, vc, :], in0=of[:, vc, :], in1=rstd[:], op=ALU.mult),
                         reads=[Tof[0][0], Trstd], writes=[Tof[0][0]])
                    P.op("pool", lambda e, vc=vc: e.tensor_tensor(out=obuf[:, h * 4 + vc, :], in0=of[:, vc, :], in1=sg[:, vc, :], op=ALU.mult),
                         reads=[Tof[0][0], Tsg], writes=[Tobuf])
            for h in range(2):
                head_body(h)
            out_toks.append(P.dma("sp", [lambda e, c=c, ts=ts, obuf=obuf: e.dma_start(out=oT[c0 + c, :, ts], in_=obuf[:, c, :]) for c in range(8)],
                                  reads=[Tobuf], sem="o%d" % (tt % 2)))
    return out_toks


TL = 1919
TU = 1792
MASKNEG = -30000.0


def rel_bucket_np(n):
    n = np.maximum(n, 0)
    nf = np.maximum(n, 1).astype(np.float32)
    large = 16 + (np.log(nf / np.float32(16)) / np.float32(math.log(1024 / 16)) * np.float32(16)).astype(np.int32)
    large = np.minimum(large, 31)
    return np.where(n < 16, n, large)


def attn_consts():
    dist = np.arange(TL) - 511
    oh = np.zeros((33, TL), np.float32)
    bk = rel_bucket_np(dist)
    for j in range(TL):
        if dist[j] < 0:
            oh[32, j] = 1.0
        else:
            oh[bk[j], j] = 1.0
    cneg = np.zeros((16, 16), np.float32)
    negown = np.full((16, 16), MASKNEG, np.float32)
    for b in range(16):
        cneg[b, b:] = -2e30
        negown[b, b] = 0.0
    e16 = np.zeros((16, 16, 128), np.float32)
    for n in range(16):
        e16[n, n, :] = 1.0
    return oh, np.broadcast_to(cneg[None], (128, 16, 16)).copy(), np.broadcast_to(negown[None], (128, 16, 16)).copy(), e16


def emit_attn(C, kind, lam_init, h_src, win, r, rb_full, qkn_d, oh_d, ident_d, cneg_d, negown_d, e16_d, lam_d, sub_d, Rd, o_dst, c0,
              nqt=S // 512):
    moba = (kind == "moba")
    if True:
        P = C.P
        ps = C.ps
        hT = h_src
        oT = o_dst
        win_segs = [(0, r * 512), (512, 1024 + r * 512), (1024, 2048 + r * 512)]
        rb_d = rb_full[:, r * 8:(r + 1) * 8]
        w = C.sb("w", [128, KC, 1536], BF16); Tw = T("w")
        P.dma("pool", [lambda e, d0=d0, s0=s0: e.dma_start(out=w[:, :, d0:d0 + 512], in_=win[:, s0:s0 + 512].rearrange("(k p) n -> p k n", p=128))
                       for d0, s0 in win_segs], writes=[Tw], sem="w")
        rbx = C.sb("rbx", [33, 8], F32)
        qkn = C.sb("qkn_s", [128, 2], F32)
        oh = C.sb("oh_s", [33, TL], F32)
        ident = C.sb("ident_s", [128, 128], F32)
        Tc = T("consts")
        fns = [lambda e: e.dma_start(out=rbx[0:32, :], in_=rb_d), lambda e: e.dma_start(out=qkn[:], in_=qkn_d),
               lambda e: e.dma_start(out=oh[:], in_=oh_d), lambda e: e.dma_start(out=ident[:], in_=ident_d)]
        if moba:
            cneg = C.sb("cneg_s", [128, 16, 16], F32)
            negown = C.sb("negown_s", [128, 16, 16], F32)
            fns += [lambda e: e.dma_start(out=cneg[:], in_=cneg_d), lambda e: e.dma_start(out=negown[:], in_=negown_d)]
        else:
            lam = C.sb("lam_s", [1, 256], F32)
            subw = C.sb("subw_s", [128, 1], F32)
            fns += [lambda e: e.dma_start(out=lam[:], in_=lam_d), lambda e: e.dma_start(out=subw[:], in_=sub_d)]
        P.dma("sp", fns, writes=[Tc], sem="c")
        if moba:
            e16 = C.sb("e16_s", [16, 16, 128], BF16)
            Te16 = T("e16")
            P.dma("pool", lambda e: e.dma_start(out=e16[:], in_=e16_d), writes=[Te16], sem="e16")
        P.op("pool", lambda e: e.memset(rbx[32:33, :], MASKNEG), reads=[Tc], writes=[Tc])
        P.op("dve", lambda e: e.tensor_scalar(out=qkn[:, 0:1], in0=qkn[:, 0:1], scalar1=0.125, scalar2=None, op0=ALU.mult), reads=[Tc], writes=[Tc])
        ones33 = C.sb("ones33", [33, 128], F32)
        onesf = C.sb("onesf", [1, 128], F32)
        bones = C.sb("bones", [128, 128], BF16)
        Tk = T("kconst")
        P.op("pool", lambda e: e.memset(ones33[:], 1.0), writes=[Tk])
        P.op("pool", lambda e: e.memset(onesf[:], 1.0), writes=[Tk])
        P.op("pool", lambda e: e.memset(bones[:], 0.0), writes=[Tk])
        P.op("pool", lambda e: e.memset(bones[0:64, 0:64], 1.0), writes=[Tk])
        P.op("pool", lambda e: e.memset(bones[64:128, 64:128], 1.0), writes=[Tk])
        c31 = C.sb("c31", [128, 8], F32); Tc31 = T("c31")
        brep = C.sb("brep", [33, 128], F32); Tbrep = T("brep")
        rsb = C.sb("rsb", [128, TL], F32); Trsb = T("rsb")
        TRd = [T("Rd%d" % h) for h in range(8)]

        def build_strip(hh):
            P.op("dve", lambda e: e.tensor_scalar(out=brep[:], in0=ones33[:], scalar1=rbx[:, hh:hh + 1], scalar2=None, op0=ALU.mult),
                 reads=[Tc, Tk], writes=[Tbrep])
            for cb in range(4):
                c0, c1 = cb * 512, min(TL, (cb + 1) * 512)
                b = C.bank("pj", (0, 1, 2, 3))

                def mmf(e, c0=c0, c1=c1, b=b):
                    return e.matmul(ps[:, b, 0:c1 - c0], lhsT=brep[:], rhs=oh[:, c0:c1], start=True, stop=True)
                P.op("pe", mmf, reads=[Tbrep, Tc], writes=[C.Tps[b]])
                P.op("act", lambda e, c0=c0, c1=c1, b=b: e.copy(out=rsb[:, c0:c1], in_=ps[:, b, 0:c1 - c0]), reads=[C.Tps[b]], writes=[Trsb])
            P.op("dve", lambda e: e.tensor_copy(out=c31[:, hh:hh + 1], in_=rsb[:, TL - 1:TL]), reads=[Trsb], writes=[Tc31])
            P.dma("sp", lambda e: e.dma_start(out=Rd.ap()[hh], in_=rsb[:]), reads=[Trsb], writes=[TRd[hh]], sem="rd")
        for hh in range(8):
            build_strip(hh)

        if not moba:
            lp = C.sb("lp", [1, 128], F32); l2 = C.sb("l2", [1, 2], F32); nlam = C.sb("nlam", [128, 1], F32); Tl = T("lam")
            P.op("dve", lambda e: e.tensor_tensor(out=lp[:, 0:64], in0=lam[:, 0:64], in1=lam[:, 64:128], op=ALU.mult), reads=[Tc], writes=[Tl])
            P.op("dve", lambda e: e.tensor_tensor(out=lp[:, 64:128], in0=lam[:, 128:192], in1=lam[:, 192:256], op=ALU.mult), reads=[Tc, Tl], writes=[Tl])
            P.op("dve", lambda e: e.reduce_sum(out=l2[:], in_=lp[:].rearrange("p (a b) -> p a b", a=2), axis=AX.X), reads=[Tl], writes=[Tl])
            P.op("act", lambda e: e.activation(out=l2[:], in_=l2[:], func=AF.Exp), reads=[Tl], writes=[Tl])
            P.op("dve", lambda e: e.tensor_tensor(out=lp[:, 0:1], in0=l2[:, 1:2], in1=l2[:, 0:1], op=ALU.subtract), reads=[Tl], writes=[Tl])
            P.op("dve", lambda e: e.tensor_scalar(out=lp[:, 0:1], in0=lp[:, 0:1], scalar1=-float(lam_init), scalar2=None, op0=ALU.add), reads=[Tl], writes=[Tl])

            def mml(e):
                return e.matmul(ps[:, 3, 0:1], lhsT=onesf[:], rhs=lp[:, 0:1], start=True, stop=True)
            P.op("pe", mml, reads=[Tl, Tk], writes=[C.Tps[3]])
            P.op("act", lambda e: e.copy(out=nlam[:], in_=ps[:, 3, 0:1]), reads=[C.Tps[3]], writes=[Tl])
            P.op("dve", lambda e: e.tensor_scalar(out=subw[:], in0=subw[:], scalar1=float(1.0 - lam_init), scalar2=None, op0=ALU.mult), reads=[Tc], writes=[Tc])

        hb = [C.sb("hb%d" % i, [128, KC, 512], BF16) for i in range(2)]
        Thb = [T("hb%d" % i) for i in range(2)]
        qT = C.sb("qT", [128, S], BF16); TqT = T("qT")
        kT = C.sb("kT", [128, S], BF16); TkT = T("kT")
        vt = C.sb("vt", [128, 32, 512], BF16); Tvt = T("vt")
        sq = C.sb("sq", [128, 512], BF16); Tsq = T("sq")
        rstd = C.sb("rstd", [128, 512], F32); Trstd = T("rstd")
        tsk = [C.sb("tsk%d" % j, [128, TU], F32) for j in range(2)]
        Ttsk = [T("tsk%d" % j) for j in range(2)]
        pt = [C.sb("pt%d" % i, [128, 512], BF16) for i in range(3)]
        Tpt = [T("pt%d" % i) for i in range(3)]
        tmp = [C.sb("tmp%d" % i, [128, 512], F32) for i in range(2)]
        Ttmp = [T("tmp%d" % i) for i in range(2)]
        ob = [C.sb("ob%d" % i, [128, 512], BF16) for i in range(2)]
        Tob = [T("ob%d" % i) for i in range(2)]
        if moba:
            qF = C.sb("qF", [128, S], F32); TqF = T("qF")
            kF = C.sb("kF", [128, S], F32); TkF = T("kF")
            km = C.sb("km", [128, 16], F32); Tkm = T("km")
            gm = C.sb("gm", [128, 16], F32); Tgm = T("gm")
            top8 = C.sb("top8", [128, 8], F32); Ttop = T("top8")
            mneg = C.sb("mneg", [128, 16], F32); Tmn = T("mneg")
            mT = [C.sb("mT%d" % j, [16, S], BF16) for j in range(2)]
            TmT = [T("mT%d" % j) for j in range(2)]
        else:
            r0 = C.sb("r0", [128, 512], F32); Tr0 = T("r0")
            a0 = C.sb("a0", [128, 512], F32); Ta0 = T("a0")
            a1 = C.sb("a1", [128, 512], F32); Ta1 = T("a1")
        out_toks = []
        cnt = {"h": 0, "pt": 0, "tmp": 0, "ob": 0}

        def qk_norm(b, wcol, dst_main, Tdst, ts, dstF=None, TdstF=None):
            P.op("act", lambda e: e.activation(out=sq[:], in_=ps[:, b, :], func=AF.Square), reads=[C.Tps[b]], writes=[Tsq])
            b2 = C.bank("nrm", (4, 5))
            mm_group(C, b2, [(bones[:], sq[:])], reads=[Tk, Tsq])
            P.op("act", lambda e: e.activation(out=rstd[:], in_=ps[:, b2, :], func=AF.Sqrt, scale=1.0 / 64, bias=C.eps[:]),
                 reads=[C.Tps[b2], C.Tones], writes=[Trstd])
            P.op("dve", lambda e: e.reciprocal(out=rstd[:], in_=rstd[:]), reads=[Trstd], writes=[Trstd])
            if dstF is not None:
                P.op("dve", lambda e: e.scalar_tensor_tensor(out=dstF[:, ts], in0=ps[:, b, :], scalar=qkn[:, wcol:wcol + 1], in1=rstd[:],
                                                             op0=ALU.mult, op1=ALU.mult), reads=[C.Tps[b], Tc, Trstd], writes=[TdstF])
                P.op("pool", lambda e: e.tensor_copy(out=dst_main[:, ts], in_=dstF[:, ts]), reads=[TdstF], writes=[Tdst])
            else:
                P.op("dve", lambda e: e.scalar_tensor_tensor(out=dst_main[:, ts], in0=ps[:, b, :], scalar=qkn[:, wcol:wcol + 1], in1=rstd[:],
                                                             op0=ALU.mult, op1=ALU.mult), reads=[C.Tps[b], Tc, Trstd], writes=[Tdst])

        def project(p, tt):
            ts = slice(tt * 512, (tt + 1) * 512)
            i = cnt["h"] % 2
            cnt["h"] += 1
            hbuf, Th = hb[i], Thb[i]
            P.dma("sp", [lambda e, k=k: e.dma_start(out=hbuf[:, k, :], in_=hT[k, :, ts]) for k in range(KC)], writes=[Th], sem="h%d" % i)
            for which, (dst, Td, dF, TdF) in enumerate([(qT, TqT, qF if moba else None, TqF if moba else None),
                                                        (kT, TkT, kF if moba else None, TkF if moba else None)]):
                b = C.bank("pj", (0, 1, 2, 3))
                col0 = which * 512 + p * 128
                mm_group(C, b, [(w[:, k, col0:col0 + 128], hbuf[:, k, :]) for k in range(KC)], reads=[Tw, Th])
                qk_norm(b, which, dst, Td, ts, dF, TdF)
            if p == 0:
                for ci in range(4):
                    b = C.bank("pj", (0, 1, 2, 3))
                    mm_group(C, b, [(hbuf[:, k, ci * 128:(ci + 1) * 128], w[:, k, 1024:1536]) for k in range(KC)], reads=[Tw, Th])
                    P.op("act", lambda e, ci=ci, b=b: e.copy(out=vt[:, tt * 4 + ci, :], in_=ps[:, b, :]), reads=[C.Tps[b]], writes=[Tvt])

        def gate_tile(j, g):
            hp = slice(64 * j, 64 * j + 64)
            gs = slice(g * 128, (g + 1) * 128)
            blk = g // 2

            def mg(e):
                return e.matmul(ps[:, 6, 0:16], lhsT=qF[hp, gs], rhs=km[hp, :], start=True, stop=True)
            P.op("pe", mg, reads=[TqF, Tkm], writes=[C.Tps[6]])
            P.op("dve", lambda e: e.tensor_tensor(out=gm[:], in0=ps[:, 6, 0:16], in1=cneg[:, blk, :], op=ALU.add), reads=[C.Tps[6], Tc], writes=[Tgm])
            P.op("dve", lambda e: e.max(out=top8[:], in_=gm[:]), reads=[Tgm], writes=[Ttop])
            P.op("dve", lambda e: e.tensor_scalar(out=top8[:, 2:3], in0=top8[:, 2:3], scalar1=-1e30, scalar2=None, op0=ALU.max), reads=[Ttop], writes=[Ttop])
            P.op("dve", lambda e: e.scalar_tensor_tensor(out=mneg[:], in0=gm[:], scalar=top8[:, 2:3], in1=negown[:, blk, :], op0=ALU.is_lt, op1=ALU.mult),
                 reads=[Tgm, Ttop, Tc], writes=[Tmn])

            def tr(e):
                return e.transpose(out=ps[0:16, 7, 0:128], in_=mneg[:], identity=ident[:])
            P.op("pe", tr, reads=[Tmn, Tc], writes=[C.Tps[7]])
            P.op("act", lambda e: e.copy(out=mT[j][:, gs], in_=ps[0:16, 7, 0:128]), reads=[C.Tps[7]], writes=[TmT[j]])

        def attn_tile(p, j, qt, kt, bo, bl, first, last):
            hp = slice(64 * j, 64 * j + 64)
            qs = slice(qt * 512, (qt + 1) * 512)
            ks = slice(kt * 128, (kt + 1) * 128)
            delta = qt * 512 - kt * 128
            bs = C.bank("s", (0, 1, 2))

            def ms(e):
                ins = e.matmul(ps[:, bs, :], lhsT=kT[hp, ks], rhs=qT[hp, qs], start=True, stop=not moba)
                if moba:
                    ins = e.matmul(ps[:, bs, :], lhsT=e16[:, kt // 2, :], rhs=mT[j][:, qs], start=False, stop=True)
                return ins
            P.op("pe", ms, reads=[TkT, TqT] + ([Te16, TmT[j]] if moba else []), writes=[C.Tps[bs]])
            ip = cnt["pt"] % 3
            cnt["pt"] += 1
            if delta >= 1024:
                P.op("act", lambda e: e.activation(out=pt[ip][:], in_=ps[:, bs, :], func=AF.Exp, bias=c31[:, 2 * p + j:2 * p + j + 1]),
                     reads=[C.Tps[bs], Tc31], writes=[Tpt[ip]])
            else:
                it = cnt["tmp"] % 2
                cnt["tmp"] += 1
                P.op("dve", lambda e: e.tensor_tensor(out=tmp[it][:], in0=ps[:, bs, :], in1=tsk[j][:, delta + 384:delta + 384 + 512], op=ALU.add),
                     reads=[C.Tps[bs], Ttsk[j]], writes=[Ttmp[it]])
                P.op("act", lambda e: e.activation(out=pt[ip][:], in_=tmp[it][:], func=AF.Exp), reads=[Ttmp[it]], writes=[Tpt[ip]])

            def mpv(e):
                if moba:
                    e.matmul(ps[hp, bo, :], lhsT=vt[:, kt, p * 128 + j * 64:p * 128 + j * 64 + 64], rhs=pt[ip][:], start=first, stop=last)
                    return e.matmul(ps[hp, bl, :], lhsT=C.ones[:, 0:64], rhs=pt[ip][:], start=first, stop=last)
                e.matmul(ps[:, bo, :], lhsT=vt[:, kt, p * 128:(p + 1) * 128], rhs=pt[ip][:], start=first, stop=last)
                return e.matmul(ps[:, bl, :], lhsT=C.ones[:], rhs=pt[ip][:], start=first, stop=last)
            P.op("pe", mpv, reads=[Tvt, Tpt[ip], C.Tones], writes=[C.Tps[bo], C.Tps[bl]])

        def finish_qt(p, qt):
            qs = slice(qt * 512, (qt + 1) * 512)
            io = cnt["ob"] % 2
            cnt["ob"] += 1
            if moba:
                P.op("dve", lambda e: e.reciprocal(out=tmp[0][:], in_=ps[:, 5, :]), reads=[C.Tps[5]], writes=[Ttmp[0]])
                P.op("dve", lambda e: e.tensor_tensor(out=ob[io][:], in0=ps[:, 4, :], in1=tmp[0][:], op=ALU.mult), reads=[C.Tps[4], Ttmp[0]], writes=[Tob[io]])
            else:
                P.op("dve", lambda e: e.reciprocal(out=r0[:], in_=ps[:, 5, :]), reads=[C.Tps[5]], writes=[Tr0])
                P.op("dve", lambda e: e.tensor_tensor(out=a0[:], in0=ps[:, 4, :], in1=r0[:], op=ALU.mult), reads=[C.Tps[4], Tr0], writes=[Ta0])
                P.op("dve", lambda e: e.reciprocal(out=r0[:], in_=ps[:, 7, :]), reads=[C.Tps[7]], writes=[Tr0])
                P.op("dve", lambda e: e.tensor_tensor(out=a1[:], in0=ps[:, 6, :], in1=r0[:], op=ALU.mult), reads=[C.Tps[6], Tr0], writes=[Ta1])
                P.op("dve", lambda e: e.scalar_tensor_tensor(out=a0[:], in0=a1[:], scalar=nlam[:], in1=a0[:], op0=ALU.mult, op1=ALU.add),
                     reads=[Ta0, Ta1, Tl], writes=[Ta0])
                P.op("act", lambda e: e.activation(out=sq[:], in_=a0[:], func=AF.Square), reads=[Ta0], writes=[Tsq])
                mm_group(C, 3, [(C.ones[:], sq[:])], reads=[C.Tones, Tsq])
                P.op("act", lambda e: e.activation(out=rstd[:], in_=ps[:, 3, :], func=AF.Sqrt, scale=1.0 / 128, bias=C.eps[:]),
                     reads=[C.Tps[3], C.Tones], writes=[Trstd])
                P.op("dve", lambda e: e.reciprocal(out=rstd[:], in_=rstd[:]), reads=[Trstd], writes=[Trstd])
                P.op("dve", lambda e: e.scalar_tensor_tensor(out=ob[io][:], in0=a0[:], scalar=subw[:], in1=rstd[:], op0=ALU.mult, op1=ALU.mult),
                     reads=[Ta0, Tc, Trstd], writes=[Tob[io]])
            out_toks.append(P.dma("sp", lambda e: e.dma_start(out=oT[c0 + p, :, qs], in_=ob[io][:]), reads=[Tob[io]], sem="o%d" % io))

        def load_tsk(p, j):
            hh = 2 * p + j
            src = bass.AP(Rd, hh * 128 * TL + 127, [[TL - 1, 128], [1, TU]])
            P.dma("sp", lambda e: e.dma_start(out=tsk[j][:], in_=src), reads=[TRd[hh]], writes=[Ttsk[j]], sem="tsk%d" % j)

        for p in range(4):
            for tt in range(8):
                project(p, tt)
            for j in range(2):
                load_tsk(p, j)
            if moba:
                P.op("dve", lambda e: e.reduce_sum(out=km[:], in_=kF[:].rearrange("p (n t) -> p n t", t=256), axis=AX.X), reads=[TkF], writes=[Tkm])
                for j in range(2):
                    for g in range(4 * nqt):
                        gate_tile(j, g)
            for qt in range(nqt):
                nk = 4 * qt + 4
                if moba:
                    for kt in range(nk):
                        for j in range(2):
                            attn_tile(p, j, qt, kt, 4, 5, kt == 0, kt == nk - 1)
                else:
                    for j in range(2):
                        for kt in range(nk):
                            attn_tile(p, j, qt, kt, 4 + 2 * j, 5 + 2 * j, kt == 0, kt == nk - 1)
                finish_qt(p, qt)
    return out_toks


def build_fused():
    nc = bass.Bass("TRN2", target_bir_lowering=False)

    def inp(name, shape, dt=F32):
        return nc.dram_tensor(name, shape, dt, kind="ExternalInput").ap()
    xT = inp("xT", [KC, 128, S])
    n1 = inp("n1", [4, 128, KC]); n2 = inp("n2", [4, 128, KC])
    wup = inp("wup", [4, D, DFF]); wdown = inp("wdown", [4, DFF, D])
    retin = inp("retin", [2, D, 6144]); retout = inp("retout", [2, 2048, D])
    mobain = inp("mobain", [D, 3072]); mobaout = inp("mobaout", [D, D])
    diffin = inp("diffin", [D, 3072]); diffout = inp("diffout", [D, D])
    rb = inp("rb", [32, 16]); qknm = inp("qknm", [128, 2]); qknd = inp("qknd", [128, 2])
    lam = inp("lam", [1, 256]); subw = inp("subw", [128, 1])
    cs_d = inp("cs", [2, 128, S]); dmat_d = inp("dmat", [4, 128, 128]); gq_d = inp("gq", [4, 128, 128]); kdcd_d = inp("kdcd", [128, 8])
    ident_d = inp("ident", [128, 128]); oh_d = inp("oh", [33, TL])
    cneg_d = inp("cneg", [128, 16, 16]); negown_d = inp("negown", [128, 16, 16]); e16_d = inp("e16", [16, 16, 128])
    xs = nc.dram_tensor("xs", [KC, 128, S], F32, kind="Internal").ap()
    hs = nc.dram_tensor("hs", [KC, 128, S], BF16, kind="Internal").ap()
    osc = nc.dram_tensor("osc", [16, 128, S], BF16, kind="Internal").ap()
    Rd = nc.dram_tensor("Rscr", [8, 128, TL], F32, kind="Internal")
    xo = nc.dram_tensor("xo", [KC, 128, S], F32, kind="ExternalOutput").ap()

    with ExitStack() as st:
        C = Ctx(nc, st)
        P = C.P

        def phase(fn):
            P.barrier()
            with ExitStack() as ph:
                C.ph = ph
                C.bank_rr = {}
                fn()
            C.ph = None
        phase(lambda: emit_phase_a(C, S, xT, None, n1=n1[0], h_dst=hs))
        for i in range(4):
            kind, j = i % 3, i // 3
            for r in range(2):
                if kind == 0:
                    phase(lambda: emit_ret(C, hs, retin[j], (2 * r, 2 * r + 1), cs_d, dmat_d, gq_d, kdcd_d, ident_d, osc, 8 * r))
                elif kind == 1:
                    phase(lambda: emit_attn(C, "moba", 0.0, hs, mobain, r, rb, qknm, oh_d, ident_d, cneg_d, negown_d, e16_d, None, None, Rd, osc, 4 * r))
                else:
                    lam_init = 0.8 - 0.6 * math.exp(-0.3 * i)
                    phase(lambda: emit_attn(C, "diff", lam_init, hs, diffin, r, rb, qknd, oh_d, ident_d, None, None, None, lam, subw, Rd, osc, 4 * r))
            wout, fo = ((retout[j], 2048) if kind == 0 else ((mobaout, 1024) if kind == 1 else (diffout, 1024)))
            last = (i == 3)
            phase(lambda: emit_phase_a(C, S, xT if i == 0 else xs, xo if last else xs, o_src=osc, wout=wout, fo=fo, wup=wup[i], wdown=wdown[i],
                                       n2=n2[i], n1=None if last else n1[i + 1], h_dst=None if last else hs))
        P.barrier()
        P.emit(st)
    return nc


_NC_CACHE = {}


def _fm(a):
    return np.ascontiguousarray(a.T.reshape(a.shape[1] // 128, 128, a.shape[0]))


def _unfm(a):
    return np.ascontiguousarray(a.reshape(-1, a.shape[2]).T)


def _nw(w):
    return np.ascontiguousarray(w.reshape(w.shape[0], KC, 128).transpose(0, 2, 1))


def kernel(x, rel_bias, norm1, norm2, w_up, w_down, ret_w_in, ret_w_out,
           moba_w_in, moba_q_norm, moba_k_norm, moba_w_out,
           diff_w_in, diff_q_norm, diff_k_norm, diff_lambda, diff_subln, diff_w_out):
    f32 = np.float32
    A = lambda a: np.ascontiguousarray(np.asarray(a, f32))
    x = A(x)
    if "fused" not in _NC_CACHE:
        _NC_CACHE["fused"] = build_fused()
    nc = _NC_CACHE["fused"]
    cs, ph = ret_consts()
    oh, cneg, negown, e16 = attn_consts()
    shared = {
        "n1": _nw(A(norm1)), "n2": _nw(A(norm2)), "wup": A(w_up), "wdown": A(w_down),
        "retin": A(ret_w_in), "retout": A(ret_w_out), "mobain": A(moba_w_in[0]), "mobaout": A(moba_w_out[0]),
        "diffin": A(diff_w_in[0]), "diffout": A(diff_w_out[0]), "rb": A(rel_bias),
        "qknm": A(np.stack([np.tile(A(moba_q_norm[0]), 2), np.tile(A(moba_k_norm[0]), 2)], axis=1)),
        "qknd": A(np.stack([np.tile(A(diff_q_norm[0]), 2), np.tile(A(diff_k_norm[0]), 2)], axis=1)),
        "lam": A(diff_lambda[0]).reshape(1, 256), "subw": A(diff_subln[0]).reshape(128, 1),
        "cs": cs, "dmat": A(np.stack([ph[h][0] for h in range(4)])), "gq": A(np.stack([ph[h][1] for h in range(4)])),
        "kdcd": A(np.stack([ph[h][2] for h in range(4)] + [ph[h][3] for h in range(4)], axis=1)),
        "ident": np.eye(128, dtype=f32), "oh": oh, "cneg": cneg, "negown": negown, "e16": e16,
    }
    ims = []
    for c in range(8):
        d = dict(shared)
        d["xT"] = _fm(x[c % 4])
        ims.append(d)
    res = run_bass_kernel_spmd(nc, ims, core_ids=list(range(8))).results
    out = np.empty((4, S, D), f32)
    for b in range(4):
        out[b] = _unfm(res[b]["xo"])
    return out
```

```python
import math
from contextlib import ExitStack
import numpy as np
import concourse.bass as bass
import concourse.mybir as mybir
from concourse.bass_utils import run_bass_kernel_spmd

F32 = mybir.dt.float32
BF16 = mybir.dt.bfloat16
AF = mybir.ActivationFunctionType
ALU = mybir.AluOpType
AX = mybir.AxisListType

D = 1024
KC = 8
S = 4096
NTOK = 2048
DFF = 4096
EPS = 1e-6
ENGS = ("pe", "act", "dve", "pool", "sp")


class T:
    __slots__ = ("name", "w", "r")

    def __init__(self, name=""):
        self.name = name
        self.w = None
        self.r = {}


class Prog:
    def __init__(self, nc, same_engine_sync=True):
        self.nc = nc
        self.ops = {e: [] for e in ENGS}
        self.cnt = {}
        self.seen = {e: {} for e in ENGS}
        self.same = same_engine_sync
        self.dma_sems = {}

    def _deps(self, eng, reads, writes):
        deps = {}

        def add(tok):
            if tok is None:
                return
            k, v = tok
            if deps.get(k, 0) < v:
                deps[k] = v
        for t in reads:
            add(t.w)
        for t in writes:
            add(t.w)
            for r in t.r.items():
                add(r)
        waits = []
        for k, v in deps.items():
            if k == eng and (not self.same or eng == "pe"):
                continue
            if self.seen[eng].get(k, 0) >= v:
                continue
            self.seen[eng][k] = v
            waits.append((k, v))
        return waits

    def _mark(self, tok, reads, writes):
        for t in reads:
            if t.r.get(tok[0], 0) < tok[1]:
                t.r[tok[0]] = tok[1]
        for t in writes:
            t.w = tok
            t.r = {}

    def op(self, eng, fn, reads=(), writes=()):
        waits = self._deps(eng, reads, writes)
        self.cnt[eng] = self.cnt.get(eng, 0) + 1
        tok = (eng, self.cnt[eng])
        self._mark(tok, reads, writes)
        self.ops[eng].append((fn, waits, (eng, 1)))
        return tok

    def dma(self, q, fns, reads=(), writes=(), sem=None):
        if not isinstance(fns, (list, tuple)):
            fns = [fns]
        key = ("dma", sem)
        self.dma_sems[key] = None
        waits = self._deps(q, reads, writes)
        self.cnt[key] = self.cnt.get(key, 0) + 16 * len(fns)
        tok = (key, self.cnt[key])
        self._mark(tok, reads, writes)
        for i, fn in enumerate(fns):
            self.ops[q].append((fn, waits if i == 0 else [], (key, 16)))
        return tok

    def wait_all(self, eng, toks):
        best = {}
        for k, v in toks:
            best[k] = max(best.get(k, 0), v)
        waits = []
        for k, v in best.items():
            if self.seen[eng].get(k, 0) >= v:
                continue
            self.seen[eng][k] = v
            waits.append((k, v))
        self.ops[eng].append((None, waits, None))

    def barrier(self):
        toks = list(self.cnt.items())
        for e in ENGS:
            self.wait_all(e, toks)

    def emit(self, stack):
        nc = self.nc
        sems = {}
        for e in ENGS:
            if self.ops[e]:
                sems[e] = stack.enter_context(nc.semaphore("s_" + e))
        for k in self.dma_sems:
            sems[k] = stack.enter_context(nc.semaphore("d_%s" % (k[1],)))
        block = stack.enter_context(nc.Block())
        handles = {"pe": block.tensor, "act": block.scalar, "dve": block.vector,
                   "pool": block.gpsimd, "sp": block.sync}

        def make(e):
            def body(engine):
                for fn, waits, inc in self.ops[e]:
                    for k, v in waits:
                        engine.wait_ge(sems[k], v)
                    if fn is not None:
                        fn(engine).then_inc(sems[inc[0]], inc[1])
            return body
        for e in ENGS:
            if self.ops[e]:
                handles[e](make(e))


class Ctx:
    def __init__(self, nc, st):
        self.nc, self.st = nc, st
        self.P = Prog(nc)
        self.ps = st.enter_context(nc.psum_tensor("ps", [128, 8, 512], F32))
        self.Tps = [T("ps%d" % i) for i in range(8)]
        self.ones = st.enter_context(nc.sbuf_tensor("ones", [128, 128], BF16))
        self.Tones = T("ones")
        self.eps = st.enter_context(nc.sbuf_tensor("epsc", [128, 1], F32))
        self.P.op("pool", lambda e: e.memset(self.eps[:], EPS), writes=[self.Tones])
        self.P.op("pool", lambda e: e.memset(self.ones[:], 1.0), writes=[self.Tones])
        self.bank_rr = {}

    def sb(self, name, shape, dt):
        self.nalloc = getattr(self, "nalloc", 0) + 1
        stack = self.ph if getattr(self, "ph", None) is not None else self.st
        return stack.enter_context(self.nc.sbuf_tensor("%s_%d" % (name, self.nalloc), shape, dt))

    def bank(self, group, banks):
        i = self.bank_rr.get(group, 0)
        self.bank_rr[group] = i + 1
        return banks[i % len(banks)]


def mm_group(C, bank, mms, reads, n=512, prow=128):
    ps = C.ps

    def fn(e, mms=mms):
        ins = None
        for i, (l, r) in enumerate(mms):
            ins = e.matmul(ps[0:prow, bank, 0:n], lhsT=l, rhs=r, start=(i == 0), stop=(i == len(mms) - 1))
        return ins
    return C.P.op("pe", fn, reads=reads, writes=[C.Tps[bank]])


def rmsnorm_fm(C, x, Tx, w32, Tw, out, Tout, ntok, tmp, nd=KC, banks=(6, 7), tt_list=None, xoff=0):
    P = C.P
    sq, Tsq, rstd, Trstd = tmp
    nfeat = nd * 128
    for tt in (tt_list if tt_list is not None else range(ntok // 512)):
        ts = slice(tt * 512, (tt + 1) * 512)
        xs = slice(xoff + tt * 512, xoff + (tt + 1) * 512)
        b = C.bank("nrm", banks)
        for c in range(nd):
            P.op("act", lambda e, c=c, xs=xs: e.activation(out=sq[:, c, :], in_=x[:, c, xs], func=AF.Square),
                 reads=[Tx[c][tt]], writes=[Tsq[c]])
        mm_group(C, b, [(C.ones[:], sq[:, c, :]) for c in range(nd)], reads=[C.Tones] + Tsq[:nd])
        P.op("act", lambda e, b=b: e.activation(out=rstd[:], in_=C.ps[:, b, :], func=AF.Sqrt, scale=1.0 / nfeat, bias=C.eps[:]),
             reads=[C.Tps[b], C.Tones], writes=[Trstd])
        P.op("dve", lambda e: e.reciprocal(out=rstd[:], in_=rstd[:]), reads=[Trstd], writes=[Trstd])
        for c in range(nd):
            P.op("dve", lambda e, c=c, ts=ts, xs=xs: e.scalar_tensor_tensor(
                out=out[:, c, ts], in0=x[:, c, xs], scalar=w32[:, c:c + 1], in1=rstd[:], op0=ALU.mult, op1=ALU.mult),
                reads=[Tx[c][tt], Tw, Trstd], writes=[Tout[c][tt]])


def emit_phase_a(C, ntok, x_src, x_dst, o_src=None, wout=None, fo=0, wup=None, wdown=None, n2=None, n1=None, h_dst=None, TT=1024):
    P = C.P
    do_mix, do_ffn, do_next = o_src is not None, wup is not None, h_dst is not None
    NS = TT // 512
    out_toks = []
    x = C.sb("x", [128, KC, TT], F32)
    Tx = [[T("x") for _ in range(NS)] for c in range(KC)]
    sq = C.sb("sq", [128, KC, 512], BF16)
    Tsq = [T("sq") for c in range(KC)]
    rstd = C.sb("rstd", [128, 512], F32)
    tmpn = (sq, Tsq, rstd, T("rstd"))
    hb = C.sb("hb", [128, KC, TT], BF16)
    Thb = [[T("hb") for _ in range(NS)] for c in range(KC)]
    allx = [t for c in range(KC) for t in Tx[c]]
    allh = [t for c in range(KC) for t in Thb[c]]

    def load_w(name, src):
        w = C.sb(name, [128, KC], F32)
        Tw = T(name)
        P.dma("sp", lambda e: e.dma_start(out=w[:], in_=src), writes=[Tw], sem=name)
        return w, Tw
    if do_ffn or do_mix:
        u2 = C.sb("u2", [128, 32, TT], BF16)
        Tu2 = [T("u2") for f in range(32)]
    if do_mix:
        FK = fo // 128
        wo = [C.sb("wo", [128, FK, 128], BF16) for i in range(2)]
        Two = [T("wo") for i in range(2)]
    if do_ffn:
        w2, Tw2 = load_w("n2w", n2)
        wu = [C.sb("wu", [128, KC, 512], BF16) for i in range(2)]
        Twu = [T("wu") for i in range(2)]
        wd = [C.sb("wd", [128, 32, 256], BF16) for i in range(2)]
        Twd = [T("wd") for i in range(2)]
        rl = [C.sb("rl", [128, 512], F32) for i in range(2)]
        Trl = [T("rl") for i in range(2)]
    if do_next:
        w1, Tw1 = load_w("n1w", n1)
    cnt = {"wo": 0, "wu": 0, "wd": 0, "rl": 0}

    def tile_body(tt):
        t0 = tt * TT
        P.dma("sp", [lambda e, c=c: e.dma_start(out=x[:, c, :], in_=x_src[c, :, t0:t0 + TT]) for c in range(KC)], writes=allx, sem="xin")
        if do_mix:
            ob = u2
            P.dma("sp", [lambda e, k=k: e.dma_start(out=ob[:, k, :], in_=o_src[k, :, t0:t0 + TT]) for k in range(FK)], writes=Tu2[:FK], sem="oin")
            for n in range(KC):
                sl = cnt["wo"] % 2
                cnt["wo"] += 1
                P.dma("pool", lambda e, n=n, sl=sl: e.dma_start(
                    out=wo[sl][:], in_=wout[:, n * 128:(n + 1) * 128].rearrange("(k p) n -> p k n", p=128)),
                    writes=[Two[sl]], sem="wo%d" % sl)
                for s_ in range(NS):
                    ss = slice(s_ * 512, (s_ + 1) * 512)
                    b = C.bank("dn", (4, 5))
                    mm_group(C, b, [(wo[sl][:, k, :], ob[:, k, ss]) for k in range(FK)], reads=[Two[sl]] + Tu2[:FK])
                    P.op("dve", lambda e, n=n, ss=ss, b=b: e.tensor_tensor(out=x[:, n, ss], in0=x[:, n, ss], in1=C.ps[:, b, :], op=ALU.add),
                         reads=[C.Tps[b], Tx[n][s_]], writes=[Tx[n][s_]])
        if do_ffn:
            rmsnorm_fm(C, x, Tx, w2, Tw2, hb, Thb, TT, tmpn)
            for fg in range(8):
                sl = cnt["wu"] % 2
                cnt["wu"] += 1
                P.dma("pool", lambda e, fg=fg, sl=sl: e.dma_start(
                    out=wu[sl][:], in_=wup[:, fg * 512:(fg + 1) * 512].rearrange("(k p) f -> p k f", p=128)),
                    writes=[Twu[sl]], sem="wu%d" % sl)
                for fi in range(4):
                    f = fg * 4 + fi
                    for s_ in range(NS):
                        ss = slice(s_ * 512, (s_ + 1) * 512)
                        b = C.bank("up", (0, 1, 2, 3))
                        mm_group(C, b, [(wu[sl][:, c, fi * 128:(fi + 1) * 128], hb[:, c, ss]) for c in range(KC)],
                                 reads=[Twu[sl]] + [Thb[c][s_] for c in range(KC)])
                        r = cnt["rl"] % 2
                        cnt["rl"] += 1
                        P.op("act", lambda e, b=b, r=r: e.activation(out=rl[r][:], in_=C.ps[:, b, :], func=AF.Relu),
                             reads=[C.Tps[b]], writes=[Trl[r]])
                        P.op("dve", lambda e, f=f, r=r, ss=ss: e.tensor_tensor(out=u2[:, f, ss], in0=rl[r][:], in1=rl[r][:], op=ALU.mult),
                             reads=[Trl[r]], writes=[Tu2[f]])
            for ng in range(4):
                sl = cnt["wd"] % 2
                cnt["wd"] += 1
                P.dma("pool", lambda e, ng=ng, sl=sl: e.dma_start(
                    out=wd[sl][:], in_=wdown[:, ng * 256:(ng + 1) * 256].rearrange("(k p) n -> p k n", p=128)),
                    writes=[Twd[sl]], sem="wd%d" % sl)
                for ni in range(2):
                    n = ng * 2 + ni
                    for s_ in range(NS):
                        ss = slice(s_ * 512, (s_ + 1) * 512)
                        b = C.bank("dn", (4, 5))
                        mm_group(C, b, [(wd[sl][:, f, ni * 128:(ni + 1) * 128], u2[:, f, ss]) for f in range(32)],
                                 reads=[Twd[sl]] + Tu2)
                        P.op("dve", lambda e, n=n, ss=ss, b=b: e.tensor_tensor(out=x[:, n, ss], in0=x[:, n, ss], in1=C.ps[:, b, :], op=ALU.add),
                             reads=[C.Tps[b], Tx[n][s_]], writes=[Tx[n][s_]])
        if x_dst is not None:
            out_toks.append(P.dma("sp", [lambda e, c=c: e.dma_start(out=x_dst[c, :, t0:t0 + TT], in_=x[:, c, :]) for c in range(KC)],
                                  reads=allx, sem="xout"))
        if do_next:
            rmsnorm_fm(C, x, Tx, w1, Tw1, hb, Thb, TT, tmpn)
            out_toks.append(P.dma("sp", [lambda e, c=c: e.dma_start(out=h_dst[c, :, t0:t0 + TT], in_=hb[:, c, :]) for c in range(KC)],
                                  reads=allh, sem="hout"))
    for tt in range(ntok // TT):
        tile_body(tt)
    return out_toks


RH, RDK, RDV, RC = 4, 256, 512, 128


def ret_consts():
    inv = (10000.0 ** (-np.arange(0, RDK, 2, dtype=np.float32) / np.float32(RDK))).astype(np.float32)
    ang = (np.arange(S, dtype=np.float32)[:, None] * inv[None, :]).astype(np.float32)
    cs = np.ascontiguousarray(np.stack([np.cos(ang).T, np.sin(ang).T]).astype(np.float32))
    lg = np.log(1.0 - 2.0 ** (-5.0 - np.arange(RH, dtype=np.float64)))
    pos = np.arange(RC, dtype=np.float64)
    per_head = []
    for h in range(RH):
        rel = pos[None, :] - pos[:, None]
        dT = np.where(rel >= 0, np.exp(np.maximum(rel, 0) * lg[h]), 0.0) * RDK ** -0.5
        gq = np.broadcast_to(np.exp((pos + 1.0) * lg[h])[None, :], (128, 128))
        kd = np.exp((RC - 1.0 - pos) * lg[h]) * RDK ** -0.5
        cd = np.full(128, np.exp(RC * lg[h]))
        per_head.append((dT.astype(np.float32), gq.astype(np.float32), kd.astype(np.float32), cd.astype(np.float32)))
    return cs, per_head


def emit_ret(C, h_src, win, hsel, cs_d, dmat_d, gq_d, kdcd_d, ident_d, o_dst, c0, ntt=S // 512):
    if True:
        P = C.P
        ps = C.ps
        oT = o_dst
        hT = h_src
        w = [C.sb("w%d" % h, [128, KC, 1536], BF16) for h in range(2)]
        Tw = [T("w%d" % h) for h in range(2)]
        for h in range(2):
            g = hsel[h]
            segs = [(0, g * 256, 256), (256, 1024 + g * 256, 256), (512, 2048 + g * 512, 512), (1024, 4096 + g * 512, 512)]
            P.dma("pool", [lambda e, h=h, d0=d0, s0=s0, n=n: e.dma_start(
                out=w[h][:, :, d0:d0 + n], in_=win[:, s0:s0 + n].rearrange("(k p) n -> p k n", p=128)) for d0, s0, n in segs],
                writes=[Tw[h]], sem="w%d" % h)
        dmat = C.sb("dmat_s", [128, 2, 128], F32)
        gq = C.sb("gq_s", [128, 2, 128], F32)
        kdcd = C.sb("kdcd_s", [128, 4], F32)
        ident = C.sb("ident_s", [128, 128], F32)
        Tc = T("consts")
        P.dma("sp", [lambda e, h=h: e.dma_start(out=dmat[:, h, :], in_=dmat_d[hsel[h]]) for h in range(2)]
              + [lambda e, h=h: e.dma_start(out=gq[:, h, :], in_=gq_d[hsel[h]]) for h in range(2)]
              + [lambda e, h=h: e.dma_start(out=kdcd[:, h:h + 1], in_=kdcd_d[:, hsel[h]:hsel[h] + 1], allow_slow_non_contiguous=True) for h in range(2)]
              + [lambda e, h=h: e.dma_start(out=kdcd[:, 2 + h:3 + h], in_=kdcd_d[:, 4 + hsel[h]:5 + hsel[h]], allow_slow_non_contiguous=True) for h in range(2)]
              + [lambda e: e.dma_start(out=ident[:], in_=ident_d)],
              writes=[Tc], sem="c")

        hb = [C.sb("hb%d" % i, [128, KC, 512], BF16) for i in range(2)]
        Thb = [T("hb%d" % i) for i in range(2)]
        csb = [C.sb("cs%d" % i, [128, 2, 512], F32) for i in range(2)]
        Tcs = [T("cs%d" % i) for i in range(2)]
        raw = C.sb("raw", [128, 2, 512], F32); Traw = T("raw")
        t1 = C.sb("t1", [128, 512], F32); Tt1 = T("t1")
        t2 = C.sb("t2", [128, 512], F32); Tt2 = T("t2")
        rot = C.sb("rot", [128, 2, 512], F32); Trot = T("rot")
        qb = C.sb("qb", [128, 2, 512], BF16); Tqb = T("qb")
        qd = C.sb("qd", [128, 2, 512], BF16); Tqd = T("qd")
        kb = C.sb("kb", [128, 2, 512], BF16); Tkb = T("kb")
        kdt = C.sb("kdt", [128, 4, 256], BF16); Tkdt = T("kdt")
        vt = C.sb("vt", [128, 4, 512], BF16); Tvt = T("vt")
        sg = C.sb("sg", [128, 4, 512], F32); Tsg = T("sg")
        of = C.sb("of", [128, 4, 512], F32); Tof = [[T("of%d_%d" % (v, 0))] for v in range(4)]
        at = C.sb("at", [128, 128], BF16); Tat = T("at")
        state = [C.sb("st%d" % h, [128, 2, 512], F32) for h in range(2)]
        stb = [C.sb("stb%d" % h, [128, 2, 512], BF16) for h in range(2)]
        Tst = [T("st%d" % h) for h in range(2)]
        Tstb = [T("stb%d" % h) for h in range(2)]
        sq = C.sb("sq", [128, 4, 512], BF16); Tsq = [T("sq%d" % c) for c in range(4)]
        rstd = C.sb("rstd", [128, 512], F32); Trstd = T("rstd")
        ob = [C.sb("ob%d" % i, [128, 8, 512], BF16) for i in range(2)]
        Tob = [T("ob%d" % i) for i in range(2)]
        out_toks = []

        for tt in range(ntt):
            ts = slice(tt * 512, (tt + 1) * 512)
            hbuf, Th = hb[tt % 2], Thb[tt % 2]
            cbuf, Tcb = csb[tt % 2], Tcs[tt % 2]
            P.dma("sp", [lambda e, k=k, ts=ts, hbuf=hbuf: e.dma_start(out=hbuf[:, k, :], in_=hT[k, :, ts]) for k in range(KC)],
                  writes=[Th], sem="h%d" % (tt % 2))
            P.dma("sp", [lambda e, i=i, ts=ts, cbuf=cbuf: e.dma_start(out=cbuf[:, i, :], in_=cs_d[i, :, ts]) for i in range(2)],
                  writes=[Tcb], sem="cs%d" % (tt % 2))
            obuf, Tobuf = ob[tt % 2], Tob[tt % 2]
            def head_body(h, tt=tt, ts=ts, hbuf=hbuf, Th=Th, cbuf=cbuf, Tcb=Tcb, obuf=obuf, Tobuf=Tobuf):
                def proj_fm(col0, banks):
                    b = C.bank("pj", banks)
                    mm_group(C, b, [(w[h][:, k, col0:col0 + 128], hbuf[:, k, :]) for k in range(KC)], reads=[Tw[h], Th])
                    return b

                def rotary(col0, dst_bf, Tdst, want_f32):
                    b0 = proj_fm(col0, (0, 1, 2, 3))
                    b1 = proj_fm(col0 + 128, (0, 1, 2, 3))
                    P.op("act", lambda e: e.copy(out=raw[:, 0, :], in_=ps[:, b0, :]), reads=[C.Tps[b0]], writes=[Traw])
                    P.op("act", lambda e: e.copy(out=raw[:, 1, :], in_=ps[:, b1, :]), reads=[C.Tps[b1]], writes=[Traw])
                    cos, sin = cbuf[:, 0, :], cbuf[:, 1, :]
                    P.op("dve", lambda e: e.tensor_tensor(out=t1[:], in0=raw[:, 0, :], in1=cos, op=ALU.mult), reads=[Traw, Tcb], writes=[Tt1])
                    P.op("pool", lambda e: e.tensor_tensor(out=t2[:], in0=raw[:, 1, :], in1=sin, op=ALU.mult), reads=[Traw, Tcb], writes=[Tt2])
                    P.op("dve", lambda e: e.tensor_tensor(out=rot[:, 0, :], in0=t1[:], in1=t2[:], op=ALU.subtract), reads=[Tt1, Tt2], writes=[Trot])
                    P.op("dve", lambda e: e.tensor_tensor(out=t1[:], in0=raw[:, 0, :], in1=sin, op=ALU.mult), reads=[Traw, Tcb], writes=[Tt1])
                    P.op("pool", lambda e: e.tensor_tensor(out=t2[:], in0=raw[:, 1, :], in1=cos, op=ALU.mult), reads=[Traw, Tcb], writes=[Tt2])
                    P.op("dve", lambda e: e.tensor_tensor(out=rot[:, 1, :], in0=t1[:], in1=t2[:], op=ALU.add), reads=[Tt1, Tt2], writes=[Trot])
                    P.op("act", lambda e: e.copy(out=dst_bf[:], in_=rot[:]), reads=[Trot], writes=[Tdst])

                rotary(0, qb, Tqb, False)
                for dc in range(2):
                    P.op("pool", lambda e, dc=dc: e.tensor_tensor(
                        out=qd[:, dc, :].rearrange("p (c i) -> p c i", i=128), in0=rot[:, dc, :].rearrange("p (c i) -> p c i", i=128),
                        in1=gq[:, h:h + 1, :].to_broadcast([128, 4, 128]), op=ALU.mult), reads=[Trot, Tc], writes=[Tqd])
                rotary(256, kb, Tkb, True)
                for ci in range(4):
                    for dc in range(2):
                        def tr(e, ci=ci, dc=dc):
                            return e.transpose(out=ps[:, 4, dc * 128:(dc + 1) * 128], in_=rot[:, dc, ci * 128:(ci + 1) * 128], identity=ident[:])
                        P.op("pe", tr, reads=[Trot, Tc], writes=[C.Tps[4]])
                    P.op("act", lambda e, ci=ci: e.activation(out=kdt[:, ci, :], in_=ps[:, 4, 0:256], func=AF.Copy, scale=kdcd[:, h:h + 1]),
                         reads=[C.Tps[4], Tc], writes=[Tkdt])
                for ci in range(4):
                    b = C.bank("pj", (0, 1, 2, 3))
                    mm_group(C, b, [(hbuf[:, k, ci * 128:(ci + 1) * 128], w[h][:, k, 512:1024]) for k in range(KC)], reads=[Tw[h], Th])
                    P.op("act", lambda e, ci=ci, b=b: e.copy(out=vt[:, ci, :], in_=ps[:, b, :]), reads=[C.Tps[b]], writes=[Tvt])
                for vc in range(4):
                    b = proj_fm(1024 + vc * 128, (0, 1, 2, 3))
                    P.op("act", lambda e, vc=vc, b=b: e.activation(out=sg[:, vc, :], in_=ps[:, b, :], func=AF.Silu), reads=[C.Tps[b]], writes=[Tsg])
                for ci in range(4):
                    cs_ = slice(ci * 128, (ci + 1) * 128)
                    first = (tt == 0 and ci == 0)
                    mm_group(C, 4, [(kb[:, dc, cs_], qb[:, dc, cs_]) for dc in range(2)], reads=[Tkb, Tqb], n=128)
                    P.op("dve", lambda e: e.tensor_tensor(out=at[:], in0=ps[:, 4, 0:128], in1=dmat[:, h, :], op=ALU.mult),
                         reads=[C.Tps[4], Tc], writes=[Tat])

                    def omm(e, ci=ci, cs_=cs_, first=first):
                        ins = None
                        for vc in range(4):
                            vs = slice(vc * 128, (vc + 1) * 128)
                            ins = e.matmul(ps[:, 5, vs], lhsT=vt[:, ci, vs], rhs=at[:], start=True, stop=first)
                            if not first:
                                for dc in range(2):
                                    ins = e.matmul(ps[:, 5, vs], lhsT=stb[h][:, dc, vs], rhs=qd[:, dc, cs_], start=False, stop=(dc == 1))
                        return ins
                    P.op("pe", omm, reads=[Tvt, Tat, Tstb[h], Tqd], writes=[C.Tps[5]])
                    P.op("act", lambda e, cs_=cs_: e.copy(out=of[:, :, cs_], in_=ps[:, 5, :].rearrange("p (v i) -> p v i", i=128)),
                         reads=[C.Tps[5]], writes=[Tof[0][0]])
                    for dc in range(2):
                        b = 6 + dc
                        mm_group(C, b, [(kdt[:, ci, dc * 128:(dc + 1) * 128], vt[:, ci, :])], reads=[Tkdt, Tvt])
                        if first:
                            P.op("dve", lambda e, dc=dc, b=b: e.tensor_copy(out=state[h][:, dc, :], in_=ps[:, b, :]), reads=[C.Tps[b]], writes=[Tst[h]])
                        else:
                            P.op("dve", lambda e, dc=dc, b=b: e.scalar_tensor_tensor(
                                out=state[h][:, dc, :], in0=state[h][:, dc, :], scalar=kdcd[:, 2 + h:3 + h], in1=ps[:, b, :],
                                op0=ALU.mult, op1=ALU.add), reads=[C.Tps[b], Tst[h], Tc], writes=[Tst[h]])
                    P.op("pool", lambda e: e.tensor_copy(out=stb[h][:], in_=state[h][:]), reads=[Tst[h]], writes=[Tstb[h]])
                for vc in range(4):
                    P.op("act", lambda e, vc=vc: e.activation(out=sq[:, vc, :], in_=of[:, vc, :], func=AF.Square), reads=[Tof[0][0]], writes=[Tsq[vc]])
                b = C.bank("pj", (0, 1, 2, 3))
                mm_group(C, b, [(C.ones[:], sq[:, vc, :]) for vc in range(4)], reads=[C.Tones] + Tsq)
                P.op("act", lambda e, b=b: e.activation(out=rstd[:], in_=ps[:, b, :], func=AF.Sqrt, scale=1.0 / RDV, bias=C.eps[:]),
                     reads=[C.Tps[b], C.Tones], writes=[Trstd])
                P.op("dve", lambda e: e.reciprocal(out=rstd[:], in_=rstd[:]), reads=[Trstd], writes=[Trstd])
                for vc in range(4):
                    P.op("dve", lambda e, vc=vc: e.tensor_tensor(out=of[:, vc, :], in0=of[:, vc, :], in1=rstd[:], op=ALU.mult),
                         reads=[Tof[0][0], Trstd], writes=[Tof[0][0]])
                    P.op("pool", lambda e, vc=vc: e.tensor_tensor(out=obuf[:, h * 4 + vc, :], in0=of[:, vc, :], in1=sg[:, vc, :], op=ALU.mult),
                         reads=[Tof[0][0], Tsg], writes=[Tobuf])
            for h in range(2):
                head_body(h)
            out_toks.append(P.dma("sp", [lambda e, c=c, ts=ts, obuf=obuf: e.dma_start(out=oT[c0 + c, :, ts], in_=obuf[:, c, :]) for c in range(8)],
                                  reads=[Tobuf], sem="o%d" % (tt % 2)))
    return out_toks


TL = 1919
TU = 1792
MASKNEG = -30000.0


def rel_bucket_np(n):
    n = np.maximum(n, 0)
    nf = np.maximum(n, 1).astype(np.float32)
    large = 16 + (np.log(nf / np.float32(16)) / np.float32(math.log(1024 / 16)) * np.float32(16)).astype(np.int32)
    large = np.minimum(large, 31)
    return np.where(n < 16, n, large)


def attn_consts():
    dist = np.arange(TL) - 511
    oh = np.zeros((33, TL), np.float32)
    bk = rel_bucket_np(dist)
    for j in range(TL):
        if dist[j] < 0:
            oh[32, j] = 1.0
        else:
            oh[bk[j], j] = 1.0
    cneg = np.zeros((16, 16), np.float32)
    negown = np.full((16, 16), MASKNEG, np.float32)
    for b in range(16):
        cneg[b, b:] = -2e30
        negown[b, b] = 0.0
    e16 = np.zeros((16, 16, 128), np.float32)
    for n in range(16):
        e16[n, n, :] = 1.0
    return oh, np.broadcast_to(cneg[None], (128, 16, 16)).copy(), np.broadcast_to(negown[None], (128, 16, 16)).copy(), e16


def emit_attn(C, kind, lam_init, h_src, win, r, rb_full, qkn_d, oh_d, ident_d, cneg_d, negown_d, e16_d, lam_d, sub_d, Rd, o_dst, c0,
              nqt=S // 512):
    moba = (kind == "moba")
    if True:
        P = C.P
        ps = C.ps
        hT = h_src
        oT = o_dst
        win_segs = [(0, r * 512), (512, 1024 + r * 512), (1024, 2048 + r * 512)]
        rb_d = rb_full[:, r * 8:(r + 1) * 8]
        w = C.sb("w", [128, KC, 1536], BF16); Tw = T("w")
        P.dma("pool", [lambda e, d0=d0, s0=s0: e.dma_start(out=w[:, :, d0:d0 + 512], in_=win[:, s0:s0 + 512].rearrange("(k p) n -> p k n", p=128))
                       for d0, s0 in win_segs], writes=[Tw], sem="w")
        rbx = C.sb("rbx", [33, 8], F32)
        qkn = C.sb("qkn_s", [128, 2], F32)
        oh = C.sb("oh_s", [33, TL], F32)
        ident = C.sb("ident_s", [128, 128], F32)
        Tc = T("consts")
        fns = [lambda e: e.dma_start(out=rbx[0:32, :], in_=rb_d), lambda e: e.dma_start(out=qkn[:], in_=qkn_d),
               lambda e: e.dma_start(out=oh[:], in_=oh_d), lambda e: e.dma_start(out=ident[:], in_=ident_d)]
        if moba:
            cneg = C.sb("cneg_s", [128, 16, 16], F32)
            negown = C.sb("negown_s", [128, 16, 16], F32)
            fns += [lambda e: e.dma_start(out=cneg[:], in_=cneg_d), lambda e: e.dma_start(out=negown[:], in_=negown_d)]
        else:
            lam = C.sb("lam_s", [1, 256], F32)
            subw = C.sb("subw_s", [128, 1], F32)
            fns += [lambda e: e.dma_start(out=lam[:], in_=lam_d), lambda e: e.dma_start(out=subw[:], in_=sub_d)]
        P.dma("sp", fns, writes=[Tc], sem="c")
        if moba:
            e16 = C.sb("e16_s", [128, 16, 128], BF16)
            Te16 = T("e16")
            P.op("pool", lambda e: e.memset(e16[:], 0.0), writes=[Te16])
            P.dma("pool", lambda e: e.dma_start(out=e16[0:16], in_=e16_d), writes=[Te16], sem="e16")
        P.op("pool", lambda e: e.memset(rbx[32:33, :], MASKNEG), reads=[Tc], writes=[Tc])
        P.op("dve", lambda e: e.tensor_scalar(out=qkn[:, 0:1], in0=qkn[:, 0:1], scalar1=0.125, scalar2=None, op0=ALU.mult), reads=[Tc], writes=[Tc])
        ones33 = C.sb("ones33", [33, 128], F32)
        onesf = C.sb("onesf", [1, 128], F32)
        bones = C.sb("bones", [128, 128], BF16)
        Tk = T("kconst")
        P.op("pool", lambda e: e.memset(ones33[:], 1.0), writes=[Tk])
        P.op("pool", lambda e: e.memset(onesf[:], 1.0), writes=[Tk])
        P.op("pool", lambda e: e.memset(bones[:], 0.0), writes=[Tk])
        P.op("pool", lambda e: e.memset(bones[0:64, 0:64], 1.0), writes=[Tk])
        P.op("pool", lambda e: e.memset(bones[64:128, 64:128], 1.0), writes=[Tk])
        hm = C.sb("hm", [128, 2], F32)
        P.op("pool", lambda e: e.memset(hm[:], 0.0), writes=[Tk])
        P.op("pool", lambda e: e.memset(hm[0:64, 0:1], 1.0), writes=[Tk])
        P.op("pool", lambda e: e.memset(hm[64:128, 1:2], 1.0), writes=[Tk])
        c31 = C.sb("c31", [128, 8], F32); Tc31 = T("c31")
        brep = C.sb("brep", [33, 128], F32); Tbrep = T("brep")
        rsb = C.sb("rsb", [128, TL], F32); Trsb = T("rsb")
        TRd = [T("Rd%d" % h) for h in range(8)]

        def build_strip(hh):
            P.op("dve", lambda e: e.tensor_scalar(out=brep[:], in0=ones33[:], scalar1=rbx[:, hh:hh + 1], scalar2=None, op0=ALU.mult),
                 reads=[Tc, Tk], writes=[Tbrep])
            for cb in range(4):
                c0, c1 = cb * 512, min(TL, (cb + 1) * 512)
                b = C.bank("pj", (0, 1, 2, 3))

                def mmf(e, c0=c0, c1=c1, b=b):
                    return e.matmul(ps[:, b, 0:c1 - c0], lhsT=brep[:], rhs=oh[:, c0:c1], start=True, stop=True)
                P.op("pe", mmf, reads=[Tbrep, Tc], writes=[C.Tps[b]])
                P.op("act", lambda e, c0=c0, c1=c1, b=b: e.copy(out=rsb[:, c0:c1], in_=ps[:, b, 0:c1 - c0]), reads=[C.Tps[b]], writes=[Trsb])
            P.op("dve", lambda e: e.tensor_copy(out=c31[:, hh:hh + 1], in_=rsb[:, TL - 1:TL]), reads=[Trsb], writes=[Tc31])
            P.dma("sp", lambda e: e.dma_start(out=Rd.ap()[hh], in_=rsb[:]), reads=[Trsb], writes=[TRd[hh]], sem="rd")
        for hh in range(8):
            build_strip(hh)

        if not moba:
            lp = C.sb("lp", [1, 128], F32); l2 = C.sb("l2", [1, 2], F32); nlam = C.sb("nlam", [128, 1], F32); Tl = T("lam")
            P.op("dve", lambda e: e.tensor_tensor(out=lp[:, 0:64], in0=lam[:, 0:64], in1=lam[:, 64:128], op=ALU.mult), reads=[Tc], writes=[Tl])
            P.op("dve", lambda e: e.tensor_tensor(out=lp[:, 64:128], in0=lam[:, 128:192], in1=lam[:, 192:256], op=ALU.mult), reads=[Tc, Tl], writes=[Tl])
            P.op("dve", lambda e: e.reduce_sum(out=l2[:], in_=lp[:].rearrange("p (a b) -> p a b", a=2), axis=AX.X), reads=[Tl], writes=[Tl])
            P.op("act", lambda e: e.activation(out=l2[:], in_=l2[:], func=AF.Exp), reads=[Tl], writes=[Tl])
            P.op("dve", lambda e: e.tensor_tensor(out=lp[:, 0:1], in0=l2[:, 1:2], in1=l2[:, 0:1], op=ALU.subtract), reads=[Tl], writes=[Tl])
            P.op("dve", lambda e: e.tensor_scalar(out=lp[:, 0:1], in0=lp[:, 0:1], scalar1=-float(lam_init), scalar2=None, op0=ALU.add), reads=[Tl], writes=[Tl])

            def mml(e):
                return e.matmul(ps[:, 3, 0:1], lhsT=onesf[:], rhs=lp[:, 0:1], start=True, stop=True)
            P.op("pe", mml, reads=[Tl, Tk], writes=[C.Tps[3]])
            P.op("act", lambda e: e.copy(out=nlam[:], in_=ps[:, 3, 0:1]), reads=[C.Tps[3]], writes=[Tl])
            P.op("dve", lambda e: e.tensor_scalar(out=subw[:], in0=subw[:], scalar1=float(1.0 - lam_init), scalar2=None, op0=ALU.mult), reads=[Tc], writes=[Tc])

        hb = [C.sb("hb%d" % i, [128, KC, 512], BF16) for i in range(2)]
        Thb = [T("hb%d" % i) for i in range(2)]
        if not moba:
            qT = C.sb("qT", [128, S], BF16)
        TqT = T("qT")
        kT = C.sb("kT", [128, S], BF16); TkT = T("kT")
        qz = [C.sb("qz%d" % j, [128, S], BF16) for j in range(2)]
        Tqz = [T("qz%d" % j) for j in range(2)]
        vt = C.sb("vt", [128, 32, 512], BF16); Tvt = T("vt")
        sq = C.sb("sq", [128, 512], BF16); Tsq = T("sq")
        rstd = C.sb("rstd", [128, 512], F32); Trstd = T("rstd")
        tsk = [C.sb("tsk%d" % j, [128, TU], F32) for j in range(2)]
        Ttsk = [T("tsk%d" % j) for j in range(2)]
        pt = [C.sb("pt%d" % i, [128, 512], BF16) for i in range(5)]
        Tpt = [T("pt%d" % i) for i in range(5)]
        tmp = [C.sb("tmp%d" % i, [128, 512], F32) for i in range(3)]
        Ttmp = [T("tmp%d" % i) for i in range(3)]
        ob = [C.sb("ob%d" % i, [128, 512], BF16) for i in range(2)]
        Tob = [T("ob%d" % i) for i in range(2)]
        if moba:
            qF = C.sb("qF", [128, S], F32); TqF = T("qF")
            kF = C.sb("kF", [128, S], F32); TkF = T("kF")
            km = C.sb("km", [128, 16], F32); Tkm = T("km")
            gm = C.sb("gm", [128, 16], F32); Tgm = T("gm")
            top8 = C.sb("top8", [128, 8], F32); Ttop = T("top8")
            mneg = C.sb("mneg", [128, 16], F32); Tmn = T("mneg")
            mT = [C.sb("mT%d" % j, [128, S], BF16) for j in range(2)]
            TmT = [T("mT%d" % j) for j in range(2)]
            for j in range(2):
                P.op("pool", lambda e, j=j: e.memset(mT[j][:], 0.0), writes=[TmT[j]])
        r0 = C.sb("r0", [128, 512], F32); Tr0 = T("r0")
        a0 = C.sb("a0", [128, 512], F32); Ta0 = T("a0")
        if not moba:
            a1 = C.sb("a1", [128, 512], F32); Ta1 = T("a1")
        out_toks = []
        cnt = {"h": 0, "pt": 0, "tmp": 0, "ob": 0}

        def qk_norm(b, wcol, dst_main, Tdst, ts, dstF=None, TdstF=None):
            P.op("act", lambda e: e.activation(out=sq[:], in_=ps[:, b, :], func=AF.Square), reads=[C.Tps[b]], writes=[Tsq])
            b2 = C.bank("nrm", (4, 5))
            mm_group(C, b2, [(bones[:], sq[:])], reads=[Tk, Tsq])
            P.op("act", lambda e: e.activation(out=rstd[:], in_=ps[:, b2, :], func=AF.Sqrt, scale=1.0 / 64, bias=C.eps[:]),
                 reads=[C.Tps[b2], C.Tones], writes=[Trstd])
            P.op("dve", lambda e: e.reciprocal(out=rstd[:], in_=rstd[:]), reads=[Trstd], writes=[Trstd])
            if dstF is not None:
                P.op("dve", lambda e: e.scalar_tensor_tensor(out=dstF[:, ts], in0=ps[:, b, :], scalar=qkn[:, wcol:wcol + 1], in1=rstd[:],
                                                             op0=ALU.mult, op1=ALU.mult), reads=[C.Tps[b], Tc, Trstd], writes=[TdstF])
                if dst_main is not None:
                    P.op("pool", lambda e: e.tensor_copy(out=dst_main[:, ts], in_=dstF[:, ts]), reads=[TdstF], writes=[Tdst])
            else:
                P.op("dve", lambda e: e.scalar_tensor_tensor(out=dst_main[:, ts], in0=ps[:, b, :], scalar=qkn[:, wcol:wcol + 1], in1=rstd[:],
                                                             op0=ALU.mult, op1=ALU.mult), reads=[C.Tps[b], Tc, Trstd], writes=[Tdst])

        def project(p, tt):
            ts = slice(tt * 512, (tt + 1) * 512)
            i = cnt["h"] % 2
            cnt["h"] += 1
            hbuf, Th = hb[i], Thb[i]
            P.dma("sp", [lambda e, k=k: e.dma_start(out=hbuf[:, k, :], in_=hT[k, :, ts]) for k in range(KC)], writes=[Th], sem="h%d" % i)
            for which, (dst, Td, dF, TdF) in enumerate([(None if moba else qT, TqT, qF if moba else None, TqF if moba else None),
                                                        (kT, TkT, kF if moba else None, TkF if moba else None)]):
                b = C.bank("pj", (0, 1, 2, 3))
                col0 = which * 512 + p * 128
                mm_group(C, b, [(w[:, k, col0:col0 + 128], hbuf[:, k, :]) for k in range(KC)], reads=[Tw, Th])
                qk_norm(b, which, dst, Td, ts, dF, TdF)
                if which == 0:
                    for j in range(2):
                        qsrc, Tqsrc = (qF, TqF) if moba else (qT, TqT)
                        P.op("pool", lambda e, j=j: e.tensor_scalar(out=qz[j][:, ts], in0=qsrc[:, ts], scalar1=hm[:, j:j + 1], scalar2=None, op0=ALU.mult),
                             reads=[Tqsrc, Tk], writes=[Tqz[j]])
            if p == 0:
                for ci in range(4):
                    b = C.bank("pj", (0, 1, 2, 3))
                    mm_group(C, b, [(hbuf[:, k, ci * 128:(ci + 1) * 128], w[:, k, 1024:1536]) for k in range(KC)], reads=[Tw, Th])
                    P.op("act", lambda e, ci=ci, b=b: e.copy(out=vt[:, tt * 4 + ci, :], in_=ps[:, b, :]), reads=[C.Tps[b]], writes=[Tvt])

        def gate_tile(j, g):
            hp = slice(64 * j, 64 * j + 64)
            gs = slice(g * 128, (g + 1) * 128)
            blk = g // 2

            def mg(e):
                return e.matmul(ps[:, 6, 0:16], lhsT=qF[hp, gs], rhs=km[hp, :], start=True, stop=True)
            P.op("pe", mg, reads=[TqF, Tkm], writes=[C.Tps[6]])
            P.op("dve", lambda e: e.tensor_tensor(out=gm[:], in0=ps[:, 6, 0:16], in1=cneg[:, blk, :], op=ALU.add), reads=[C.Tps[6], Tc], writes=[Tgm])
            P.op("dve", lambda e: e.max(out=top8[:], in_=gm[:]), reads=[Tgm], writes=[Ttop])
            P.op("dve", lambda e: e.tensor_scalar(out=top8[:, 2:3], in0=top8[:, 2:3], scalar1=-1e30, scalar2=None, op0=ALU.max), reads=[Ttop], writes=[Ttop])
            P.op("dve", lambda e: e.scalar_tensor_tensor(out=mneg[:], in0=gm[:], scalar=top8[:, 2:3], in1=negown[:, blk, :], op0=ALU.is_lt, op1=ALU.mult),
                 reads=[Tgm, Ttop, Tc], writes=[Tmn])

            def tr(e):
                return e.transpose(out=ps[0:16, 7, 0:128], in_=mneg[:], identity=ident[:])
            P.op("pe", tr, reads=[Tmn, Tc], writes=[C.Tps[7]])
            P.op("act", lambda e: e.copy(out=mT[j][0:16, gs], in_=ps[0:16, 7, 0:128]), reads=[C.Tps[7]], writes=[TmT[j]])

        def stage1(p, j, qt, kt):
            hp = slice(64 * j, 64 * j + 64)
            qs = slice(qt * 512, (qt + 1) * 512)
            ks = slice(kt * 128, (kt + 1) * 128)
            delta = qt * 512 - kt * 128
            bs = C.bank("s", (0, 1, 2, 3))

            def ms(e):
                ins = e.matmul(ps[:, bs, :], lhsT=kT[:, ks], rhs=qz[j][:, qs], start=True, stop=not moba)
                if moba:
                    ins = e.matmul(ps[:, bs, :], lhsT=e16[:, kt // 2, :], rhs=mT[j][:, qs], start=False, stop=True)
                return ins
            P.op("pe", ms, reads=[TkT, Tqz[j]] + ([Te16, TmT[j]] if moba else []), writes=[C.Tps[bs]])
            ip = cnt["pt"] % len(pt)
            cnt["pt"] += 1
            if delta >= 1024:
                P.op("act", lambda e: e.activation(out=pt[ip][:], in_=ps[:, bs, :], func=AF.Exp, bias=c31[:, 2 * p + j:2 * p + j + 1]),
                     reads=[C.Tps[bs], Tc31], writes=[Tpt[ip]])
            else:
                it = cnt["tmp"] % len(tmp)
                cnt["tmp"] += 1
                P.op("dve", lambda e: e.tensor_tensor(out=tmp[it][:], in0=ps[:, bs, :], in1=tsk[j][:, delta + 384:delta + 384 + 512], op=ALU.add),
                     reads=[C.Tps[bs], Ttsk[j]], writes=[Ttmp[it]])
                P.op("act", lambda e: e.activation(out=pt[ip][:], in_=tmp[it][:], func=AF.Exp), reads=[Ttmp[it]], writes=[Tpt[ip]])
            return ip

        def stage2(p, j, kt, ip, bo, bl, first, last):
            hp = slice(64 * j, 64 * j + 64)

            def mpv(e):
                e.matmul(ps[:, bo, :], lhsT=vt[:, kt, p * 128:(p + 1) * 128], rhs=pt[ip][:], start=first, stop=last)
                return e.matmul(ps[:, bl, :], lhsT=C.ones[:], rhs=pt[ip][:], start=first, stop=last)
            P.op("pe", mpv, reads=[Tvt, Tpt[ip], C.Tones], writes=[C.Tps[bo], C.Tps[bl]])

        def finish_head(j, bo, bl):
            if moba:
                hp = slice(64 * j, 64 * j + 64)
                P.op("dve", lambda e: e.reciprocal(out=r0[hp, :], in_=ps[hp, bl, :]), reads=[C.Tps[bl]], writes=[Tr0])
                P.op("dve", lambda e: e.tensor_tensor(out=a0[hp, :], in0=ps[hp, bo, :], in1=r0[hp, :], op=ALU.mult), reads=[C.Tps[bo], Tr0], writes=[Ta0])
                return
            a, Ta = (a0, Ta0) if j == 0 else (a1, Ta1)
            P.op("dve", lambda e: e.reciprocal(out=r0[:], in_=ps[:, bl, :]), reads=[C.Tps[bl]], writes=[Tr0])
            P.op("dve", lambda e: e.tensor_tensor(out=a[:], in0=ps[:, bo, :], in1=r0[:], op=ALU.mult), reads=[C.Tps[bo], Tr0], writes=[Ta])

        def finish_qt(p, qt, bo, bl):
            qs = slice(qt * 512, (qt + 1) * 512)
            io = cnt["ob"] % 2
            cnt["ob"] += 1
            if moba:
                P.op("pool", lambda e: e.tensor_copy(out=ob[io][:], in_=a0[:]), reads=[Ta0], writes=[Tob[io]])
            else:
                P.op("dve", lambda e: e.scalar_tensor_tensor(out=a0[:], in0=a1[:], scalar=nlam[:], in1=a0[:], op0=ALU.mult, op1=ALU.add),
                     reads=[Ta0, Ta1, Tl], writes=[Ta0])
                P.op("act", lambda e: e.activation(out=sq[:], in_=a0[:], func=AF.Square), reads=[Ta0], writes=[Tsq])
                bn = C.bank("s", (0, 1, 2, 3))
                mm_group(C, bn, [(C.ones[:], sq[:])], reads=[C.Tones, Tsq])
                P.op("act", lambda e: e.activation(out=rstd[:], in_=ps[:, bn, :], func=AF.Sqrt, scale=1.0 / 128, bias=C.eps[:]),
                     reads=[C.Tps[bn], C.Tones], writes=[Trstd])
                P.op("dve", lambda e: e.reciprocal(out=rstd[:], in_=rstd[:]), reads=[Trstd], writes=[Trstd])
                P.op("dve", lambda e: e.scalar_tensor_tensor(out=ob[io][:], in0=a0[:], scalar=subw[:], in1=rstd[:], op0=ALU.mult, op1=ALU.mult),
                     reads=[Ta0, Tc, Trstd], writes=[Tob[io]])
            out_toks.append(P.dma("sp", lambda e: e.dma_start(out=oT[c0 + p, :, qs], in_=ob[io][:]), reads=[Tob[io]], sem="o%d" % io))

        def attention(p):
            PD = 3
            tiles = []
            for qt in range(nqt):
                nk = 4 * qt + 4
                if True:
                    for j in range(2):
                        bo, bl = 4 + 2 * j, 5 + 2 * j
                        for kt in range(nk):
                            lastt = (kt == nk - 1)
                            tiles.append((j, qt, kt, bo, bl, kt == 0, lastt, (j, bo, bl) if lastt else None, (qt, bo, bl) if (lastt and j == 1) else None))
            pend = []

            def retire():
                (j, qt, kt, bo, bl, first, last, fh, fq), ip = pend.pop(0)
                stage2(p, j, kt, ip, bo, bl, first, last)
                if fh is not None:
                    finish_head(*fh)
                if fq is not None:
                    finish_qt(p, *fq)
            for tl in tiles:
                ip = stage1(p, tl[0], tl[1], tl[2])
                pend.append((tl, ip))
                if len(pend) > PD:
                    retire()
            while pend:
                retire()

        def load_tsk(p, j):
            hh = 2 * p + j
            src = bass.AP(Rd, hh * 128 * TL + 127, [[TL - 1, 128], [1, TU]])
            P.dma("sp", lambda e: e.dma_start(out=tsk[j][:], in_=src), reads=[TRd[hh]], writes=[Ttsk[j]], sem="tsk%d" % j)

        for p in range(4):
            for tt in range(8):
                project(p, tt)
            for j in range(2):
                load_tsk(p, j)
            if moba:
                P.op("dve", lambda e: e.reduce_sum(out=km[:], in_=kF[:].rearrange("p (n t) -> p n t", t=256), axis=AX.X), reads=[TkF], writes=[Tkm])
                for j in range(2):
                    for g in range(4 * nqt):
                        gate_tile(j, g)
            attention(p)
    return out_toks


def build_fused():
    nc = bass.Bass("TRN2", target_bir_lowering=False)

    def inp(name, shape, dt=F32):
        return nc.dram_tensor(name, shape, dt, kind="ExternalInput").ap()
    xT = inp("xT", [KC, 128, S])
    n1 = inp("n1", [4, 128, KC]); n2 = inp("n2", [4, 128, KC])
    wup = inp("wup", [4, D, DFF]); wdown = inp("wdown", [4, DFF, D])
    retin = inp("retin", [2, D, 6144]); retout = inp("retout", [2, 2048, D])
    mobain = inp("mobain", [D, 3072]); mobaout = inp("mobaout", [D, D])
    diffin = inp("diffin", [D, 3072]); diffout = inp("diffout", [D, D])
    rb = inp("rb", [32, 16]); qknm = inp("qknm", [128, 2]); qknd = inp("qknd", [128, 2])
    lam = inp("lam", [1, 256]); subw = inp("subw", [128, 1])
    cs_d = inp("cs", [2, 128, S]); dmat_d = inp("dmat", [4, 128, 128]); gq_d = inp("gq", [4, 128, 128]); kdcd_d = inp("kdcd", [128, 8])
    ident_d = inp("ident", [128, 128]); oh_d = inp("oh", [33, TL])
    cneg_d = inp("cneg", [128, 16, 16]); negown_d = inp("negown", [128, 16, 16]); e16_d = inp("e16", [16, 16, 128])
    xs = nc.dram_tensor("xs", [KC, 128, S], F32, kind="Internal").ap()
    hs = nc.dram_tensor("hs", [KC, 128, S], BF16, kind="Internal").ap()
    osc = nc.dram_tensor("osc", [16, 128, S], BF16, kind="Internal").ap()
    Rd = nc.dram_tensor("Rscr", [8, 128, TL], F32, kind="Internal")
    xo = nc.dram_tensor("xo", [KC, 128, S], F32, kind="ExternalOutput").ap()

    with ExitStack() as st:
        C = Ctx(nc, st)
        P = C.P

        def phase(fn):
            P.barrier()
            with ExitStack() as ph:
                C.ph = ph
                C.bank_rr = {}
                fn()
            C.ph = None
        phase(lambda: emit_phase_a(C, S, xT, None, n1=n1[0], h_dst=hs))
        for i in range(4):
            kind, j = i % 3, i // 3
            for r in range(2):
                if kind == 0:
                    phase(lambda: emit_ret(C, hs, retin[j], (2 * r, 2 * r + 1), cs_d, dmat_d, gq_d, kdcd_d, ident_d, osc, 8 * r))
                elif kind == 1:
                    phase(lambda: emit_attn(C, "moba", 0.0, hs, mobain, r, rb, qknm, oh_d, ident_d, cneg_d, negown_d, e16_d, None, None, Rd, osc, 4 * r))
                else:
                    lam_init = 0.8 - 0.6 * math.exp(-0.3 * i)
                    phase(lambda: emit_attn(C, "diff", lam_init, hs, diffin, r, rb, qknd, oh_d, ident_d, None, None, None, lam, subw, Rd, osc, 4 * r))
            wout, fo = ((retout[j], 2048) if kind == 0 else ((mobaout, 1024) if kind == 1 else (diffout, 1024)))
            last = (i == 3)
            phase(lambda: emit_phase_a(C, S, xT if i == 0 else xs, xo if last else xs, o_src=osc, wout=wout, fo=fo, wup=wup[i], wdown=wdown[i],
                                       n2=n2[i], n1=None if last else n1[i + 1], h_dst=None if last else hs))
        P.barrier()
        P.emit(st)
    return nc


_NC_CACHE = {}


def _fm(a):
    return np.ascontiguousarray(a.T.reshape(a.shape[1] // 128, 128, a.shape[0]))


def _unfm(a):
    return np.ascontiguousarray(a.reshape(-1, a.shape[2]).T)


def _nw(w):
    return np.ascontiguousarray(w.reshape(w.shape[0], KC, 128).transpose(0, 2, 1))


def kernel(x, rel_bias, norm1, norm2, w_up, w_down, ret_w_in, ret_w_out,
           moba_w_in, moba_q_norm, moba_k_norm, moba_w_out,
           diff_w_in, diff_q_norm, diff_k_norm, diff_lambda, diff_subln, diff_w_out):
    f32 = np.float32
    A = lambda a: np.ascontiguousarray(np.asarray(a, f32))
    x = A(x)
    if "fused" not in _NC_CACHE:
        _NC_CACHE["fused"] = build_fused()
    nc = _NC_CACHE["fused"]
    cs, ph = ret_consts()
    oh, cneg, negown, e16 = attn_consts()
    shared = {
        "n1": _nw(A(norm1)), "n2": _nw(A(norm2)), "wup": A(w_up), "wdown": A(w_down),
        "retin": A(ret_w_in), "retout": A(ret_w_out), "mobain": A(moba_w_in[0]), "mobaout": A(moba_w_out[0]),
        "diffin": A(diff_w_in[0]), "diffout": A(diff_w_out[0]), "rb": A(rel_bias),
        "qknm": A(np.stack([np.tile(A(moba_q_norm[0]), 2), np.tile(A(moba_k_norm[0]), 2)], axis=1)),
        "qknd": A(np.stack([np.tile(A(diff_q_norm[0]), 2), np.tile(A(diff_k_norm[0]), 2)], axis=1)),
        "lam": A(diff_lambda[0]).reshape(1, 256), "subw": A(diff_subln[0]).reshape(128, 1),
        "cs": cs, "dmat": A(np.stack([ph[h][0] for h in range(4)])), "gq": A(np.stack([ph[h][1] for h in range(4)])),
        "kdcd": A(np.stack([ph[h][2] for h in range(4)] + [ph[h][3] for h in range(4)], axis=1)),
        "ident": np.eye(128, dtype=f32), "oh": oh, "cneg": cneg, "negown": negown, "e16": e16,
    }
    ims = []
    for c in range(8):
        d = dict(shared)
        d["xT"] = _fm(x[c % 4])
        ims.append(d)
    res = run_bass_kernel_spmd(nc, ims, core_ids=list(range(8))).results
    out = np.empty((4, S, D), f32)
    for b in range(4):
        out[b] = _unfm(res[b]["xo"])
    return out
```

```python
import math
from contextlib import ExitStack
import numpy as np
import concourse.bass as bass
import concourse.mybir as mybir
from concourse.bass_utils import run_bass_kernel_spmd

F32 = mybir.dt.float32
BF16 = mybir.dt.bfloat16
F32R = mybir.dt.float32r
AF = mybir.ActivationFunctionType
ALU = mybir.AluOpType
AX = mybir.AxisListType

D = 1024
KC = 8
S = 4096
NTOK = 2048
DFF = 4096
EPS = 1e-6
ENGS = ("pe", "act", "dve", "pool", "sp")


class T:
    __slots__ = ("name", "w", "r")

    def __init__(self, name=""):
        self.name = name
        self.w = None
        self.r = {}


class Prog:
    def __init__(self, nc, same_engine_sync=True):
        self.nc = nc
        self.ops = {e: [] for e in ENGS}
        self.cnt = {}
        self.seen = {e: {} for e in ENGS}
        self.same = same_engine_sync
        self.dma_sems = {}

    def _deps(self, eng, reads, writes):
        deps = {}

        def add(tok):
            if tok is None:
                return
            k, v = tok
            if deps.get(k, 0) < v:
                deps[k] = v
        for t in reads:
            add(t.w)
        for t in writes:
            add(t.w)
            for r in t.r.items():
                add(r)
        waits = []
        for k, v in deps.items():
            if k == eng and (not self.same or eng == "pe"):
                continue
            if self.seen[eng].get(k, 0) >= v:
                continue
            self.seen[eng][k] = v
            waits.append((k, v))
        return waits

    def _mark(self, tok, reads, writes):
        for t in reads:
            if t.r.get(tok[0], 0) < tok[1]:
                t.r[tok[0]] = tok[1]
        for t in writes:
            t.w = tok
            t.r = {}

    def op(self, eng, fn, reads=(), writes=()):
        waits = self._deps(eng, reads, writes)
        self.cnt[eng] = self.cnt.get(eng, 0) + 1
        tok = (eng, self.cnt[eng])
        self._mark(tok, reads, writes)
        self.ops[eng].append((fn, waits, (eng, 1)))
        return tok

    def dma(self, q, fns, reads=(), writes=(), sem=None):
        if not isinstance(fns, (list, tuple)):
            fns = [fns]
        key = ("dma", sem)
        self.dma_sems[key] = None
        waits = self._deps(q, reads, writes)
        self.cnt[key] = self.cnt.get(key, 0) + 16 * len(fns)
        tok = (key, self.cnt[key])
        self._mark(tok, reads, writes)
        for i, fn in enumerate(fns):
            self.ops[q].append((fn, waits if i == 0 else [], (key, 16)))
        return tok

    def wait_all(self, eng, toks):
        best = {}
        for k, v in toks:
            best[k] = max(best.get(k, 0), v)
        waits = []
        for k, v in best.items():
            if self.seen[eng].get(k, 0) >= v:
                continue
            self.seen[eng][k] = v
            waits.append((k, v))
        self.ops[eng].append((None, waits, None))

    def barrier(self):
        toks = list(self.cnt.items())
        for e in ENGS:
            self.wait_all(e, toks)

    def emit(self, stack):
        nc = self.nc
        sems = {}
        for e in ENGS:
            if self.ops[e]:
                sems[e] = stack.enter_context(nc.semaphore("s_" + e))
        for k in self.dma_sems:
            sems[k] = stack.enter_context(nc.semaphore("d_%s" % (k[1],)))
        block = stack.enter_context(nc.Block())
        handles = {"pe": block.tensor, "act": block.scalar, "dve": block.vector,
                   "pool": block.gpsimd, "sp": block.sync}

        def make(e):
            def body(engine):
                for fn, waits, inc in self.ops[e]:
                    for k, v in waits:
                        engine.wait_ge(sems[k], v)
                    if fn is not None:
                        fn(engine).then_inc(sems[inc[0]], inc[1])
            return body
        for e in ENGS:
            if self.ops[e]:
                handles[e](make(e))


class Ctx:
    def __init__(self, nc, st):
        self.nc, self.st = nc, st
        self.P = Prog(nc)
        self.ps = st.enter_context(nc.psum_tensor("ps", [128, 8, 512], F32))
        self.Tps = [T("ps%d" % i) for i in range(8)]
        self.ones = st.enter_context(nc.sbuf_tensor("ones", [128, 128], BF16))
        self.Tones = T("ones")
        self.eps = st.enter_context(nc.sbuf_tensor("epsc", [128, 1], F32))
        self.P.op("pool", lambda e: e.memset(self.eps[:], EPS), writes=[self.Tones])
        self.P.op("pool", lambda e: e.memset(self.ones[:], 1.0), writes=[self.Tones])
        self.bank_rr = {}

    def sb(self, name, shape, dt):
        self.nalloc = getattr(self, "nalloc", 0) + 1
        stack = self.ph if getattr(self, "ph", None) is not None else self.st
        return stack.enter_context(self.nc.sbuf_tensor("%s_%d" % (name, self.nalloc), shape, dt))

    def bank(self, group, banks):
        i = self.bank_rr.get(group, 0)
        self.bank_rr[group] = i + 1
        return banks[i % len(banks)]


def mm_group(C, bank, mms, reads, n=512, prow=128):
    ps = C.ps

    def fn(e, mms=mms):
        ins = None
        for i, (l, r) in enumerate(mms):
            ins = e.matmul(ps[0:prow, bank, 0:n], lhsT=l, rhs=r, start=(i == 0), stop=(i == len(mms) - 1))
        return ins
    return C.P.op("pe", fn, reads=reads, writes=[C.Tps[bank]])


def rmsnorm_fm(C, x, Tx, w32, Tw, out, Tout, ntok, tmp, nd=KC, banks=(6, 7), tt_list=None, xoff=0):
    P = C.P
    sq, Tsq, rstd, Trstd = tmp
    nfeat = nd * 128
    for tt in (tt_list if tt_list is not None else range(ntok // 512)):
        ts = slice(tt * 512, (tt + 1) * 512)
        xs = slice(xoff + tt * 512, xoff + (tt + 1) * 512)
        b = C.bank("nrm", banks)
        for c in range(nd):
            P.op("act", lambda e, c=c, xs=xs: e.activation(out=sq[:, c, :], in_=x[:, c, xs], func=AF.Square),
                 reads=[Tx[c][tt]], writes=[Tsq[c]])
        mm_group(C, b, [(C.ones[:], sq[:, c, :]) for c in range(nd)], reads=[C.Tones] + Tsq[:nd])
        P.op("act", lambda e, b=b: e.activation(out=rstd[:], in_=C.ps[:, b, :], func=AF.Sqrt, scale=1.0 / nfeat, bias=C.eps[:]),
             reads=[C.Tps[b], C.Tones], writes=[Trstd])
        P.op("dve", lambda e: e.reciprocal(out=rstd[:], in_=rstd[:]), reads=[Trstd], writes=[Trstd])
        for c in range(nd):
            P.op("dve", lambda e, c=c, ts=ts, xs=xs: e.scalar_tensor_tensor(
                out=out[:, c, ts], in0=x[:, c, xs], scalar=w32[:, c:c + 1], in1=rstd[:], op0=ALU.mult, op1=ALU.mult),
                reads=[Tx[c][tt], Tw, Trstd], writes=[Tout[c][tt]])


def emit_phase_a(C, ntok, x_src, x_dst, o_src=None, wout=None, fo=0, wup=None, wdown=None, n2=None, n1=None, h_dst=None, TT=1024):
    P = C.P
    do_mix, do_ffn, do_next = o_src is not None, wup is not None, h_dst is not None
    NS = TT // 512
    out_toks = []
    x = C.sb("x", [128, KC, TT], F32)
    Tx = [[T("x") for _ in range(NS)] for c in range(KC)]
    sq = C.sb("sq", [128, KC, 512], BF16)
    Tsq = [T("sq") for c in range(KC)]
    rstd = C.sb("rstd", [128, 512], F32)
    tmpn = (sq, Tsq, rstd, T("rstd"))
    hb = C.sb("hb", [128, KC, TT], BF16)
    Thb = [[T("hb") for _ in range(NS)] for c in range(KC)]
    allx = [t for c in range(KC) for t in Tx[c]]
    allh = [t for c in range(KC) for t in Thb[c]]

    def load_w(name, src):
        w = C.sb(name, [128, KC], F32)
        Tw = T(name)
        P.dma("sp", lambda e: e.dma_start(out=w[:], in_=src), writes=[Tw], sem=name)
        return w, Tw
    if do_ffn or do_mix:
        u2 = C.sb("u2", [128, 32, TT], BF16)
        Tu2 = [T("u2") for f in range(32)]
    if do_mix:
        FK = fo // 128
        wo = [C.sb("wo", [128, FK, 128], BF16) for i in range(2)]
        Two = [T("wo") for i in range(2)]
    if do_ffn:
        w2, Tw2 = load_w("n2w", n2)
        wu = [C.sb("wu", [128, KC, 512], BF16) for i in range(2)]
        Twu = [T("wu") for i in range(2)]
        wd = [C.sb("wd", [128, 32, 256], BF16) for i in range(2)]
        Twd = [T("wd") for i in range(2)]
        rl = [C.sb("rl", [128, 512], F32) for i in range(2)]
        Trl = [T("rl") for i in range(2)]
    if do_next:
        w1, Tw1 = load_w("n1w", n1)
    cnt = {"wo": 0, "wu": 0, "wd": 0, "rl": 0}

    def tile_body(tt):
        t0 = tt * TT
        P.dma("sp", [lambda e, c=c: e.dma_start(out=x[:, c, :], in_=x_src[c, :, t0:t0 + TT]) for c in range(KC)], writes=allx, sem="xin")
        if do_mix:
            ob = u2
            P.dma("sp", [lambda e, k=k: e.dma_start(out=ob[:, k, :], in_=o_src[k, :, t0:t0 + TT]) for k in range(FK)], writes=Tu2[:FK], sem="oin")
            for n in range(KC):
                sl = cnt["wo"] % 2
                cnt["wo"] += 1
                P.dma("pool", lambda e, n=n, sl=sl: e.dma_start(
                    out=wo[sl][:], in_=wout[:, n * 128:(n + 1) * 128].rearrange("(k p) n -> p k n", p=128)),
                    writes=[Two[sl]], sem="wo%d" % sl)
                for s_ in range(NS):
                    ss = slice(s_ * 512, (s_ + 1) * 512)
                    b = C.bank("dn", (4, 5))
                    mm_group(C, b, [(wo[sl][:, k, :], ob[:, k, ss]) for k in range(FK)], reads=[Two[sl]] + Tu2[:FK])
                    P.op("dve", lambda e, n=n, ss=ss, b=b: e.tensor_tensor(out=x[:, n, ss], in0=x[:, n, ss], in1=C.ps[:, b, :], op=ALU.add),
                         reads=[C.Tps[b], Tx[n][s_]], writes=[Tx[n][s_]])
        if do_ffn:
            rmsnorm_fm(C, x, Tx, w2, Tw2, hb, Thb, TT, tmpn)
            for fg in range(8):
                sl = cnt["wu"] % 2
                cnt["wu"] += 1
                P.dma("pool", lambda e, fg=fg, sl=sl: e.dma_start(
                    out=wu[sl][:], in_=wup[:, fg * 512:(fg + 1) * 512].rearrange("(k p) f -> p k f", p=128)),
                    writes=[Twu[sl]], sem="wu%d" % sl)
                for fi in range(4):
                    f = fg * 4 + fi
                    for s_ in range(NS):
                        ss = slice(s_ * 512, (s_ + 1) * 512)
                        b = C.bank("up", (0, 1, 2, 3))
                        mm_group(C, b, [(wu[sl][:, c, fi * 128:(fi + 1) * 128], hb[:, c, ss]) for c in range(KC)],
                                 reads=[Twu[sl]] + [Thb[c][s_] for c in range(KC)])
                        r = cnt["rl"] % 2
                        cnt["rl"] += 1
                        P.op("act", lambda e, b=b, r=r: e.activation(out=rl[r][:], in_=C.ps[:, b, :], func=AF.Relu),
                             reads=[C.Tps[b]], writes=[Trl[r]])
                        P.op("dve", lambda e, f=f, r=r, ss=ss: e.tensor_tensor(out=u2[:, f, ss], in0=rl[r][:], in1=rl[r][:], op=ALU.mult),
                             reads=[Trl[r]], writes=[Tu2[f]])
            for ng in range(4):
                sl = cnt["wd"] % 2
                cnt["wd"] += 1
                P.dma("pool", lambda e, ng=ng, sl=sl: e.dma_start(
                    out=wd[sl][:], in_=wdown[:, ng * 256:(ng + 1) * 256].rearrange("(k p) n -> p k n", p=128)),
                    writes=[Twd[sl]], sem="wd%d" % sl)
                for ni in range(2):
                    n = ng * 2 + ni
                    for s_ in range(NS):
                        ss = slice(s_ * 512, (s_ + 1) * 512)
                        b = C.bank("dn", (4, 5))
                        mm_group(C, b, [(wd[sl][:, f, ni * 128:(ni + 1) * 128], u2[:, f, ss]) for f in range(32)],
                                 reads=[Twd[sl]] + Tu2)
                        P.op("dve", lambda e, n=n, ss=ss, b=b: e.tensor_tensor(out=x[:, n, ss], in0=x[:, n, ss], in1=C.ps[:, b, :], op=ALU.add),
                             reads=[C.Tps[b], Tx[n][s_]], writes=[Tx[n][s_]])
        if x_dst is not None:
            out_toks.append(P.dma("sp", [lambda e, c=c: e.dma_start(out=x_dst[c, :, t0:t0 + TT], in_=x[:, c, :]) for c in range(KC)],
                                  reads=allx, sem="xout"))
        if do_next:
            rmsnorm_fm(C, x, Tx, w1, Tw1, hb, Thb, TT, tmpn)
            out_toks.append(P.dma("sp", [lambda e, c=c: e.dma_start(out=h_dst[c, :, t0:t0 + TT], in_=hb[:, c, :]) for c in range(KC)],
                                  reads=allh, sem="hout"))
    for tt in range(ntok // TT):
        tile_body(tt)
    return out_toks


RH, RDK, RDV, RC = 4, 256, 512, 128


def ret_consts():
    inv = (10000.0 ** (-np.arange(0, RDK, 2, dtype=np.float32) / np.float32(RDK))).astype(np.float32)
    ang = (np.arange(S, dtype=np.float32)[:, None] * inv[None, :]).astype(np.float32)
    cs = np.ascontiguousarray(np.stack([np.cos(ang).T, np.sin(ang).T]).astype(np.float32))
    lg = np.log(1.0 - 2.0 ** (-5.0 - np.arange(RH, dtype=np.float64)))
    pos = np.arange(RC, dtype=np.float64)
    per_head = []
    for h in range(RH):
        rel = pos[None, :] - pos[:, None]
        dT = np.where(rel >= 0, np.exp(np.maximum(rel, 0) * lg[h]), 0.0) * RDK ** -0.5
        gq = np.broadcast_to(np.exp((pos + 1.0) * lg[h])[None, :], (128, 128))
        kd = np.exp((RC - 1.0 - pos) * lg[h]) * RDK ** -0.5
        cd = np.full(128, np.exp(RC * lg[h]))
        per_head.append((dT.astype(np.float32), gq.astype(np.float32), kd.astype(np.float32), cd.astype(np.float32)))
    return cs, per_head


def emit_ret(C, h_src, win, hsel, cs_d, dmat_d, gq_d, kdcd_d, ident_d, o_dst, c0, ntt=S // 512):
    if True:
        P = C.P
        ps = C.ps
        oT = o_dst
        hT = h_src
        w = [C.sb("w%d" % h, [128, KC, 1536], BF16) for h in range(2)]
        Tw = [T("w%d" % h) for h in range(2)]
        for h in range(2):
            g = hsel[h]
            segs = [(0, g * 256, 256), (256, 1024 + g * 256, 256), (512, 2048 + g * 512, 512), (1024, 4096 + g * 512, 512)]
            P.dma("pool", [lambda e, h=h, d0=d0, s0=s0, n=n: e.dma_start(
                out=w[h][:, :, d0:d0 + n], in_=win[:, s0:s0 + n].rearrange("(k p) n -> p k n", p=128)) for d0, s0, n in segs],
                writes=[Tw[h]], sem="w%d" % h)
        dmat = C.sb("dmat_s", [128, 2, 128], F32)
        gq = C.sb("gq_s", [128, 2, 128], F32)
        kdcd = C.sb("kdcd_s", [128, 4], F32)
        ident = C.sb("ident_s", [128, 128], F32)
        Tc = T("consts")
        P.dma("sp", [lambda e, h=h: e.dma_start(out=dmat[:, h, :], in_=dmat_d[hsel[h]]) for h in range(2)]
              + [lambda e, h=h: e.dma_start(out=gq[:, h, :], in_=gq_d[hsel[h]]) for h in range(2)]
              + [lambda e, h=h: e.dma_start(out=kdcd[:, h:h + 1], in_=kdcd_d[:, hsel[h]:hsel[h] + 1], allow_slow_non_contiguous=True) for h in range(2)]
              + [lambda e, h=h: e.dma_start(out=kdcd[:, 2 + h:3 + h], in_=kdcd_d[:, 4 + hsel[h]:5 + hsel[h]], allow_slow_non_contiguous=True) for h in range(2)]
              + [lambda e: e.dma_start(out=ident[:], in_=ident_d)],
              writes=[Tc], sem="c")

        hb = [C.sb("hb%d" % i, [128, KC, 512], BF16) for i in range(2)]
        Thb = [T("hb%d" % i) for i in range(2)]
        csb = [C.sb("cs%d" % i, [128, 2, 512], F32) for i in range(2)]
        Tcs = [T("cs%d" % i) for i in range(2)]
        raw = C.sb("raw", [128, 2, 512], F32); Traw = T("raw")
        t1 = C.sb("t1", [128, 512], F32); Tt1 = T("t1")
        t2 = C.sb("t2", [128, 512], F32); Tt2 = T("t2")
        rot = C.sb("rot", [128, 2, 512], F32); Trot = T("rot")
        qb = C.sb("qb", [128, 2, 512], BF16); Tqb = T("qb")
        qd = C.sb("qd", [128, 2, 512], BF16); Tqd = T("qd")
        kb = C.sb("kb", [128, 2, 512], BF16); Tkb = T("kb")
        kdt = C.sb("kdt", [128, 4, 256], BF16); Tkdt = T("kdt")
        vt = C.sb("vt", [128, 4, 512], BF16); Tvt = T("vt")
        sg = C.sb("sg", [128, 4, 512], F32); Tsg = T("sg")
        of = C.sb("of", [128, 4, 512], F32); Tof = [[T("of%d_%d" % (v, 0))] for v in range(4)]
        at = C.sb("at", [128, 128], BF16); Tat = T("at")
        state = [C.sb("st%d" % h, [128, 2, 512], F32) for h in range(2)]
        stb = [C.sb("stb%d" % h, [128, 2, 512], BF16) for h in range(2)]
        Tst = [[T("st%d_%d" % (h, dc)) for dc in range(2)] for h in range(2)]
        Tstb = [T("stb%d" % h) for h in range(2)]
        sq = C.sb("sq", [128, 4, 512], BF16); Tsq = [T("sq%d" % c) for c in range(4)]
        rstd = C.sb("rstd", [128, 512], F32); Trstd = T("rstd")
        ob = [C.sb("ob%d" % i, [128, 8, 512], BF16) for i in range(2)]
        Tob = [T("ob%d" % i) for i in range(2)]
        out_toks = []

        for tt in range(ntt):
            ts = slice(tt * 512, (tt + 1) * 512)
            hbuf, Th = hb[tt % 2], Thb[tt % 2]
            cbuf, Tcb = csb[tt % 2], Tcs[tt % 2]
            P.dma("sp", [lambda e, k=k, ts=ts, hbuf=hbuf: e.dma_start(out=hbuf[:, k, :], in_=hT[k, :, ts]) for k in range(KC)],
                  writes=[Th], sem="h%d" % (tt % 2))
            P.dma("sp", [lambda e, i=i, ts=ts, cbuf=cbuf: e.dma_start(out=cbuf[:, i, :], in_=cs_d[i, :, ts]) for i in range(2)],
                  writes=[Tcb], sem="cs%d" % (tt % 2))
            obuf, Tobuf = ob[tt % 2], Tob[tt % 2]
            def head_body(h, tt=tt, ts=ts, hbuf=hbuf, Th=Th, cbuf=cbuf, Tcb=Tcb, obuf=obuf, Tobuf=Tobuf):
                def proj_fm(col0, banks):
                    b = C.bank("pj", banks)
                    mm_group(C, b, [(w[h][:, k, col0:col0 + 128], hbuf[:, k, :]) for k in range(KC)], reads=[Tw[h], Th])
                    return b

                def rotary(col0, dst_bf, Tdst, want_f32):
                    b0 = proj_fm(col0, (0, 1, 2, 3))
                    b1 = proj_fm(col0 + 128, (0, 1, 2, 3))
                    P.op("act", lambda e: e.copy(out=raw[:, 0, :], in_=ps[:, b0, :]), reads=[C.Tps[b0]], writes=[Traw])
                    P.op("act", lambda e: e.copy(out=raw[:, 1, :], in_=ps[:, b1, :]), reads=[C.Tps[b1]], writes=[Traw])
                    cos, sin = cbuf[:, 0, :], cbuf[:, 1, :]
                    P.op("dve", lambda e: e.tensor_tensor(out=t1[:], in0=raw[:, 0, :], in1=cos, op=ALU.mult), reads=[Traw, Tcb], writes=[Tt1])
                    P.op("pool", lambda e: e.tensor_tensor(out=t2[:], in0=raw[:, 1, :], in1=sin, op=ALU.mult), reads=[Traw, Tcb], writes=[Tt2])
                    P.op("dve", lambda e: e.tensor_tensor(out=rot[:, 0, :], in0=t1[:], in1=t2[:], op=ALU.subtract), reads=[Tt1, Tt2], writes=[Trot])
                    P.op("dve", lambda e: e.tensor_tensor(out=t1[:], in0=raw[:, 0, :], in1=sin, op=ALU.mult), reads=[Traw, Tcb], writes=[Tt1])
                    P.op("pool", lambda e: e.tensor_tensor(out=t2[:], in0=raw[:, 1, :], in1=cos, op=ALU.mult), reads=[Traw, Tcb], writes=[Tt2])
                    P.op("dve", lambda e: e.tensor_tensor(out=rot[:, 1, :], in0=t1[:], in1=t2[:], op=ALU.add), reads=[Tt1, Tt2], writes=[Trot])
                    P.op("act", lambda e: e.copy(out=dst_bf[:], in_=rot[:]), reads=[Trot], writes=[Tdst])

                rotary(0, qb, Tqb, False)
                for dc in range(2):
                    P.op("pool", lambda e, dc=dc: e.tensor_tensor(
                        out=qd[:, dc, :].rearrange("p (c i) -> p c i", i=128), in0=rot[:, dc, :].rearrange("p (c i) -> p c i", i=128),
                        in1=gq[:, h:h + 1, :].to_broadcast([128, 4, 128]), op=ALU.mult), reads=[Trot, Tc], writes=[Tqd])
                rotary(256, kb, Tkb, True)
                for ci in range(4):
                    for dc in range(2):
                        def tr(e, ci=ci, dc=dc):
                            return e.transpose(out=ps[:, 4, dc * 128:(dc + 1) * 128], in_=rot[:, dc, ci * 128:(ci + 1) * 128], identity=ident[:])
                        P.op("pe", tr, reads=[Trot, Tc], writes=[C.Tps[4]])
                    P.op("act", lambda e, ci=ci: e.activation(out=kdt[:, ci, :], in_=ps[:, 4, 0:256], func=AF.Copy, scale=kdcd[:, h:h + 1]),
                         reads=[C.Tps[4], Tc], writes=[Tkdt])
                for ci in range(4):
                    b = C.bank("pj", (0, 1, 2, 3))
                    mm_group(C, b, [(hbuf[:, k, ci * 128:(ci + 1) * 128], w[h][:, k, 512:1024]) for k in range(KC)], reads=[Tw[h], Th])
                    P.op("act", lambda e, ci=ci, b=b: e.copy(out=vt[:, ci, :], in_=ps[:, b, :]), reads=[C.Tps[b]], writes=[Tvt])
                for vc in range(4):
                    b = proj_fm(1024 + vc * 128, (0, 1, 2, 3))
                    P.op("act", lambda e, vc=vc, b=b: e.activation(out=sg[:, vc, :], in_=ps[:, b, :], func=AF.Silu), reads=[C.Tps[b]], writes=[Tsg])
                for ci in range(4):
                    cs_ = slice(ci * 128, (ci + 1) * 128)
                    first = (tt == 0 and ci == 0)
                    mm_group(C, 4, [(kb[:, dc, cs_], qb[:, dc, cs_]) for dc in range(2)], reads=[Tkb, Tqb], n=128)
                    P.op("dve", lambda e: e.tensor_tensor(out=at[:], in0=ps[:, 4, 0:128], in1=dmat[:, h, :], op=ALU.mult),
                         reads=[C.Tps[4], Tc], writes=[Tat])

                    for dc in range(2):
                        b = 6 + dc
                        mm_group(C, b, [(kdt[:, ci, dc * 128:(dc + 1) * 128], vt[:, ci, :])], reads=[Tkdt, Tvt])
                        if first:
                            P.op("dve", lambda e, dc=dc, b=b: e.tensor_copy(out=state[h][:, dc, :], in_=ps[:, b, :]), reads=[C.Tps[b]], writes=[Tst[h][dc]])
                        else:
                            P.op("dve", lambda e, dc=dc, b=b: e.scalar_tensor_tensor(
                                out=state[h][:, dc, :], in0=state[h][:, dc, :], scalar=kdcd[:, 2 + h:3 + h], in1=ps[:, b, :],
                                op0=ALU.mult, op1=ALU.add), reads=[C.Tps[b], Tst[h][dc], Tc], writes=[Tst[h][dc]])
                    def omm(e, ci=ci, cs_=cs_, first=first):
                        ins = None
                        for vc in range(4):
                            vs = slice(vc * 128, (vc + 1) * 128)
                            ins = e.matmul(ps[:, 5, vs], lhsT=vt[:, ci, vs], rhs=at[:], start=True, stop=first)
                            if not first:
                                for dc in range(2):
                                    ins = e.matmul(ps[:, 5, vs], lhsT=stb[h][:, dc, vs], rhs=qd[:, dc, cs_], start=False, stop=(dc == 1))
                        return ins
                    P.op("pe", omm, reads=[Tvt, Tat, Tstb[h], Tqd], writes=[C.Tps[5]])
                    P.op("act", lambda e, cs_=cs_: e.copy(out=of[:, :, cs_], in_=ps[:, 5, :].rearrange("p (v i) -> p v i", i=128)),
                         reads=[C.Tps[5]], writes=[Tof[0][0]])
                    P.op("pool", lambda e: e.tensor_copy(out=stb[h][:], in_=state[h][:]), reads=Tst[h], writes=[Tstb[h]])
                for vc in range(4):
                    P.op("act", lambda e, vc=vc: e.activation(out=sq[:, vc, :], in_=of[:, vc, :], func=AF.Square), reads=[Tof[0][0]], writes=[Tsq[vc]])
                b = C.bank("pj", (0, 1, 2, 3))
                mm_group(C, b, [(C.ones[:], sq[:, vc, :]) for vc in range(4)], reads=[C.Tones] + Tsq)
                P.op("act", lambda e, b=b: e.activation(out=rstd[:], in_=ps[:, b, :], func=AF.Sqrt, scale=1.0 / RDV, bias=C.eps[:]),
                     reads=[C.Tps[b], C.Tones], writes=[Trstd])
                P.op("dve", lambda e: e.reciprocal(out=rstd[:], in_=rstd[:]), reads=[Trstd], writes=[Trstd])
                for vc in range(4):
                    P.op("dve", lambda e, vc=vc: e.tensor_tensor(out=of[:, vc, :], in0=of[:, vc, :], in1=rstd[:], op=ALU.mult),
                         reads=[Tof[0][0], Trstd], writes=[Tof[0][0]])
                    P.op("pool", lambda e, vc=vc: e.tensor_tensor(out=obuf[:, h * 4 + vc, :], in0=of[:, vc, :], in1=sg[:, vc, :], op=ALU.mult),
                         reads=[Tof[0][0], Tsg], writes=[Tobuf])
            for h in range(2):
                head_body(h)
            out_toks.append(P.dma("sp", [lambda e, c=c, ts=ts, obuf=obuf: e.dma_start(out=oT[c0 + c, :, ts], in_=obuf[:, c, :]) for c in range(8)],
                                  reads=[Tobuf], sem="o%d" % (tt % 2)))
    return out_toks


TL = 1919
TU = 1792
MASKNEG = -30000.0


def rel_bucket_np(n):
    n = np.maximum(n, 0)
    nf = np.maximum(n, 1).astype(np.float32)
    large = 16 + (np.log(nf / np.float32(16)) / np.float32(math.log(1024 / 16)) * np.float32(16)).astype(np.int32)
    large = np.minimum(large, 31)
    return np.where(n < 16, n, large)


def attn_consts():
    dist = np.arange(TL) - 511
    oh = np.zeros((33, TL), np.float32)
    bk = rel_bucket_np(dist)
    for j in range(TL):
        if dist[j] < 0:
            oh[32, j] = 1.0
        else:
            oh[bk[j], j] = 1.0
    cneg = np.zeros((16, 16), np.float32)
    negown = np.full((16, 16), MASKNEG, np.float32)
    for b in range(16):
        cneg[b, b:] = -2e30
        negown[b, b] = 0.0
    e16 = np.zeros((16, 16, 128), np.float32)
    for n in range(16):
        e16[n, n, :] = 1.0
    return oh, np.broadcast_to(cneg[None], (128, 16, 16)).copy(), np.broadcast_to(negown[None], (128, 16, 16)).copy(), e16


def emit_attn(C, kind, lam_init, h_src, win, r, rb_full, qkn_d, oh_d, ident_d, cneg_d, negown_d, e16_d, lam_d, sub_d, Rd, o_dst, c0,
              nqt=S // 512):
    moba = (kind == "moba")
    if True:
        P = C.P
        ps = C.ps
        hT = h_src
        oT = o_dst
        win_segs = [(0, r * 512), (512, 1024 + r * 512), (1024, 2048 + r * 512)]
        rb_d = rb_full[:, r * 8:(r + 1) * 8]
        w = C.sb("w", [128, KC, 1536], BF16); Tw = T("w")
        P.dma("pool", [lambda e, d0=d0, s0=s0: e.dma_start(out=w[:, :, d0:d0 + 512], in_=win[:, s0:s0 + 512].rearrange("(k p) n -> p k n", p=128))
                       for d0, s0 in win_segs], writes=[Tw], sem="w")
        rbx = C.sb("rbx", [33, 8], F32)
        qkn = C.sb("qkn_s", [128, 2], F32)
        oh = C.sb("oh_s", [33, TL], F32)
        ident = C.sb("ident_s", [128, 128], F32)
        Tc = T("consts")
        fns = [lambda e: e.dma_start(out=rbx[0:32, :], in_=rb_d), lambda e: e.dma_start(out=qkn[:], in_=qkn_d),
               lambda e: e.dma_start(out=oh[:], in_=oh_d), lambda e: e.dma_start(out=ident[:], in_=ident_d)]
        if moba:
            cneg = C.sb("cneg_s", [128, 16, 16], F32)
            negown = C.sb("negown_s", [128, 16, 16], F32)
            fns += [lambda e: e.dma_start(out=cneg[:], in_=cneg_d), lambda e: e.dma_start(out=negown[:], in_=negown_d)]
        else:
            lam = C.sb("lam_s", [1, 256], F32)
            subw = C.sb("subw_s", [128, 1], F32)
            fns += [lambda e: e.dma_start(out=lam[:], in_=lam_d), lambda e: e.dma_start(out=subw[:], in_=sub_d)]
        P.dma("sp", fns, writes=[Tc], sem="c")
        if moba:
            e16 = C.sb("e16_s", [128, 16, 128], BF16)
            Te16 = T("e16")
            P.op("pool", lambda e: e.memset(e16[:], 0.0), writes=[Te16])
            P.dma("pool", lambda e: e.dma_start(out=e16[0:16], in_=e16_d), writes=[Te16], sem="e16")
        P.op("pool", lambda e: e.memset(rbx[32:33, :], MASKNEG), reads=[Tc], writes=[Tc])
        identr = C.sb("identr", [128, 128], F32R)
        P.op("act", lambda e: e.copy(out=identr[:], in_=ident[:]), reads=[Tc], writes=[Tc])
        P.op("dve", lambda e: e.tensor_scalar(out=qkn[:, 0:1], in0=qkn[:, 0:1], scalar1=0.125, scalar2=None, op0=ALU.mult), reads=[Tc], writes=[Tc])
        ones33 = C.sb("ones33", [33, 128], F32)
        onesf = C.sb("onesf", [1, 128], F32)
        bones = C.sb("bones", [128, 128], BF16)
        Tk = T("kconst")
        P.op("pool", lambda e: e.memset(ones33[:], 1.0), writes=[Tk])
        P.op("pool", lambda e: e.memset(onesf[:], 1.0), writes=[Tk])
        P.op("pool", lambda e: e.memset(bones[:], 0.0), writes=[Tk])
        P.op("pool", lambda e: e.memset(bones[0:64, 0:64], 1.0), writes=[Tk])
        P.op("pool", lambda e: e.memset(bones[64:128, 64:128], 1.0), writes=[Tk])
        hm = C.sb("hm", [128, 2], F32)
        P.op("pool", lambda e: e.memset(hm[:], 0.0), writes=[Tk])
        P.op("pool", lambda e: e.memset(hm[0:64, 0:1], 1.0), writes=[Tk])
        P.op("pool", lambda e: e.memset(hm[64:128, 1:2], 1.0), writes=[Tk])
        c31 = C.sb("c31", [128, 8], F32); Tc31 = T("c31")
        brep = C.sb("brep", [33, 128], F32); Tbrep = T("brep")
        rsb = C.sb("rsb", [128, TL], F32); Trsb = T("rsb")
        TRd = [T("Rd%d" % h) for h in range(8)]

        def build_strip(hh):
            P.op("dve", lambda e: e.tensor_scalar(out=brep[:], in0=ones33[:], scalar1=rbx[:, hh:hh + 1], scalar2=None, op0=ALU.mult),
                 reads=[Tc, Tk], writes=[Tbrep])
            for cb in range(4):
                c0, c1 = cb * 512, min(TL, (cb + 1) * 512)
                b = C.bank("pj", (0, 1, 2, 3))

                def mmf(e, c0=c0, c1=c1, b=b):
                    return e.matmul(ps[:, b, 0:c1 - c0], lhsT=brep[:], rhs=oh[:, c0:c1], start=True, stop=True)
                P.op("pe", mmf, reads=[Tbrep, Tc], writes=[C.Tps[b]])
                P.op("act", lambda e, c0=c0, c1=c1, b=b: e.copy(out=rsb[:, c0:c1], in_=ps[:, b, 0:c1 - c0]), reads=[C.Tps[b]], writes=[Trsb])
            P.op("dve", lambda e: e.tensor_copy(out=c31[:, hh:hh + 1], in_=rsb[:, TL - 1:TL]), reads=[Trsb], writes=[Tc31])
            P.dma("sp", lambda e: e.dma_start(out=Rd.ap()[hh], in_=rsb[:]), reads=[Trsb], writes=[TRd[hh]], sem="rd")
        for hh in range(8):
            build_strip(hh)

        if not moba:
            lp = C.sb("lp", [1, 128], F32); l2 = C.sb("l2", [1, 2], F32); nlam = C.sb("nlam", [128, 1], F32); Tl = T("lam")
            P.op("dve", lambda e: e.tensor_tensor(out=lp[:, 0:64], in0=lam[:, 0:64], in1=lam[:, 64:128], op=ALU.mult), reads=[Tc], writes=[Tl])
            P.op("dve", lambda e: e.tensor_tensor(out=lp[:, 64:128], in0=lam[:, 128:192], in1=lam[:, 192:256], op=ALU.mult), reads=[Tc, Tl], writes=[Tl])
            P.op("dve", lambda e: e.reduce_sum(out=l2[:], in_=lp[:].rearrange("p (a b) -> p a b", a=2), axis=AX.X), reads=[Tl], writes=[Tl])
            P.op("act", lambda e: e.activation(out=l2[:], in_=l2[:], func=AF.Exp), reads=[Tl], writes=[Tl])
            P.op("dve", lambda e: e.tensor_tensor(out=lp[:, 0:1], in0=l2[:, 1:2], in1=l2[:, 0:1], op=ALU.subtract), reads=[Tl], writes=[Tl])
            P.op("dve", lambda e: e.tensor_scalar(out=lp[:, 0:1], in0=lp[:, 0:1], scalar1=-float(lam_init), scalar2=None, op0=ALU.add), reads=[Tl], writes=[Tl])

            def mml(e):
                return e.matmul(ps[:, 3, 0:1], lhsT=onesf[:], rhs=lp[:, 0:1], start=True, stop=True)
            P.op("pe", mml, reads=[Tl, Tk], writes=[C.Tps[3]])
            P.op("act", lambda e: e.copy(out=nlam[:], in_=ps[:, 3, 0:1]), reads=[C.Tps[3]], writes=[Tl])
            P.op("dve", lambda e: e.tensor_scalar(out=subw[:], in0=subw[:], scalar1=float(1.0 - lam_init), scalar2=None, op0=ALU.mult), reads=[Tc], writes=[Tc])

        hb = [C.sb("hb%d" % i, [128, KC, 512], BF16) for i in range(2)]
        Thb = [T("hb%d" % i) for i in range(2)]
        if not moba:
            qT = C.sb("qT", [128, S], BF16)
        TqT = T("qT")
        kT = C.sb("kT", [128, S], BF16); TkT = T("kT")
        qz = [C.sb("qz%d" % j, [128, S], BF16) for j in range(2)]
        Tqz = [T("qz%d" % j) for j in range(2)]
        vt = C.sb("vt", [128, 32, 512], BF16); Tvt = T("vt")
        sq = C.sb("sq", [128, 512], BF16); Tsq = T("sq")
        rstd = C.sb("rstd", [128, 512], F32); Trstd = T("rstd")
        tsk = [C.sb("tsk%d" % j, [128, TU], F32) for j in range(2)]
        Ttsk = [T("tsk%d" % j) for j in range(2)]
        pt = [C.sb("pt%d" % i, [128, 512], BF16) for i in range(6)]
        Tpt = [T("pt%d" % i) for i in range(6)]
        tmp = [C.sb("tmp%d" % i, [128, 512], F32) for i in range(1)]
        Ttmp = [T("tmp%d" % i) for i in range(1)]
        ob = [C.sb("ob%d" % i, [128, 512], BF16) for i in range(2)]
        Tob = [T("ob%d" % i) for i in range(2)]
        if moba:
            qF = C.sb("qF", [128, S], F32); TqF = T("qF")
            kF = C.sb("kF", [128, S], F32); TkF = T("kF")
            km = C.sb("km", [128, 16], F32); Tkm = T("km")
            gm = C.sb("gm", [128, 16], F32); Tgm = T("gm")
            top8 = C.sb("top8", [128, 8], F32); Ttop = T("top8")
            mneg = C.sb("mneg", [128, 16], F32); Tmn = T("mneg")
            mT = [C.sb("mT%d" % j, [128, S], BF16) for j in range(2)]
            TmT = [T("mT%d" % j) for j in range(2)]
            for j in range(2):
                P.op("pool", lambda e, j=j: e.memset(mT[j][:], 0.0), writes=[TmT[j]])
        r0 = C.sb("r0", [128, 512], F32); Tr0 = T("r0")
        a0 = C.sb("a0", [128, 512], F32); Ta0 = T("a0")
        if not moba:
            a1 = C.sb("a1", [128, 512], F32); Ta1 = T("a1")
        out_toks = []
        cnt = {"h": 0, "pt": 0, "tmp": 0, "ob": 0}

        def qk_norm(b, wcol, dst_main, Tdst, ts, dstF=None, TdstF=None):
            P.op("act", lambda e: e.activation(out=sq[:], in_=ps[:, b, :], func=AF.Square), reads=[C.Tps[b]], writes=[Tsq])
            b2 = C.bank("nrm", (4, 5))
            mm_group(C, b2, [(bones[:], sq[:])], reads=[Tk, Tsq])
            P.op("act", lambda e: e.activation(out=rstd[:], in_=ps[:, b2, :], func=AF.Sqrt, scale=1.0 / 64, bias=C.eps[:]),
                 reads=[C.Tps[b2], C.Tones], writes=[Trstd])
            P.op("dve", lambda e: e.reciprocal(out=rstd[:], in_=rstd[:]), reads=[Trstd], writes=[Trstd])
            if dstF is not None:
                P.op("dve", lambda e: e.scalar_tensor_tensor(out=dstF[:, ts], in0=ps[:, b, :], scalar=qkn[:, wcol:wcol + 1], in1=rstd[:],
                                                             op0=ALU.mult, op1=ALU.mult), reads=[C.Tps[b], Tc, Trstd], writes=[TdstF])
                if dst_main is not None:
                    P.op("pool", lambda e: e.tensor_copy(out=dst_main[:, ts], in_=dstF[:, ts]), reads=[TdstF], writes=[Tdst])
            else:
                P.op("dve", lambda e: e.scalar_tensor_tensor(out=dst_main[:, ts], in0=ps[:, b, :], scalar=qkn[:, wcol:wcol + 1], in1=rstd[:],
                                                             op0=ALU.mult, op1=ALU.mult), reads=[C.Tps[b], Tc, Trstd], writes=[Tdst])

        def project(p, tt):
            ts = slice(tt * 512, (tt + 1) * 512)
            i = cnt["h"] % 2
            cnt["h"] += 1
            hbuf, Th = hb[i], Thb[i]
            P.dma("sp", [lambda e, k=k: e.dma_start(out=hbuf[:, k, :], in_=hT[k, :, ts]) for k in range(KC)], writes=[Th], sem="h%d" % i)
            for which, (dst, Td, dF, TdF) in enumerate([(None if moba else qT, TqT, qF if moba else None, TqF if moba else None),
                                                        (kT, TkT, kF if moba else None, TkF if moba else None)]):
                b = C.bank("pj", (0, 1, 2, 3))
                col0 = which * 512 + p * 128
                mm_group(C, b, [(w[:, k, col0:col0 + 128], hbuf[:, k, :]) for k in range(KC)], reads=[Tw, Th])
                qk_norm(b, which, dst, Td, ts, dF, TdF)
                if which == 0:
                    for j in range(2):
                        qsrc, Tqsrc = (qF, TqF) if moba else (qT, TqT)
                        P.op("pool", lambda e, j=j: e.tensor_scalar(out=qz[j][:, ts], in0=qsrc[:, ts], scalar1=hm[:, j:j + 1], scalar2=None, op0=ALU.mult),
                             reads=[Tqsrc, Tk], writes=[Tqz[j]])
            if p == 0:
                for ci in range(4):
                    b = C.bank("pj", (0, 1, 2, 3))
                    mm_group(C, b, [(hbuf[:, k, ci * 128:(ci + 1) * 128], w[:, k, 1024:1536]) for k in range(KC)], reads=[Tw, Th])
                    P.op("act", lambda e, ci=ci, b=b: e.copy(out=vt[:, tt * 4 + ci, :], in_=ps[:, b, :]), reads=[C.Tps[b]], writes=[Tvt])

        def gate_tile(j, g):
            hp = slice(64 * j, 64 * j + 64)
            gs = slice(g * 128, (g + 1) * 128)
            blk = g // 2

            def mg(e):
                return e.matmul(ps[:, 6, 0:16], lhsT=qF[hp, gs], rhs=km[hp, :], start=True, stop=True)
            P.op("pe", mg, reads=[TqF, Tkm], writes=[C.Tps[6]])
            P.op("dve", lambda e: e.tensor_tensor(out=gm[:], in0=ps[:, 6, 0:16], in1=cneg[:, blk, :], op=ALU.add), reads=[C.Tps[6], Tc], writes=[Tgm])
            P.op("dve", lambda e: e.max(out=top8[:], in_=gm[:]), reads=[Tgm], writes=[Ttop])
            P.op("dve", lambda e: e.tensor_scalar(out=top8[:, 2:3], in0=top8[:, 2:3], scalar1=-1e30, scalar2=None, op0=ALU.max), reads=[Ttop], writes=[Ttop])
            P.op("dve", lambda e: e.scalar_tensor_tensor(out=mneg[:], in0=gm[:], scalar=top8[:, 2:3], in1=negown[:, blk, :], op0=ALU.is_lt, op1=ALU.mult),
                 reads=[Tgm, Ttop, Tc], writes=[Tmn])

            def tr(e):
                return e.transpose(out=ps[0:16, 7, 0:128], in_=mneg[:], identity=ident[:])
            P.op("pe", tr, reads=[Tmn, Tc], writes=[C.Tps[7]])
            P.op("act", lambda e: e.copy(out=mT[j][0:16, gs], in_=ps[0:16, 7, 0:128]), reads=[C.Tps[7]], writes=[TmT[j]])

        def stage1(p, j, qt, kt):
            hp = slice(64 * j, 64 * j + 64)
            qs = slice(qt * 512, (qt + 1) * 512)
            ks = slice(kt * 128, (kt + 1) * 128)
            delta = qt * 512 - kt * 128
            bs = C.bank("s", (0, 1, 2, 3))

            near = delta < 1024

            def ms(e):
                ins = e.matmul(ps[:, bs, :], lhsT=kT[:, ks], rhs=qz[j][:, qs], start=True, stop=not (moba or near))
                if moba:
                    ins = e.matmul(ps[:, bs, :], lhsT=e16[:, kt // 2, :], rhs=mT[j][:, qs], start=False, stop=not near)
                if near:
                    ins = e.matmul(ps[:, bs, :], lhsT=identr[:], rhs=tsk[j][:, delta + 384:delta + 384 + 512].bitcast(F32R),
                                   start=False, stop=True)
                return ins
            P.op("pe", ms, reads=[TkT, Tqz[j], Tc] + ([Te16, TmT[j]] if moba else []) + ([Ttsk[j]] if near else []), writes=[C.Tps[bs]])
            ip = cnt["pt"] % len(pt)
            cnt["pt"] += 1
            if near:
                P.op("act", lambda e: e.activation(out=pt[ip][:], in_=ps[:, bs, :], func=AF.Exp), reads=[C.Tps[bs]], writes=[Tpt[ip]])
            else:
                P.op("act", lambda e: e.activation(out=pt[ip][:], in_=ps[:, bs, :], func=AF.Exp, bias=c31[:, 2 * p + j:2 * p + j + 1]),
                     reads=[C.Tps[bs], Tc31], writes=[Tpt[ip]])
            return ip

        def stage2(p, j, kt, ip, bo, bl, first, last):
            hp = slice(64 * j, 64 * j + 64)

            def mpv(e):
                e.matmul(ps[:, bo, :], lhsT=vt[:, kt, p * 128:(p + 1) * 128], rhs=pt[ip][:], start=first, stop=last)
                return e.matmul(ps[:, bl, :], lhsT=C.ones[:], rhs=pt[ip][:], start=first, stop=last)
            P.op("pe", mpv, reads=[Tvt, Tpt[ip], C.Tones], writes=[C.Tps[bo], C.Tps[bl]])

        def finish_head(j, bo, bl):
            if moba:
                hp = slice(64 * j, 64 * j + 64)
                P.op("dve", lambda e: e.reciprocal(out=r0[hp, :], in_=ps[hp, bl, :]), reads=[C.Tps[bl]], writes=[Tr0])
                P.op("dve", lambda e: e.tensor_tensor(out=a0[hp, :], in0=ps[hp, bo, :], in1=r0[hp, :], op=ALU.mult), reads=[C.Tps[bo], Tr0], writes=[Ta0])
                return
            a, Ta = (a0, Ta0) if j == 0 else (a1, Ta1)
            P.op("dve", lambda e: e.reciprocal(out=r0[:], in_=ps[:, bl, :]), reads=[C.Tps[bl]], writes=[Tr0])
            P.op("dve", lambda e: e.tensor_tensor(out=a[:], in0=ps[:, bo, :], in1=r0[:], op=ALU.mult), reads=[C.Tps[bo], Tr0], writes=[Ta])

        def finish_qt(p, qt, bo, bl):
            qs = slice(qt * 512, (qt + 1) * 512)
            io = cnt["ob"] % 2
            cnt["ob"] += 1
            if moba:
                P.op("pool", lambda e: e.tensor_copy(out=ob[io][:], in_=a0[:]), reads=[Ta0], writes=[Tob[io]])
            else:
                P.op("dve", lambda e: e.scalar_tensor_tensor(out=a0[:], in0=a1[:], scalar=nlam[:], in1=a0[:], op0=ALU.mult, op1=ALU.add),
                     reads=[Ta0, Ta1, Tl], writes=[Ta0])
                P.op("act", lambda e: e.activation(out=sq[:], in_=a0[:], func=AF.Square), reads=[Ta0], writes=[Tsq])
                bn = C.bank("s", (0, 1, 2, 3))
                mm_group(C, bn, [(C.ones[:], sq[:])], reads=[C.Tones, Tsq])
                P.op("act", lambda e: e.activation(out=rstd[:], in_=ps[:, bn, :], func=AF.Sqrt, scale=1.0 / 128, bias=C.eps[:]),
                     reads=[C.Tps[bn], C.Tones], writes=[Trstd])
                P.op("dve", lambda e: e.reciprocal(out=rstd[:], in_=rstd[:]), reads=[Trstd], writes=[Trstd])
                P.op("dve", lambda e: e.scalar_tensor_tensor(out=ob[io][:], in0=a0[:], scalar=subw[:], in1=rstd[:], op0=ALU.mult, op1=ALU.mult),
                     reads=[Ta0, Tc, Trstd], writes=[Tob[io]])
            out_toks.append(P.dma("sp", lambda e: e.dma_start(out=oT[c0 + p, :, qs], in_=ob[io][:]), reads=[Tob[io]], sem="o%d" % io))

        def attention(p):
            PD = 3
            tiles = []
            for qt in range(nqt):
                nk = 4 * qt + 4
                if True:
                    for j in range(2):
                        bo, bl = 4 + 2 * j, 5 + 2 * j
                        for kt in range(nk):
                            lastt = (kt == nk - 1)
                            tiles.append((j, qt, kt, bo, bl, kt == 0, lastt, (j, bo, bl) if lastt else None, (qt, bo, bl) if (lastt and j == 1) else None))
            pend = []

            def retire():
                (j, qt, kt, bo, bl, first, last, fh, fq), ip = pend.pop(0)
                stage2(p, j, kt, ip, bo, bl, first, last)
                if fh is not None:
                    finish_head(*fh)
                if fq is not None:
                    finish_qt(p, *fq)
            for tl in tiles:
                ip = stage1(p, tl[0], tl[1], tl[2])
                pend.append((tl, ip))
                if len(pend) > PD:
                    retire()
            while pend:
                retire()

        def load_tsk(p, j):
            hh = 2 * p + j
            src = bass.AP(Rd, hh * 128 * TL + 127, [[TL - 1, 128], [1, TU]])
            P.dma("sp", lambda e: e.dma_start(out=tsk[j][:], in_=src), reads=[TRd[hh]], writes=[Ttsk[j]], sem="tsk%d" % j)
            P.op("act", lambda e: e.copy(out=tsk[j][:].bitcast(F32R), in_=tsk[j][:]), reads=[Ttsk[j]], writes=[Ttsk[j]])

        for p in range(4):
            for tt in range(8):
                project(p, tt)
            for j in range(2):
                load_tsk(p, j)
            if moba:
                P.op("dve", lambda e: e.reduce_sum(out=km[:], in_=kF[:].rearrange("p (n t) -> p n t", t=256), axis=AX.X), reads=[TkF], writes=[Tkm])
                for j in range(2):
                    for g in range(4 * nqt):
                        gate_tile(j, g)
            attention(p)
    return out_toks


def build_fused():
    nc = bass.Bass("TRN2", target_bir_lowering=False)

    def inp(name, shape, dt=F32):
        return nc.dram_tensor(name, shape, dt, kind="ExternalInput").ap()
    xT = inp("xT", [KC, 128, S])
    n1 = inp("n1", [4, 128, KC]); n2 = inp("n2", [4, 128, KC])
    wup = inp("wup", [4, D, DFF]); wdown = inp("wdown", [4, DFF, D])
    retin = inp("retin", [2, D, 6144]); retout = inp("retout", [2, 2048, D])
    mobain = inp("mobain", [D, 3072]); mobaout = inp("mobaout", [D, D])
    diffin = inp("diffin", [D, 3072]); diffout = inp("diffout", [D, D])
    rb = inp("rb", [32, 16]); qknm = inp("qknm", [128, 2]); qknd = inp("qknd", [128, 2])
    lam = inp("lam", [1, 256]); subw = inp("subw", [128, 1])
    cs_d = inp("cs", [2, 128, S]); dmat_d = inp("dmat", [4, 128, 128]); gq_d = inp("gq", [4, 128, 128]); kdcd_d = inp("kdcd", [128, 8])
    ident_d = inp("ident", [128, 128]); oh_d = inp("oh", [33, TL])
    cneg_d = inp("cneg", [128, 16, 16]); negown_d = inp("negown", [128, 16, 16]); e16_d = inp("e16", [16, 16, 128])
    xs = nc.dram_tensor("xs", [KC, 128, S], F32, kind="Internal").ap()
    hs = nc.dram_tensor("hs", [KC, 128, S], BF16, kind="Internal").ap()
    osc = nc.dram_tensor("osc", [16, 128, S], BF16, kind="Internal").ap()
    Rd = nc.dram_tensor("Rscr", [8, 128, TL], F32, kind="Internal")
    xo = nc.dram_tensor("xo", [KC, 128, S], F32, kind="ExternalOutput").ap()

    with ExitStack() as st:
        C = Ctx(nc, st)
        P = C.P

        def phase(fn):
            P.barrier()
            with ExitStack() as ph:
                C.ph = ph
                C.bank_rr = {}
                fn()
            C.ph = None
        phase(lambda: emit_phase_a(C, S, xT, None, n1=n1[0], h_dst=hs))
        for i in range(4):
            kind, j = i % 3, i // 3
            for r in range(2):
                if kind == 0:
                    phase(lambda: emit_ret(C, hs, retin[j], (2 * r, 2 * r + 1), cs_d, dmat_d, gq_d, kdcd_d, ident_d, osc, 8 * r))
                elif kind == 1:
                    phase(lambda: emit_attn(C, "moba", 0.0, hs, mobain, r, rb, qknm, oh_d, ident_d, cneg_d, negown_d, e16_d, None, None, Rd, osc, 4 * r))
                else:
                    lam_init = 0.8 - 0.6 * math.exp(-0.3 * i)
                    phase(lambda: emit_attn(C, "diff", lam_init, hs, diffin, r, rb, qknd, oh_d, ident_d, None, None, None, lam, subw, Rd, osc, 4 * r))
            wout, fo = ((retout[j], 2048) if kind == 0 else ((mobaout, 1024) if kind == 1 else (diffout, 1024)))
            last = (i == 3)
            phase(lambda: emit_phase_a(C, S, xT if i == 0 else xs, xo if last else xs, o_src=osc, wout=wout, fo=fo, wup=wup[i], wdown=wdown[i],
                                       n2=n2[i], n1=None if last else n1[i + 1], h_dst=None if last else hs))
        P.barrier()
        P.emit(st)
    return nc


_NC_CACHE = {}


def _fm(a):
    return np.ascontiguousarray(a.T.reshape(a.shape[1] // 128, 128, a.shape[0]))


def _unfm(a):
    return np.ascontiguousarray(a.reshape(-1, a.shape[2]).T)


def _nw(w):
    return np.ascontiguousarray(w.reshape(w.shape[0], KC, 128).transpose(0, 2, 1))


def kernel(x, rel_bias, norm1, norm2, w_up, w_down, ret_w_in, ret_w_out,
           moba_w_in, moba_q_norm, moba_k_norm, moba_w_out,
           diff_w_in, diff_q_norm, diff_k_norm, diff_lambda, diff_subln, diff_w_out):
    f32 = np.float32
    A = lambda a: np.ascontiguousarray(np.asarray(a, f32))
    x = A(x)
    if "fused" not in _NC_CACHE:
        _NC_CACHE["fused"] = build_fused()
    nc = _NC_CACHE["fused"]
    cs, ph = ret_consts()
    oh, cneg, negown, e16 = attn_consts()
    shared = {
        "n1": _nw(A(norm1)), "n2": _nw(A(norm2)), "wup": A(w_up), "wdown": A(w_down),
        "retin": A(ret_w_in), "retout": A(ret_w_out), "mobain": A(moba_w_in[0]), "mobaout": A(moba_w_out[0]),
        "diffin": A(diff_w_in[0]), "diffout": A(diff_w_out[0]), "rb": A(rel_bias),
        "qknm": A(np.stack([np.tile(A(moba_q_norm[0]), 2), np.tile(A(moba_k_norm[0]), 2)], axis=1)),
        "qknd": A(np.stack([np.tile(A(diff_q_norm[0]), 2), np.tile(A(diff_k_norm[0]), 2)], axis=1)),
        "lam": A(diff_lambda[0]).reshape(1, 256), "subw": A(diff_subln[0]).reshape(128, 1),
        "cs": cs, "dmat": A(np.stack([ph[h][0] for h in range(4)])), "gq": A(np.stack([ph[h][1] for h in range(4)])),
        "kdcd": A(np.stack([ph[h][2] for h in range(4)] + [ph[h][3] for h in range(4)], axis=1)),
        "ident": np.eye(128, dtype=f32), "oh": oh, "cneg": cneg, "negown": negown, "e16": e16,
    }
    ims = []
    for c in range(8):
        d = dict(shared)
        d["xT"] = _fm(x[c % 4])
        ims.append(d)
    res = run_bass_kernel_spmd(nc, ims, core_ids=list(range(8))).results
    out = np.empty((4, S, D), f32)
    for b in range(4):
        out[b] = _unfm(res[b]["xo"])
    return out
```

```python
import math
from contextlib import ExitStack
import numpy as np
import concourse.bass as bass
import concourse.mybir as mybir
from concourse.bass_utils import run_bass_kernel_spmd

F32 = mybir.dt.float32
BF16 = mybir.dt.bfloat16
F32R = mybir.dt.float32r
AF = mybir.ActivationFunctionType
ALU = mybir.AluOpType
AX = mybir.AxisListType

D = 1024
KC = 8
S = 4096
NTOK = 2048
DFF = 4096
EPS = 1e-6
ENGS = ("pe", "act", "dve", "pool", "sp")


class T:
    __slots__ = ("name", "w", "r")

    def __init__(self, name=""):
        self.name = name
        self.w = None
        self.r = {}


class Prog:
    def __init__(self, nc, same_engine_sync=True):
        self.nc = nc
        self.ops = {e: [] for e in ENGS}
        self.cnt = {}
        self.seen = {e: {} for e in ENGS}
        self.same = same_engine_sync
        self.dma_sems = {}

    def _deps(self, eng, reads, writes):
        deps = {}

        def add(tok):
            if tok is None:
                return
            k, v = tok
            if deps.get(k, 0) < v:
                deps[k] = v
        for t in reads:
            add(t.w)
        for t in writes:
            add(t.w)
            for r in t.r.items():
                add(r)
        waits = []
        for k, v in deps.items():
            if k == eng and (not self.same or eng == "pe"):
                continue
            if self.seen[eng].get(k, 0) >= v:
                continue
            self.seen[eng][k] = v
            waits.append((k, v))
        return waits

    def _mark(self, tok, reads, writes):
        for t in reads:
            if t.r.get(tok[0], 0) < tok[1]:
                t.r[tok[0]] = tok[1]
        for t in writes:
            t.w = tok
            t.r = {}

    def op(self, eng, fn, reads=(), writes=()):
        waits = self._deps(eng, reads, writes)
        self.cnt[eng] = self.cnt.get(eng, 0) + 1
        tok = (eng, self.cnt[eng])
        self._mark(tok, reads, writes)
        self.ops[eng].append((fn, waits, (eng, 1)))
        return tok

    def dma(self, q, fns, reads=(), writes=(), sem=None):
        if not isinstance(fns, (list, tuple)):
            fns = [fns]
        key = ("dma", sem)
        self.dma_sems[key] = None
        waits = self._deps(q, reads, writes)
        self.cnt[key] = self.cnt.get(key, 0) + 16 * len(fns)
        tok = (key, self.cnt[key])
        self._mark(tok, reads, writes)
        for i, fn in enumerate(fns):
            self.ops[q].append((fn, waits if i == 0 else [], (key, 16)))
        return tok

    def wait_all(self, eng, toks):
        best = {}
        for k, v in toks:
            best[k] = max(best.get(k, 0), v)
        waits = []
        for k, v in best.items():
            if self.seen[eng].get(k, 0) >= v:
                continue
            self.seen[eng][k] = v
            waits.append((k, v))
        self.ops[eng].append((None, waits, None))

    def barrier(self):
        toks = list(self.cnt.items())
        for e in ENGS:
            self.wait_all(e, toks)

    def emit(self, stack):
        nc = self.nc
        sems = {}
        for e in ENGS:
            if self.ops[e]:
                sems[e] = stack.enter_context(nc.semaphore("s_" + e))
        for k in self.dma_sems:
            sems[k] = stack.enter_context(nc.semaphore("d_%s" % (k[1],)))
        block = stack.enter_context(nc.Block())
        handles = {"pe": block.tensor, "act": block.scalar, "dve": block.vector,
                   "pool": block.gpsimd, "sp": block.sync}

        def make(e):
            def body(engine):
                for fn, waits, inc in self.ops[e]:
                    for k, v in waits:
                        engine.wait_ge(sems[k], v)
                    if fn is not None:
                        fn(engine).then_inc(sems[inc[0]], inc[1])
            return body
        for e in ENGS:
            if self.ops[e]:
                handles[e](make(e))


class Ctx:
    def __init__(self, nc, st):
        self.nc, self.st = nc, st
        self.P = Prog(nc)
        self.ps = st.enter_context(nc.psum_tensor("ps", [128, 8, 512], F32))
        self.Tps = [T("ps%d" % i) for i in range(8)]
        self.ones = st.enter_context(nc.sbuf_tensor("ones", [128, 128], BF16))
        self.Tones = T("ones")
        self.eps = st.enter_context(nc.sbuf_tensor("epsc", [128, 1], F32))
        self.P.op("pool", lambda e: e.memset(self.eps[:], EPS), writes=[self.Tones])
        self.P.op("pool", lambda e: e.memset(self.ones[:], 1.0), writes=[self.Tones])
        self.bank_rr = {}

    def sb(self, name, shape, dt):
        self.nalloc = getattr(self, "nalloc", 0) + 1
        stack = self.ph if getattr(self, "ph", None) is not None else self.st
        return stack.enter_context(self.nc.sbuf_tensor("%s_%d" % (name, self.nalloc), shape, dt))

    def bank(self, group, banks):
        i = self.bank_rr.get(group, 0)
        self.bank_rr[group] = i + 1
        return banks[i % len(banks)]


def mm_group(C, bank, mms, reads, n=512, prow=128):
    ps = C.ps

    def fn(e, mms=mms):
        ins = None
        for i, (l, r) in enumerate(mms):
            ins = e.matmul(ps[0:prow, bank, 0:n], lhsT=l, rhs=r, start=(i == 0), stop=(i == len(mms) - 1))
        return ins
    return C.P.op("pe", fn, reads=reads, writes=[C.Tps[bank]])


def rmsnorm_fm(C, x, Tx, w32, Tw, out, Tout, ntok, tmp, nd=KC, banks=(6, 7), tt_list=None, xoff=0):
    P = C.P
    sq, Tsq, rstd, Trstd = tmp
    nfeat = nd * 128
    for tt in (tt_list if tt_list is not None else range(ntok // 512)):
        ts = slice(tt * 512, (tt + 1) * 512)
        xs = slice(xoff + tt * 512, xoff + (tt + 1) * 512)
        b = C.bank("nrm", banks)
        for c in range(nd):
            P.op("act", lambda e, c=c, xs=xs: e.activation(out=sq[:, c, :], in_=x[:, c, xs], func=AF.Square),
                 reads=[Tx[c][tt]], writes=[Tsq[c]])
        mm_group(C, b, [(C.ones[:], sq[:, c, :]) for c in range(nd)], reads=[C.Tones] + Tsq[:nd])
        P.op("act", lambda e, b=b: e.activation(out=rstd[:], in_=C.ps[:, b, :], func=AF.Sqrt, scale=1.0 / nfeat, bias=C.eps[:]),
             reads=[C.Tps[b], C.Tones], writes=[Trstd])
        P.op("dve", lambda e: e.reciprocal(out=rstd[:], in_=rstd[:]), reads=[Trstd], writes=[Trstd])
        for c in range(nd):
            P.op("dve", lambda e, c=c, ts=ts, xs=xs: e.scalar_tensor_tensor(
                out=out[:, c, ts], in0=x[:, c, xs], scalar=w32[:, c:c + 1], in1=rstd[:], op0=ALU.mult, op1=ALU.mult),
                reads=[Tx[c][tt], Tw, Trstd], writes=[Tout[c][tt]])


def emit_phase_a(C, ntok, x_src, x_dst, o_src=None, wout=None, fo=0, wup=None, wdown=None, n2=None, n1=None, h_dst=None, TT=1024):
    P = C.P
    do_mix, do_ffn, do_next = o_src is not None, wup is not None, h_dst is not None
    NS = TT // 512
    out_toks = []
    x = C.sb("x", [128, KC, TT], F32)
    Tx = [[T("x") for _ in range(NS)] for c in range(KC)]
    sq = C.sb("sq", [128, KC, 512], BF16)
    Tsq = [T("sq") for c in range(KC)]
    rstd = C.sb("rstd", [128, 512], F32)
    tmpn = (sq, Tsq, rstd, T("rstd"))
    hb = C.sb("hb", [128, KC, TT], BF16)
    Thb = [[T("hb") for _ in range(NS)] for c in range(KC)]
    allx = [t for c in range(KC) for t in Tx[c]]
    allh = [t for c in range(KC) for t in Thb[c]]

    def load_w(name, src):
        w = C.sb(name, [128, KC], F32)
        Tw = T(name)
        P.dma("sp", lambda e: e.dma_start(out=w[:], in_=src), writes=[Tw], sem=name)
        return w, Tw
    if do_ffn or do_mix:
        u2 = C.sb("u2", [128, 32, TT], BF16)
        Tu2 = [T("u2") for f in range(32)]
    if do_mix:
        FK = fo // 128
        wo = [C.sb("wo", [128, FK, 128], BF16) for i in range(2)]
        Two = [T("wo") for i in range(2)]
    if do_ffn:
        w2, Tw2 = load_w("n2w", n2)
        wu = [C.sb("wu", [128, KC, 512], BF16) for i in range(2)]
        Twu = [T("wu") for i in range(2)]
        wd = [C.sb("wd", [128, 32, 256], BF16) for i in range(2)]
        Twd = [T("wd") for i in range(2)]
        rl = [C.sb("rl", [128, 512], F32) for i in range(2)]
        Trl = [T("rl") for i in range(2)]
    if do_next:
        w1, Tw1 = load_w("n1w", n1)
    cnt = {"wo": 0, "wu": 0, "wd": 0, "rl": 0}

    def tile_body(tt):
        t0 = tt * TT
        P.dma("sp", [lambda e, c=c: e.dma_start(out=x[:, c, :], in_=x_src[c, :, t0:t0 + TT]) for c in range(KC)], writes=allx, sem="xin")
        if do_mix:
            ob = u2
            P.dma("sp", [lambda e, k=k: e.dma_start(out=ob[:, k, :], in_=o_src[k, :, t0:t0 + TT]) for k in range(FK)], writes=Tu2[:FK], sem="oin")
            for n in range(KC):
                sl = cnt["wo"] % 2
                cnt["wo"] += 1
                P.dma("pool", lambda e, n=n, sl=sl: e.dma_start(
                    out=wo[sl][:], in_=wout[:, n * 128:(n + 1) * 128].rearrange("(k p) n -> p k n", p=128)),
                    writes=[Two[sl]], sem="wo%d" % sl)
                for s_ in range(NS):
                    ss = slice(s_ * 512, (s_ + 1) * 512)
                    b = C.bank("dn", (4, 5))
                    mm_group(C, b, [(wo[sl][:, k, :], ob[:, k, ss]) for k in range(FK)], reads=[Two[sl]] + Tu2[:FK])
                    P.op("dve", lambda e, n=n, ss=ss, b=b: e.tensor_tensor(out=x[:, n, ss], in0=x[:, n, ss], in1=C.ps[:, b, :], op=ALU.add),
                         reads=[C.Tps[b], Tx[n][s_]], writes=[Tx[n][s_]])
        if do_ffn:
            rmsnorm_fm(C, x, Tx, w2, Tw2, hb, Thb, TT, tmpn)
            for fg in range(8):
                sl = cnt["wu"] % 2
                cnt["wu"] += 1
                P.dma("pool", lambda e, fg=fg, sl=sl: e.dma_start(
                    out=wu[sl][:], in_=wup[:, fg * 512:(fg + 1) * 512].rearrange("(k p) f -> p k f", p=128)),
                    writes=[Twu[sl]], sem="wu%d" % sl)
                for fi in range(4):
                    f = fg * 4 + fi
                    for s_ in range(NS):
                        ss = slice(s_ * 512, (s_ + 1) * 512)
                        b = C.bank("up", (0, 1, 2, 3))
                        mm_group(C, b, [(wu[sl][:, c, fi * 128:(fi + 1) * 128], hb[:, c, ss]) for c in range(KC)],
                                 reads=[Twu[sl]] + [Thb[c][s_] for c in range(KC)])
                        r = cnt["rl"] % 2
                        cnt["rl"] += 1
                        P.op("act", lambda e, b=b, r=r: e.activation(out=rl[r][:], in_=C.ps[:, b, :], func=AF.Relu),
                             reads=[C.Tps[b]], writes=[Trl[r]])
                        P.op("dve", lambda e, f=f, r=r, ss=ss: e.tensor_tensor(out=u2[:, f, ss], in0=rl[r][:], in1=rl[r][:], op=ALU.mult),
                             reads=[Trl[r]], writes=[Tu2[f]])
            for ng in range(4):
                sl = cnt["wd"] % 2
                cnt["wd"] += 1
                P.dma("pool", lambda e, ng=ng, sl=sl: e.dma_start(
                    out=wd[sl][:], in_=wdown[:, ng * 256:(ng + 1) * 256].rearrange("(k p) n -> p k n", p=128)),
                    writes=[Twd[sl]], sem="wd%d" % sl)
                for ni in range(2):
                    n = ng * 2 + ni
                    for s_ in range(NS):
                        ss = slice(s_ * 512, (s_ + 1) * 512)
                        b = C.bank("dn", (4, 5))
                        mm_group(C, b, [(wd[sl][:, f, ni * 128:(ni + 1) * 128], u2[:, f, ss]) for f in range(32)],
                                 reads=[Twd[sl]] + Tu2)
                        P.op("dve", lambda e, n=n, ss=ss, b=b: e.tensor_tensor(out=x[:, n, ss], in0=x[:, n, ss], in1=C.ps[:, b, :], op=ALU.add),
                             reads=[C.Tps[b], Tx[n][s_]], writes=[Tx[n][s_]])
        if x_dst is not None:
            out_toks.append(P.dma("sp", [lambda e, c=c: e.dma_start(out=x_dst[c, :, t0:t0 + TT], in_=x[:, c, :]) for c in range(KC)],
                                  reads=allx, sem="xout"))
        if do_next:
            rmsnorm_fm(C, x, Tx, w1, Tw1, hb, Thb, TT, tmpn)
            out_toks.append(P.dma("sp", [lambda e, c=c: e.dma_start(out=h_dst[c, :, t0:t0 + TT], in_=hb[:, c, :]) for c in range(KC)],
                                  reads=allh, sem="hout"))
    for tt in range(ntok // TT):
        tile_body(tt)
    return out_toks


RH, RDK, RDV, RC = 4, 256, 512, 128


def ret_consts():
    inv = (10000.0 ** (-np.arange(0, RDK, 2, dtype=np.float32) / np.float32(RDK))).astype(np.float32)
    ang = (np.arange(S, dtype=np.float32)[:, None] * inv[None, :]).astype(np.float32)
    cs = np.ascontiguousarray(np.stack([np.cos(ang).T, np.sin(ang).T]).astype(np.float32))
    lg = np.log(1.0 - 2.0 ** (-5.0 - np.arange(RH, dtype=np.float64)))
    pos = np.arange(RC, dtype=np.float64)
    per_head = []
    for h in range(RH):
        rel = pos[None, :] - pos[:, None]
        dT = np.where(rel >= 0, np.exp(np.maximum(rel, 0) * lg[h]), 0.0) * RDK ** -0.5
        gq = np.broadcast_to(np.exp((pos + 1.0) * lg[h])[None, :], (128, 128))
        kd = np.exp((RC - 1.0 - pos) * lg[h]) * RDK ** -0.5
        cd = np.full(128, np.exp(RC * lg[h]))
        per_head.append((dT.astype(np.float32), gq.astype(np.float32), kd.astype(np.float32), cd.astype(np.float32)))
    return cs, per_head


def emit_ret(C, h_src, win, hsel, cs_d, dmat_d, gq_d, kdcd_d, ident_d, o_dst, c0, ntt=S // 512):
    if True:
        P = C.P
        ps = C.ps
        oT = o_dst
        hT = h_src
        w = [C.sb("w%d" % h, [128, KC, 1536], BF16) for h in range(2)]
        Tw = [T("w%d" % h) for h in range(2)]
        for h in range(2):
            g = hsel[h]
            segs = [(0, g * 256, 256), (256, 1024 + g * 256, 256), (512, 2048 + g * 512, 512), (1024, 4096 + g * 512, 512)]
            P.dma("pool", [lambda e, h=h, d0=d0, s0=s0, n=n: e.dma_start(
                out=w[h][:, :, d0:d0 + n], in_=win[:, s0:s0 + n].rearrange("(k p) n -> p k n", p=128)) for d0, s0, n in segs],
                writes=[Tw[h]], sem="w%d" % h)
        dmat = C.sb("dmat_s", [128, 2, 128], F32)
        gq = C.sb("gq_s", [128, 2, 128], F32)
        kdcd = C.sb("kdcd_s", [128, 4], F32)
        ident = C.sb("ident_s", [128, 128], F32)
        Tc = T("consts")
        P.dma("sp", [lambda e, h=h: e.dma_start(out=dmat[:, h, :], in_=dmat_d[hsel[h]]) for h in range(2)]
              + [lambda e, h=h: e.dma_start(out=gq[:, h, :], in_=gq_d[hsel[h]]) for h in range(2)]
              + [lambda e, h=h: e.dma_start(out=kdcd[:, h:h + 1], in_=kdcd_d[:, hsel[h]:hsel[h] + 1], allow_slow_non_contiguous=True) for h in range(2)]
              + [lambda e, h=h: e.dma_start(out=kdcd[:, 2 + h:3 + h], in_=kdcd_d[:, 4 + hsel[h]:5 + hsel[h]], allow_slow_non_contiguous=True) for h in range(2)]
              + [lambda e: e.dma_start(out=ident[:], in_=ident_d)],
              writes=[Tc], sem="c")

        hb = [C.sb("hb%d" % i, [128, KC, 512], BF16) for i in range(2)]
        Thb = [T("hb%d" % i) for i in range(2)]
        csb = [C.sb("cs%d" % i, [128, 2, 512], F32) for i in range(2)]
        Tcs = [T("cs%d" % i) for i in range(2)]
        raw = C.sb("raw", [128, 2, 512], F32); Traw = T("raw")
        t1 = C.sb("t1", [128, 512], F32); Tt1 = T("t1")
        t2 = C.sb("t2", [128, 512], F32); Tt2 = T("t2")
        rot = C.sb("rot", [128, 2, 512], F32); Trot = T("rot")
        qb = C.sb("qb", [128, 2, 512], BF16); Tqb = T("qb")
        qd = C.sb("qd", [128, 2, 512], BF16); Tqd = T("qd")
        kb = C.sb("kb", [128, 2, 512], BF16); Tkb = T("kb")
        kdt = C.sb("kdt", [128, 4, 256], BF16); Tkdt = T("kdt")
        vt = C.sb("vt", [128, 4, 512], BF16); Tvt = T("vt")
        sg = C.sb("sg", [128, 4, 512], F32); Tsg = T("sg")
        of = C.sb("of", [128, 4, 512], F32); Tof = [[T("of%d_%d" % (v, 0))] for v in range(4)]
        at = C.sb("at", [128, 128], BF16); Tat = T("at")
        state = [C.sb("st%d" % h, [128, 2, 512], F32) for h in range(2)]
        stb = [C.sb("stb%d" % h, [128, 2, 512], BF16) for h in range(2)]
        Tst = [[T("st%d_%d" % (h, dc)) for dc in range(2)] for h in range(2)]
        Tstb = [T("stb%d" % h) for h in range(2)]
        sq = C.sb("sq", [128, 4, 512], BF16); Tsq = [T("sq%d" % c) for c in range(4)]
        rstd = C.sb("rstd", [128, 512], F32); Trstd = T("rstd")
        ob = [C.sb("ob%d" % i, [128, 8, 512], BF16) for i in range(2)]
        Tob = [T("ob%d" % i) for i in range(2)]
        out_toks = []

        for tt in range(ntt):
            ts = slice(tt * 512, (tt + 1) * 512)
            hbuf, Th = hb[tt % 2], Thb[tt % 2]
            cbuf, Tcb = csb[tt % 2], Tcs[tt % 2]
            P.dma("sp", [lambda e, k=k, ts=ts, hbuf=hbuf: e.dma_start(out=hbuf[:, k, :], in_=hT[k, :, ts]) for k in range(KC)],
                  writes=[Th], sem="h%d" % (tt % 2))
            P.dma("sp", [lambda e, i=i, ts=ts, cbuf=cbuf: e.dma_start(out=cbuf[:, i, :], in_=cs_d[i, :, ts]) for i in range(2)],
                  writes=[Tcb], sem="cs%d" % (tt % 2))
            obuf, Tobuf = ob[tt % 2], Tob[tt % 2]
            def head_body(h, tt=tt, ts=ts, hbuf=hbuf, Th=Th, cbuf=cbuf, Tcb=Tcb, obuf=obuf, Tobuf=Tobuf):
                def proj_fm(col0, banks):
                    b = C.bank("pj", banks)
                    mm_group(C, b, [(w[h][:, k, col0:col0 + 128], hbuf[:, k, :]) for k in range(KC)], reads=[Tw[h], Th])
                    return b

                def rotary(col0, dst_bf, Tdst, want_f32):
                    b0 = proj_fm(col0, (0, 1, 2, 3))
                    b1 = proj_fm(col0 + 128, (0, 1, 2, 3))
                    P.op("act", lambda e: e.copy(out=raw[:, 0, :], in_=ps[:, b0, :]), reads=[C.Tps[b0]], writes=[Traw])
                    P.op("act", lambda e: e.copy(out=raw[:, 1, :], in_=ps[:, b1, :]), reads=[C.Tps[b1]], writes=[Traw])
                    cos, sin = cbuf[:, 0, :], cbuf[:, 1, :]
                    P.op("dve", lambda e: e.tensor_tensor(out=t1[:], in0=raw[:, 0, :], in1=cos, op=ALU.mult), reads=[Traw, Tcb], writes=[Tt1])
                    P.op("pool", lambda e: e.tensor_tensor(out=t2[:], in0=raw[:, 1, :], in1=sin, op=ALU.mult), reads=[Traw, Tcb], writes=[Tt2])
                    P.op("dve", lambda e: e.tensor_tensor(out=rot[:, 0, :], in0=t1[:], in1=t2[:], op=ALU.subtract), reads=[Tt1, Tt2], writes=[Trot])
                    P.op("dve", lambda e: e.tensor_tensor(out=t1[:], in0=raw[:, 0, :], in1=sin, op=ALU.mult), reads=[Traw, Tcb], writes=[Tt1])
                    P.op("pool", lambda e: e.tensor_tensor(out=t2[:], in0=raw[:, 1, :], in1=cos, op=ALU.mult), reads=[Traw, Tcb], writes=[Tt2])
                    P.op("dve", lambda e: e.tensor_tensor(out=rot[:, 1, :], in0=t1[:], in1=t2[:], op=ALU.add), reads=[Tt1, Tt2], writes=[Trot])
                    P.op("act", lambda e: e.copy(out=dst_bf[:], in_=rot[:]), reads=[Trot], writes=[Tdst])

                rotary(0, qb, Tqb, False)
                for dc in range(2):
                    P.op("pool", lambda e, dc=dc: e.tensor_tensor(
                        out=qd[:, dc, :].rearrange("p (c i) -> p c i", i=128), in0=rot[:, dc, :].rearrange("p (c i) -> p c i", i=128),
                        in1=gq[:, h:h + 1, :].to_broadcast([128, 4, 128]), op=ALU.mult), reads=[Trot, Tc], writes=[Tqd])
                rotary(256, kb, Tkb, True)
                for ci in range(4):
                    for dc in range(2):
                        def tr(e, ci=ci, dc=dc):
                            return e.transpose(out=ps[:, 4, dc * 128:(dc + 1) * 128], in_=rot[:, dc, ci * 128:(ci + 1) * 128], identity=ident[:])
                        P.op("pe", tr, reads=[Trot, Tc], writes=[C.Tps[4]])
                    P.op("act", lambda e, ci=ci: e.activation(out=kdt[:, ci, :], in_=ps[:, 4, 0:256], func=AF.Copy, scale=kdcd[:, h:h + 1]),
                         reads=[C.Tps[4], Tc], writes=[Tkdt])
                for ci in range(4):
                    b = C.bank("pj", (0, 1, 2, 3))
                    mm_group(C, b, [(hbuf[:, k, ci * 128:(ci + 1) * 128], w[h][:, k, 512:1024]) for k in range(KC)], reads=[Tw[h], Th])
                    P.op("act", lambda e, ci=ci, b=b: e.copy(out=vt[:, ci, :], in_=ps[:, b, :]), reads=[C.Tps[b]], writes=[Tvt])
                for vc in range(4):
                    b = proj_fm(1024 + vc * 128, (0, 1, 2, 3))
                    P.op("act", lambda e, vc=vc, b=b: e.activation(out=sg[:, vc, :], in_=ps[:, b, :], func=AF.Silu), reads=[C.Tps[b]], writes=[Tsg])
                for ci in range(4):
                    cs_ = slice(ci * 128, (ci + 1) * 128)
                    first = (tt == 0 and ci == 0)
                    mm_group(C, 4, [(kb[:, dc, cs_], qb[:, dc, cs_]) for dc in range(2)], reads=[Tkb, Tqb], n=128)
                    P.op("dve", lambda e: e.tensor_tensor(out=at[:], in0=ps[:, 4, 0:128], in1=dmat[:, h, :], op=ALU.mult),
                         reads=[C.Tps[4], Tc], writes=[Tat])

                    for dc in range(2):
                        b = 6 + dc
                        mm_group(C, b, [(kdt[:, ci, dc * 128:(dc + 1) * 128], vt[:, ci, :])], reads=[Tkdt, Tvt])
                        if first:
                            P.op("dve", lambda e, dc=dc, b=b: e.tensor_copy(out=state[h][:, dc, :], in_=ps[:, b, :]), reads=[C.Tps[b]], writes=[Tst[h][dc]])
                        else:
                            P.op("dve", lambda e, dc=dc, b=b: e.scalar_tensor_tensor(
                                out=state[h][:, dc, :], in0=state[h][:, dc, :], scalar=kdcd[:, 2 + h:3 + h], in1=ps[:, b, :],
                                op0=ALU.mult, op1=ALU.add), reads=[C.Tps[b], Tst[h][dc], Tc], writes=[Tst[h][dc]])
                    def omm(e, ci=ci, cs_=cs_, first=first):
                        ins = None
                        for vc in range(4):
                            vs = slice(vc * 128, (vc + 1) * 128)
                            ins = e.matmul(ps[:, 5, vs], lhsT=vt[:, ci, vs], rhs=at[:], start=True, stop=first)
                            if not first:
                                for dc in range(2):
                                    ins = e.matmul(ps[:, 5, vs], lhsT=stb[h][:, dc, vs], rhs=qd[:, dc, cs_], start=False, stop=(dc == 1))
                        return ins
                    P.op("pe", omm, reads=[Tvt, Tat, Tstb[h], Tqd], writes=[C.Tps[5]])
                    P.op("act", lambda e, cs_=cs_: e.copy(out=of[:, :, cs_], in_=ps[:, 5, :].rearrange("p (v i) -> p v i", i=128)),
                         reads=[C.Tps[5]], writes=[Tof[0][0]])
                    P.op("pool", lambda e: e.tensor_copy(out=stb[h][:], in_=state[h][:]), reads=Tst[h], writes=[Tstb[h]])
                for vc in range(4):
                    P.op("act", lambda e, vc=vc: e.activation(out=sq[:, vc, :], in_=of[:, vc, :], func=AF.Square), reads=[Tof[0][0]], writes=[Tsq[vc]])
                b = C.bank("pj", (0, 1, 2, 3))
                mm_group(C, b, [(C.ones[:], sq[:, vc, :]) for vc in range(4)], reads=[C.Tones] + Tsq)
                P.op("act", lambda e, b=b: e.activation(out=rstd[:], in_=ps[:, b, :], func=AF.Sqrt, scale=1.0 / RDV, bias=C.eps[:]),
                     reads=[C.Tps[b], C.Tones], writes=[Trstd])
                P.op("dve", lambda e: e.reciprocal(out=rstd[:], in_=rstd[:]), reads=[Trstd], writes=[Trstd])
                for vc in range(4):
                    P.op("dve", lambda e, vc=vc: e.tensor_tensor(out=of[:, vc, :], in0=of[:, vc, :], in1=rstd[:], op=ALU.mult),
                         reads=[Tof[0][0], Trstd], writes=[Tof[0][0]])
                    P.op("pool", lambda e, vc=vc: e.tensor_tensor(out=obuf[:, h * 4 + vc, :], in0=of[:, vc, :], in1=sg[:, vc, :], op=ALU.mult),
                         reads=[Tof[0][0], Tsg], writes=[Tobuf])
            for h in range(2):
                head_body(h)
            out_toks.append(P.dma("sp", [lambda e, c=c, ts=ts, obuf=obuf: e.dma_start(out=oT[c0 + c, :, ts], in_=obuf[:, c, :]) for c in range(8)],
                                  reads=[Tobuf], sem="o%d" % (tt % 2)))
    return out_toks


TL = 1919
TU = 1792
MASKNEG = -30000.0


def rel_bucket_np(n):
    n = np.maximum(n, 0)
    nf = np.maximum(n, 1).astype(np.float32)
    large = 16 + (np.log(nf / np.float32(16)) / np.float32(math.log(1024 / 16)) * np.float32(16)).astype(np.int32)
    large = np.minimum(large, 31)
    return np.where(n < 16, n, large)


def attn_consts():
    dist = np.arange(TL) - 511
    oh = np.zeros((33, TL), np.float32)
    bk = rel_bucket_np(dist)
    for j in range(TL):
        if dist[j] < 0:
            oh[32, j] = 1.0
        else:
            oh[bk[j], j] = 1.0
    cneg = np.zeros((16, 16), np.float32)
    negown = np.full((16, 16), MASKNEG, np.float32)
    for b in range(16):
        cneg[b, b:] = -2e30
        negown[b, b] = 0.0
    e16 = np.zeros((16, 16, 128), np.float32)
    for n in range(16):
        e16[n, n, :] = 1.0
    return oh, np.broadcast_to(cneg[None], (128, 16, 16)).copy(), np.broadcast_to(negown[None], (128, 16, 16)).copy(), e16


def emit_attn(C, kind, lam_init, h_src, win, r, rb_full, qkn_d, oh_d, ident_d, cneg_d, negown_d, e16_d, lam_d, sub_d, Rd, o_dst, c0,
              nqt=S // 512):
    moba = (kind == "moba")
    if True:
        P = C.P
        ps = C.ps
        hT = h_src
        oT = o_dst
        win_segs = [(0, r * 512), (512, 1024 + r * 512), (1024, 2048 + r * 512)]
        rb_d = rb_full[:, r * 8:(r + 1) * 8]
        w = C.sb("w", [128, KC, 1536], BF16); Tw = T("w")
        P.dma("pool", [lambda e, d0=d0, s0=s0: e.dma_start(out=w[:, :, d0:d0 + 512], in_=win[:, s0:s0 + 512].rearrange("(k p) n -> p k n", p=128))
                       for d0, s0 in win_segs], writes=[Tw], sem="w")
        rbx = C.sb("rbx", [33, 8], F32)
        qkn = C.sb("qkn_s", [128, 2], F32)
        oh = C.sb("oh_s", [33, TL], F32)
        ident = C.sb("ident_s", [128, 128], F32)
        Tc = T("consts")
        fns = [lambda e: e.dma_start(out=rbx[0:32, :], in_=rb_d), lambda e: e.dma_start(out=qkn[:], in_=qkn_d),
               lambda e: e.dma_start(out=oh[:], in_=oh_d), lambda e: e.dma_start(out=ident[:], in_=ident_d)]
        if moba:
            cneg = C.sb("cneg_s", [128, 16, 16], F32)
            negown = C.sb("negown_s", [128, 16, 16], F32)
            fns += [lambda e: e.dma_start(out=cneg[:], in_=cneg_d), lambda e: e.dma_start(out=negown[:], in_=negown_d)]
        else:
            lam = C.sb("lam_s", [1, 256], F32)
            subw = C.sb("subw_s", [128, 1], F32)
            fns += [lambda e: e.dma_start(out=lam[:], in_=lam_d), lambda e: e.dma_start(out=subw[:], in_=sub_d)]
        P.dma("sp", fns, writes=[Tc], sem="c")
        if moba:
            e16 = C.sb("e16_s", [128, 16, 128], BF16)
            Te16 = T("e16")
            P.op("pool", lambda e: e.memset(e16[:], 0.0), writes=[Te16])
            P.dma("pool", lambda e: e.dma_start(out=e16[0:16], in_=e16_d), writes=[Te16], sem="e16")
        P.op("pool", lambda e: e.memset(rbx[32:33, :], MASKNEG), reads=[Tc], writes=[Tc])
        identr = C.sb("identr", [128, 128], F32R)
        P.op("act", lambda e: e.copy(out=identr[:], in_=ident[:]), reads=[Tc], writes=[Tc])
        P.op("dve", lambda e: e.tensor_scalar(out=qkn[:, 0:1], in0=qkn[:, 0:1], scalar1=0.125, scalar2=None, op0=ALU.mult), reads=[Tc], writes=[Tc])
        ones33 = C.sb("ones33", [33, 128], F32)
        onesf = C.sb("onesf", [1, 128], F32)
        bones = C.sb("bones", [128, 128], BF16)
        Tk = T("kconst")
        P.op("pool", lambda e: e.memset(ones33[:], 1.0), writes=[Tk])
        P.op("pool", lambda e: e.memset(onesf[:], 1.0), writes=[Tk])
        P.op("pool", lambda e: e.memset(bones[:], 0.0), writes=[Tk])
        P.op("pool", lambda e: e.memset(bones[0:64, 0:64], 1.0), writes=[Tk])
        P.op("pool", lambda e: e.memset(bones[64:128, 64:128], 1.0), writes=[Tk])
        hm = C.sb("hm", [128, 2], F32)
        P.op("pool", lambda e: e.memset(hm[:], 0.0), writes=[Tk])
        P.op("pool", lambda e: e.memset(hm[0:64, 0:1], 1.0), writes=[Tk])
        P.op("pool", lambda e: e.memset(hm[64:128, 1:2], 1.0), writes=[Tk])
        c31 = C.sb("c31", [128, 8], F32); Tc31 = T("c31")
        brep = C.sb("brep", [33, 128], F32); Tbrep = T("brep")
        rsb = C.sb("rsb", [128, TL], F32); Trsb = T("rsb")
        TRd = [T("Rd%d" % h) for h in range(8)]

        def build_strip(hh):
            P.op("dve", lambda e: e.tensor_scalar(out=brep[:], in0=ones33[:], scalar1=rbx[:, hh:hh + 1], scalar2=None, op0=ALU.mult),
                 reads=[Tc, Tk], writes=[Tbrep])
            for cb in range(4):
                c0, c1 = cb * 512, min(TL, (cb + 1) * 512)
                b = C.bank("pj", (0, 1, 2, 3))

                def mmf(e, c0=c0, c1=c1, b=b):
                    return e.matmul(ps[:, b, 0:c1 - c0], lhsT=brep[:], rhs=oh[:, c0:c1], start=True, stop=True)
                P.op("pe", mmf, reads=[Tbrep, Tc], writes=[C.Tps[b]])
                P.op("act", lambda e, c0=c0, c1=c1, b=b: e.copy(out=rsb[:, c0:c1], in_=ps[:, b, 0:c1 - c0]), reads=[C.Tps[b]], writes=[Trsb])
            P.op("dve", lambda e: e.tensor_copy(out=c31[:, hh:hh + 1], in_=rsb[:, TL - 1:TL]), reads=[Trsb], writes=[Tc31])
            P.dma("sp", lambda e: e.dma_start(out=Rd.ap()[hh], in_=rsb[:]), reads=[Trsb], writes=[TRd[hh]], sem="rd")
        for hh in range(8):
            build_strip(hh)

        if not moba:
            lp = C.sb("lp", [1, 128], F32); l2 = C.sb("l2", [1, 2], F32); nlam = C.sb("nlam", [128, 1], F32); Tl = T("lam")
            P.op("dve", lambda e: e.tensor_tensor(out=lp[:, 0:64], in0=lam[:, 0:64], in1=lam[:, 64:128], op=ALU.mult), reads=[Tc], writes=[Tl])
            P.op("dve", lambda e: e.tensor_tensor(out=lp[:, 64:128], in0=lam[:, 128:192], in1=lam[:, 192:256], op=ALU.mult), reads=[Tc, Tl], writes=[Tl])
            P.op("dve", lambda e: e.reduce_sum(out=l2[:], in_=lp[:].rearrange("p (a b) -> p a b", a=2), axis=AX.X), reads=[Tl], writes=[Tl])
            P.op("act", lambda e: e.activation(out=l2[:], in_=l2[:], func=AF.Exp), reads=[Tl], writes=[Tl])
            P.op("dve", lambda e: e.tensor_tensor(out=lp[:, 0:1], in0=l2[:, 1:2], in1=l2[:, 0:1], op=ALU.subtract), reads=[Tl], writes=[Tl])
            P.op("dve", lambda e: e.tensor_scalar(out=lp[:, 0:1], in0=lp[:, 0:1], scalar1=-float(lam_init), scalar2=None, op0=ALU.add), reads=[Tl], writes=[Tl])

            def mml(e):
                return e.matmul(ps[:, 3, 0:1], lhsT=onesf[:], rhs=lp[:, 0:1], start=True, stop=True)
            P.op("pe", mml, reads=[Tl, Tk], writes=[C.Tps[3]])
            P.op("act", lambda e: e.copy(out=nlam[:], in_=ps[:, 3, 0:1]), reads=[C.Tps[3]], writes=[Tl])
            P.op("dve", lambda e: e.tensor_scalar(out=subw[:], in0=subw[:], scalar1=float(1.0 - lam_init), scalar2=None, op0=ALU.mult), reads=[Tc], writes=[Tc])

        hb = [C.sb("hb%d" % i, [128, KC, 512], BF16) for i in range(2)]
        Thb = [T("hb%d" % i) for i in range(2)]
        TqT = T("qT")
        kT = C.sb("kT", [128, S], BF16); TkT = T("kT")
        qz = [C.sb("qz%d" % j, [128, S], BF16) for j in range(2)]
        Tqz = [T("qz%d" % j) for j in range(2)]
        for j in range(2):
            P.op("pool", lambda e, j=j: e.memset(qz[j][:], 0.0), writes=[Tqz[j]])
        vt = C.sb("vt", [128, 32, 512], BF16); Tvt = T("vt")
        sq = C.sb("sq", [128, 512], BF16); Tsq = T("sq")
        rstd = C.sb("rstd", [128, 512], F32); Trstd = T("rstd")
        tsk = [C.sb("tsk%d" % j, [128, TU], F32) for j in range(2)]
        Ttsk = [T("tsk%d" % j) for j in range(2)]
        pt = [C.sb("pt%d" % i, [128, 512], BF16) for i in range(6)]
        Tpt = [T("pt%d" % i) for i in range(6)]
        tmp = [C.sb("tmp%d" % i, [128, 512], F32) for i in range(1)]
        Ttmp = [T("tmp%d" % i) for i in range(1)]
        ob = [C.sb("ob%d" % i, [128, 512], BF16) for i in range(2)]
        Tob = [T("ob%d" % i) for i in range(2)]
        if moba:
            qF = C.sb("qF", [128, S], F32); TqF = T("qF")
            kF = C.sb("kF", [128, S], F32); TkF = T("kF")
            km = C.sb("km", [128, 16], F32); Tkm = T("km")
            gm = C.sb("gm", [128, 16], F32); Tgm = T("gm")
            top8 = C.sb("top8", [128, 8], F32); Ttop = T("top8")
            mneg = C.sb("mneg", [128, 16], F32); Tmn = T("mneg")
            mT = [C.sb("mT%d" % j, [128, S], BF16) for j in range(2)]
            TmT = [T("mT%d" % j) for j in range(2)]
            for j in range(2):
                P.op("pool", lambda e, j=j: e.memset(mT[j][:], 0.0), writes=[TmT[j]])
        r0 = C.sb("r0", [128, 512], F32); Tr0 = T("r0")
        a0 = C.sb("a0", [128, 512], F32); Ta0 = T("a0")
        if not moba:
            a1 = C.sb("a1", [128, 512], F32); Ta1 = T("a1")
        out_toks = []
        cnt = {"h": 0, "pt": 0, "tmp": 0, "ob": 0}

        def qk_norm(b, wcol, dst_main, Tdst, ts, dstF=None, TdstF=None):
            P.op("act", lambda e: e.activation(out=sq[:], in_=ps[:, b, :], func=AF.Square), reads=[C.Tps[b]], writes=[Tsq])
            b2 = C.bank("nrm", (4, 5))
            mm_group(C, b2, [(bones[:], sq[:])], reads=[Tk, Tsq])
            P.op("act", lambda e: e.activation(out=rstd[:], in_=ps[:, b2, :], func=AF.Sqrt, scale=1.0 / 64, bias=C.eps[:]),
                 reads=[C.Tps[b2], C.Tones], writes=[Trstd])
            P.op("dve", lambda e: e.reciprocal(out=rstd[:], in_=rstd[:]), reads=[Trstd], writes=[Trstd])
            if dstF is not None:
                P.op("dve", lambda e: e.scalar_tensor_tensor(out=dstF[:, ts], in0=ps[:, b, :], scalar=qkn[:, wcol:wcol + 1], in1=rstd[:],
                                                             op0=ALU.mult, op1=ALU.mult), reads=[C.Tps[b], Tc, Trstd], writes=[TdstF])
                if wcol == 0:
                    for j in range(2):
                        hp = slice(64 * j, 64 * j + 64)
                        P.op("act", lambda e, j=j, hp=hp: e.copy(out=qz[j][hp, ts], in_=dstF[hp, ts]), reads=[TdstF], writes=[Tqz[j]])
                else:
                    P.op("act", lambda e: e.copy(out=dst_main[:, ts], in_=dstF[:, ts]), reads=[TdstF], writes=[Tdst])
            elif wcol == 0:
                for j in range(2):
                    hp = slice(64 * j, 64 * j + 64)
                    P.op("dve", lambda e, j=j, hp=hp: e.scalar_tensor_tensor(out=qz[j][hp, ts], in0=ps[hp, b, :], scalar=qkn[hp, 0:1], in1=rstd[hp, :],
                                                                           op0=ALU.mult, op1=ALU.mult), reads=[C.Tps[b], Tc, Trstd], writes=[Tqz[j]])
            else:
                P.op("dve", lambda e: e.scalar_tensor_tensor(out=dst_main[:, ts], in0=ps[:, b, :], scalar=qkn[:, wcol:wcol + 1], in1=rstd[:],
                                                             op0=ALU.mult, op1=ALU.mult), reads=[C.Tps[b], Tc, Trstd], writes=[Tdst])

        def project(p, tt):
            ts = slice(tt * 512, (tt + 1) * 512)
            i = cnt["h"] % 2
            cnt["h"] += 1
            hbuf, Th = hb[i], Thb[i]
            P.dma("sp", [lambda e, k=k: e.dma_start(out=hbuf[:, k, :], in_=hT[k, :, ts]) for k in range(KC)], writes=[Th], sem="h%d" % i)
            for which, (dst, Td, dF, TdF) in enumerate([(None, TqT, qF if moba else None, TqF if moba else None),
                                                        (kT, TkT, kF if moba else None, TkF if moba else None)]):
                b = C.bank("pj", (0, 1, 2, 3))
                col0 = which * 512 + p * 128
                mm_group(C, b, [(w[:, k, col0:col0 + 128], hbuf[:, k, :]) for k in range(KC)], reads=[Tw, Th])
                qk_norm(b, which, dst, Td, ts, dF, TdF)
            if p == 0:
                for ci in range(4):
                    b = C.bank("pj", (0, 1, 2, 3))
                    mm_group(C, b, [(hbuf[:, k, ci * 128:(ci + 1) * 128], w[:, k, 1024:1536]) for k in range(KC)], reads=[Tw, Th])
                    P.op("act", lambda e, ci=ci, b=b: e.copy(out=vt[:, tt * 4 + ci, :], in_=ps[:, b, :]), reads=[C.Tps[b]], writes=[Tvt])

        def gate_tile(j, g):
            hp = slice(64 * j, 64 * j + 64)
            gs = slice(g * 128, (g + 1) * 128)
            blk = g // 2

            def mg(e):
                return e.matmul(ps[:, 6, 0:16], lhsT=qF[hp, gs], rhs=km[hp, :], start=True, stop=True)
            P.op("pe", mg, reads=[TqF, Tkm], writes=[C.Tps[6]])
            P.op("dve", lambda e: e.tensor_tensor(out=gm[:], in0=ps[:, 6, 0:16], in1=cneg[:, blk, :], op=ALU.add), reads=[C.Tps[6], Tc], writes=[Tgm])
            P.op("dve", lambda e: e.max(out=top8[:], in_=gm[:]), reads=[Tgm], writes=[Ttop])
            P.op("dve", lambda e: e.tensor_scalar(out=top8[:, 2:3], in0=top8[:, 2:3], scalar1=-1e30, scalar2=None, op0=ALU.max), reads=[Ttop], writes=[Ttop])
            P.op("dve", lambda e: e.scalar_tensor_tensor(out=mneg[:], in0=gm[:], scalar=top8[:, 2:3], in1=negown[:, blk, :], op0=ALU.is_lt, op1=ALU.mult),
                 reads=[Tgm, Ttop, Tc], writes=[Tmn])

            def tr(e):
                return e.transpose(out=ps[0:16, 7, 0:128], in_=mneg[:], identity=ident[:])
            P.op("pe", tr, reads=[Tmn, Tc], writes=[C.Tps[7]])
            P.op("act", lambda e: e.copy(out=mT[j][0:16, gs], in_=ps[0:16, 7, 0:128]), reads=[C.Tps[7]], writes=[TmT[j]])

        def stage1(p, j, qt, kt):
            hp = slice(64 * j, 64 * j + 64)
            qs = slice(qt * 512, (qt + 1) * 512)
            ks = slice(kt * 128, (kt + 1) * 128)
            delta = qt * 512 - kt * 128
            bs = C.bank("s", (0, 1, 2, 3))

            near = delta < 1024

            def ms(e):
                ins = e.matmul(ps[:, bs, :], lhsT=kT[:, ks], rhs=qz[j][:, qs], start=True, stop=not (moba or near))
                if moba:
                    ins = e.matmul(ps[:, bs, :], lhsT=e16[:, kt // 2, :], rhs=mT[j][:, qs], start=False, stop=not near)
                if near:
                    ins = e.matmul(ps[:, bs, :], lhsT=identr[:], rhs=tsk[j][:, delta + 384:delta + 384 + 512].bitcast(F32R),
                                   start=False, stop=True)
                return ins
            P.op("pe", ms, reads=[TkT, Tqz[j], Tc] + ([Te16, TmT[j]] if moba else []) + ([Ttsk[j]] if near else []), writes=[C.Tps[bs]])
            ip = cnt["pt"] % len(pt)
            cnt["pt"] += 1
            if near:
                P.op("act", lambda e: e.activation(out=pt[ip][:], in_=ps[:, bs, :], func=AF.Exp), reads=[C.Tps[bs]], writes=[Tpt[ip]])
            else:
                P.op("act", lambda e: e.activation(out=pt[ip][:], in_=ps[:, bs, :], func=AF.Exp, bias=c31[:, 2 * p + j:2 * p + j + 1]),
                     reads=[C.Tps[bs], Tc31], writes=[Tpt[ip]])
            return ip

        def stage2(p, j, kt, ip, bo, bl, first, last):
            hp = slice(64 * j, 64 * j + 64)

            def mpv(e):
                e.matmul(ps[:, bo, :], lhsT=vt[:, kt, p * 128:(p + 1) * 128], rhs=pt[ip][:], start=first, stop=last)
                return e.matmul(ps[:, bl, :], lhsT=C.ones[:], rhs=pt[ip][:], start=first, stop=last)
            P.op("pe", mpv, reads=[Tvt, Tpt[ip], C.Tones], writes=[C.Tps[bo], C.Tps[bl]])

        def finish_head(j, bo, bl):
            if moba:
                hp = slice(64 * j, 64 * j + 64)
                P.op("dve", lambda e: e.reciprocal(out=r0[hp, :], in_=ps[hp, bl, :]), reads=[C.Tps[bl]], writes=[Tr0])
                P.op("dve", lambda e: e.tensor_tensor(out=a0[hp, :], in0=ps[hp, bo, :], in1=r0[hp, :], op=ALU.mult), reads=[C.Tps[bo], Tr0], writes=[Ta0])
                return
            a, Ta = (a0, Ta0) if j == 0 else (a1, Ta1)
            P.op("dve", lambda e: e.reciprocal(out=r0[:], in_=ps[:, bl, :]), reads=[C.Tps[bl]], writes=[Tr0])
            P.op("dve", lambda e: e.tensor_tensor(out=a[:], in0=ps[:, bo, :], in1=r0[:], op=ALU.mult), reads=[C.Tps[bo], Tr0], writes=[Ta])

        def finish_qt(p, qt, bo, bl):
            qs = slice(qt * 512, (qt + 1) * 512)
            io = cnt["ob"] % 2
            cnt["ob"] += 1
            if moba:
                P.op("pool", lambda e: e.tensor_copy(out=ob[io][:], in_=a0[:]), reads=[Ta0], writes=[Tob[io]])
            else:
                P.op("dve", lambda e: e.scalar_tensor_tensor(out=a0[:], in0=a1[:], scalar=nlam[:], in1=a0[:], op0=ALU.mult, op1=ALU.add),
                     reads=[Ta0, Ta1, Tl], writes=[Ta0])
                P.op("act", lambda e: e.activation(out=sq[:], in_=a0[:], func=AF.Square), reads=[Ta0], writes=[Tsq])
                bn = C.bank("s", (0, 1, 2, 3))
                mm_group(C, bn, [(C.ones[:], sq[:])], reads=[C.Tones, Tsq])
                P.op("act", lambda e: e.activation(out=rstd[:], in_=ps[:, bn, :], func=AF.Sqrt, scale=1.0 / 128, bias=C.eps[:]),
                     reads=[C.Tps[bn], C.Tones], writes=[Trstd])
                P.op("dve", lambda e: e.reciprocal(out=rstd[:], in_=rstd[:]), reads=[Trstd], writes=[Trstd])
                P.op("dve", lambda e: e.scalar_tensor_tensor(out=ob[io][:], in0=a0[:], scalar=subw[:], in1=rstd[:], op0=ALU.mult, op1=ALU.mult),
                     reads=[Ta0, Tc, Trstd], writes=[Tob[io]])
            out_toks.append(P.dma("sp", lambda e: e.dma_start(out=oT[c0 + p, :, qs], in_=ob[io][:]), reads=[Tob[io]], sem="o%d" % io))

        def attention(p):
            PD = 3
            tiles = []
            for qt in range(nqt):
                nk = 4 * qt + 4
                if True:
                    for j in range(2):
                        bo, bl = 4 + 2 * j, 5 + 2 * j
                        for kt in range(nk):
                            lastt = (kt == nk - 1)
                            tiles.append((j, qt, kt, bo, bl, kt == 0, lastt, (j, bo, bl) if lastt else None, (qt, bo, bl) if (lastt and j == 1) else None))
            pend = []

            def retire():
                (j, qt, kt, bo, bl, first, last, fh, fq), ip = pend.pop(0)
                stage2(p, j, kt, ip, bo, bl, first, last)
                if fh is not None:
                    finish_head(*fh)
                if fq is not None:
                    finish_qt(p, *fq)
            for tl in tiles:
                ip = stage1(p, tl[0], tl[1], tl[2])
                pend.append((tl, ip))
                if len(pend) > PD:
                    retire()
            while pend:
                retire()

        def load_tsk(p, j):
            hh = 2 * p + j
            src = bass.AP(Rd, hh * 128 * TL + 127, [[TL - 1, 128], [1, TU]])
            P.dma("sp", lambda e: e.dma_start(out=tsk[j][:], in_=src), reads=[TRd[hh]], writes=[Ttsk[j]], sem="tsk%d" % j)
            P.op("act", lambda e: e.copy(out=tsk[j][:].bitcast(F32R), in_=tsk[j][:]), reads=[Ttsk[j]], writes=[Ttsk[j]])

        for p in range(4):
            for tt in range(8):
                project(p, tt)
            for j in range(2):
                load_tsk(p, j)
            if moba:
                P.op("dve", lambda e: e.reduce_sum(out=km[:], in_=kF[:].rearrange("p (n t) -> p n t", t=256), axis=AX.X), reads=[TkF], writes=[Tkm])
                for j in range(2):
                    for g in range(4 * nqt):
                        gate_tile(j, g)
            attention(p)
    return out_toks


def build_fused():
    nc = bass.Bass("TRN2", target_bir_lowering=False)

    def inp(name, shape, dt=F32):
        return nc.dram_tensor(name, shape, dt, kind="ExternalInput").ap()
    xT = inp("xT", [KC, 128, S])
    n1 = inp("n1", [4, 128, KC]); n2 = inp("n2", [4, 128, KC])
    wup = inp("wup", [4, D, DFF]); wdown = inp("wdown", [4, DFF, D])
    retin = inp("retin", [2, D, 6144]); retout = inp("retout", [2, 2048, D])
    mobain = inp("mobain", [D, 3072]); mobaout = inp("mobaout", [D, D])
    diffin = inp("diffin", [D, 3072]); diffout = inp("diffout", [D, D])
    rb = inp("rb", [32, 16]); qknm = inp("qknm", [128, 2]); qknd = inp("qknd", [128, 2])
    lam = inp("lam", [1, 256]); subw = inp("subw", [128, 1])
    cs_d = inp("cs", [2, 128, S]); dmat_d = inp("dmat", [4, 128, 128]); gq_d = inp("gq", [4, 128, 128]); kdcd_d = inp("kdcd", [128, 8])
    ident_d = inp("ident", [128, 128]); oh_d = inp("oh", [33, TL])
    cneg_d = inp("cneg", [128, 16, 16]); negown_d = inp("negown", [128, 16, 16]); e16_d = inp("e16", [16, 16, 128])
    xs = nc.dram_tensor("xs", [KC, 128, S], F32, kind="Internal").ap()
    hs = nc.dram_tensor("hs", [KC, 128, S], BF16, kind="Internal").ap()
    osc = nc.dram_tensor("osc", [16, 128, S], BF16, kind="Internal").ap()
    Rd = nc.dram_tensor("Rscr", [8, 128, TL], F32, kind="Internal")
    xo = nc.dram_tensor("xo", [KC, 128, S], F32, kind="ExternalOutput").ap()

    with ExitStack() as st:
        C = Ctx(nc, st)
        P = C.P

        def phase(fn):
            P.barrier()
            with ExitStack() as ph:
                C.ph = ph
                C.bank_rr = {}
                fn()
            C.ph = None
        phase(lambda: emit_phase_a(C, S, xT, None, n1=n1[0], h_dst=hs))
        for i in range(4):
            kind, j = i % 3, i // 3
            for r in range(2):
                if kind == 0:
                    phase(lambda: emit_ret(C, hs, retin[j], (2 * r, 2 * r + 1), cs_d, dmat_d, gq_d, kdcd_d, ident_d, osc, 8 * r))
                elif kind == 1:
                    phase(lambda: emit_attn(C, "moba", 0.0, hs, mobain, r, rb, qknm, oh_d, ident_d, cneg_d, negown_d, e16_d, None, None, Rd, osc, 4 * r))
                else:
                    lam_init = 0.8 - 0.6 * math.exp(-0.3 * i)
                    phase(lambda: emit_attn(C, "diff", lam_init, hs, diffin, r, rb, qknd, oh_d, ident_d, None, None, None, lam, subw, Rd, osc, 4 * r))
            wout, fo = ((retout[j], 2048) if kind == 0 else ((mobaout, 1024) if kind == 1 else (diffout, 1024)))
            last = (i == 3)
            phase(lambda: emit_phase_a(C, S, xT if i == 0 else xs, xo if last else xs, o_src=osc, wout=wout, fo=fo, wup=wup[i], wdown=wdown[i],
                                       n2=n2[i], n1=None if last else n1[i + 1], h_dst=None if last else hs))
        P.barrier()
        P.emit(st)
    return nc


_NC_CACHE = {}


def _fm(a):
    return np.ascontiguousarray(a.T.reshape(a.shape[1] // 128, 128, a.shape[0]))


def _unfm(a):
    return np.ascontiguousarray(a.reshape(-1, a.shape[2]).T)


def _nw(w):
    return np.ascontiguousarray(w.reshape(w.shape[0], KC, 128).transpose(0, 2, 1))


def kernel(x, rel_bias, norm1, norm2, w_up, w_down, ret_w_in, ret_w_out,
           moba_w_in, moba_q_norm, moba_k_norm, moba_w_out,
           diff_w_in, diff_q_norm, diff_k_norm, diff_lambda, diff_subln, diff_w_out):
    f32 = np.float32
    A = lambda a: np.ascontiguousarray(np.asarray(a, f32))
    x = A(x)
    if "fused" not in _NC_CACHE:
        _NC_CACHE["fused"] = build_fused()
    nc = _NC_CACHE["fused"]
    cs, ph = ret_consts()
    oh, cneg, negown, e16 = attn_consts()
    shared = {
        "n1": _nw(A(norm1)), "n2": _nw(A(norm2)), "wup": A(w_up), "wdown": A(w_down),
        "retin": A(ret_w_in), "retout": A(ret_w_out), "mobain": A(moba_w_in[0]), "mobaout": A(moba_w_out[0]),
        "diffin": A(diff_w_in[0]), "diffout": A(diff_w_out[0]), "rb": A(rel_bias),
        "qknm": A(np.stack([np.tile(A(moba_q_norm[0]), 2), np.tile(A(moba_k_norm[0]), 2)], axis=1)),
        "qknd": A(np.stack([np.tile(A(diff_q_norm[0]), 2), np.tile(A(diff_k_norm[0]), 2)], axis=1)),
        "lam": A(diff_lambda[0]).reshape(1, 256), "subw": A(diff_subln[0]).reshape(128, 1),
        "cs": cs, "dmat": A(np.stack([ph[h][0] for h in range(4)])), "gq": A(np.stack([ph[h][1] for h in range(4)])),
        "kdcd": A(np.stack([ph[h][2] for h in range(4)] + [ph[h][3] for h in range(4)], axis=1)),
        "ident": np.eye(128, dtype=f32), "oh": oh, "cneg": cneg, "negown": negown, "e16": e16,
    }
    ims = []
    for c in range(8):
        d = dict(shared)
        d["xT"] = _fm(x[c % 4])
        ims.append(d)
    res = run_bass_kernel_spmd(nc, ims, core_ids=list(range(8))).results
    out = np.empty((4, S, D), f32)
    for b in range(4):
        out[b] = _unfm(res[b]["xo"])
    return out
```

```python
import math
from contextlib import ExitStack
import numpy as np
import concourse.bass as bass
import concourse.mybir as mybir
from concourse.bass_utils import run_bass_kernel_spmd

F32 = mybir.dt.float32
BF16 = mybir.dt.bfloat16
F32R = mybir.dt.float32r
AF = mybir.ActivationFunctionType
ALU = mybir.AluOpType
AX = mybir.AxisListType

D = 1024
KC = 8
S = 4096
NTOK = 2048
DFF = 4096
EPS = 1e-6
ENGS = ("pe", "act", "dve", "pool", "sp")


class T:
    __slots__ = ("name", "w", "r")

    def __init__(self, name=""):
        self.name = name
        self.w = None
        self.r = {}


class Prog:
    def __init__(self, nc, same_engine_sync=True):
        self.nc = nc
        self.ops = {e: [] for e in ENGS}
        self.cnt = {}
        self.seen = {e: {} for e in ENGS}
        self.same = same_engine_sync
        self.dma_sems = {}

    def _deps(self, eng, reads, writes):
        deps = {}

        def add(tok):
            if tok is None:
                return
            k, v = tok
            if deps.get(k, 0) < v:
                deps[k] = v
        for t in reads:
            add(t.w)
        for t in writes:
            add(t.w)
            for r in t.r.items():
                add(r)
        waits = []
        for k, v in deps.items():
            if k == eng and (not self.same or eng == "pe"):
                continue
            if self.seen[eng].get(k, 0) >= v:
                continue
            self.seen[eng][k] = v
            waits.append((k, v))
        return waits

    def _mark(self, tok, reads, writes):
        for t in reads:
            if t.r.get(tok[0], 0) < tok[1]:
                t.r[tok[0]] = tok[1]
        for t in writes:
            t.w = tok
            t.r = {}

    def op(self, eng, fn, reads=(), writes=()):
        waits = self._deps(eng, reads, writes)
        self.cnt[eng] = self.cnt.get(eng, 0) + 1
        tok = (eng, self.cnt[eng])
        self._mark(tok, reads, writes)
        self.ops[eng].append((fn, waits, (eng, 1)))
        return tok

    def dma(self, q, fns, reads=(), writes=(), sem=None):
        if not isinstance(fns, (list, tuple)):
            fns = [fns]
        key = ("dma", sem)
        self.dma_sems[key] = None
        waits = self._deps(q, reads, writes)
        self.cnt[key] = self.cnt.get(key, 0) + 16 * len(fns)
        tok = (key, self.cnt[key])
        self._mark(tok, reads, writes)
        for i, fn in enumerate(fns):
            self.ops[q].append((fn, waits if i == 0 else [], (key, 16)))
        return tok

    def wait_all(self, eng, toks):
        best = {}
        for k, v in toks:
            best[k] = max(best.get(k, 0), v)
        waits = []
        for k, v in best.items():
            if self.seen[eng].get(k, 0) >= v:
                continue
            self.seen[eng][k] = v
            waits.append((k, v))
        self.ops[eng].append((None, waits, None))

    def barrier(self):
        toks = list(self.cnt.items())
        for e in ENGS:
            self.wait_all(e, toks)

    def emit(self, stack):
        nc = self.nc
        sems = {}
        for e in ENGS:
            if self.ops[e]:
                sems[e] = stack.enter_context(nc.semaphore("s_" + e))
        for k in self.dma_sems:
            sems[k] = stack.enter_context(nc.semaphore("d_%s" % (k[1],)))
        block = stack.enter_context(nc.Block())
        handles = {"pe": block.tensor, "act": block.scalar, "dve": block.vector,
                   "pool": block.gpsimd, "sp": block.sync}

        def make(e):
            def body(engine):
                for fn, waits, inc in self.ops[e]:
                    for k, v in waits:
                        engine.wait_ge(sems[k], v)
                    if fn is not None:
                        fn(engine).then_inc(sems[inc[0]], inc[1])
            return body
        for e in ENGS:
            if self.ops[e]:
                handles[e](make(e))


class Ctx:
    def __init__(self, nc, st):
        self.nc, self.st = nc, st
        self.P = Prog(nc)
        self.ps = st.enter_context(nc.psum_tensor("ps", [128, 8, 512], F32))
        self.Tps = [T("ps%d" % i) for i in range(8)]
        self.ones = st.enter_context(nc.sbuf_tensor("ones", [128, 128], BF16))
        self.Tones = T("ones")
        self.eps = st.enter_context(nc.sbuf_tensor("epsc", [128, 1], F32))
        self.P.op("pool", lambda e: e.memset(self.eps[:], EPS), writes=[self.Tones])
        self.P.op("pool", lambda e: e.memset(self.ones[:], 1.0), writes=[self.Tones])
        self.bank_rr = {}

    def sb(self, name, shape, dt):
        self.nalloc = getattr(self, "nalloc", 0) + 1
        stack = self.ph if getattr(self, "ph", None) is not None else self.st
        return stack.enter_context(self.nc.sbuf_tensor("%s_%d" % (name, self.nalloc), shape, dt))

    def bank(self, group, banks):
        i = self.bank_rr.get(group, 0)
        self.bank_rr[group] = i + 1
        return banks[i % len(banks)]


def mm_group(C, bank, mms, reads, n=512, prow=128):
    ps = C.ps

    def fn(e, mms=mms):
        ins = None
        for i, (l, r) in enumerate(mms):
            ins = e.matmul(ps[0:prow, bank, 0:n], lhsT=l, rhs=r, start=(i == 0), stop=(i == len(mms) - 1))
        return ins
    return C.P.op("pe", fn, reads=reads, writes=[C.Tps[bank]])


def rmsnorm_fm(C, x, Tx, w32, Tw, out, Tout, ntok, tmp, nd=KC, banks=(6, 7), tt_list=None, xoff=0):
    P = C.P
    sq, Tsq, rstd, Trstd = tmp
    nfeat = nd * 128
    for tt in (tt_list if tt_list is not None else range(ntok // 512)):
        ts = slice(tt * 512, (tt + 1) * 512)
        xs = slice(xoff + tt * 512, xoff + (tt + 1) * 512)
        b = C.bank("nrm", banks)
        for c in range(nd):
            P.op("act", lambda e, c=c, xs=xs: e.activation(out=sq[:, c, :], in_=x[:, c, xs], func=AF.Square),
                 reads=[Tx[c][tt]], writes=[Tsq[c]])
        mm_group(C, b, [(C.ones[:], sq[:, c, :]) for c in range(nd)], reads=[C.Tones] + Tsq[:nd])
        P.op("act", lambda e, b=b: e.activation(out=rstd[:], in_=C.ps[:, b, :], func=AF.Sqrt, scale=1.0 / nfeat, bias=C.eps[:]),
             reads=[C.Tps[b], C.Tones], writes=[Trstd])
        P.op("dve", lambda e: e.reciprocal(out=rstd[:], in_=rstd[:]), reads=[Trstd], writes=[Trstd])
        for c in range(nd):
            P.op("dve", lambda e, c=c, ts=ts, xs=xs: e.scalar_tensor_tensor(
                out=out[:, c, ts], in0=x[:, c, xs], scalar=w32[:, c:c + 1], in1=rstd[:], op0=ALU.mult, op1=ALU.mult),
                reads=[Tx[c][tt], Tw, Trstd], writes=[Tout[c][tt]])


def emit_phase_a(C, ntok, x_src, x_dst, o_src=None, wout=None, fo=0, wup=None, wdown=None, n2=None, n1=None, h_dst=None, TT=1024):
    P = C.P
    do_mix, do_ffn, do_next = o_src is not None, wup is not None, h_dst is not None
    NS = TT // 512
    out_toks = []
    x = C.sb("x", [128, KC, TT], F32)
    Tx = [[T("x") for _ in range(NS)] for c in range(KC)]
    sq = C.sb("sq", [128, KC, 512], BF16)
    Tsq = [T("sq") for c in range(KC)]
    rstd = C.sb("rstd", [128, 512], F32)
    tmpn = (sq, Tsq, rstd, T("rstd"))
    hb = C.sb("hb", [128, KC, TT], BF16)
    Thb = [[T("hb") for _ in range(NS)] for c in range(KC)]
    allx = [t for c in range(KC) for t in Tx[c]]
    allh = [t for c in range(KC) for t in Thb[c]]

    def load_w(name, src):
        w = C.sb(name, [128, KC], F32)
        Tw = T(name)
        P.dma("sp", lambda e: e.dma_start(out=w[:], in_=src), writes=[Tw], sem=name)
        return w, Tw
    if do_ffn or do_mix:
        u2 = C.sb("u2", [128, 32, TT], BF16)
        Tu2 = [T("u2") for f in range(32)]
    if do_mix:
        FK = fo // 128
        wo = [C.sb("wo", [128, FK, 128], BF16) for i in range(2)]
        Two = [T("wo") for i in range(2)]
    if do_ffn:
        w2, Tw2 = load_w("n2w", n2)
        wu = [C.sb("wu", [128, KC, 512], BF16) for i in range(2)]
        Twu = [T("wu") for i in range(2)]
        wd = [C.sb("wd", [128, 32, 256], BF16) for i in range(2)]
        Twd = [T("wd") for i in range(2)]
        rl = [C.sb("rl", [128, 512], F32) for i in range(2)]
        Trl = [T("rl") for i in range(2)]
    if do_next:
        w1, Tw1 = load_w("n1w", n1)
    cnt = {"wo": 0, "wu": 0, "wd": 0, "rl": 0}

    def tile_body(tt):
        t0 = tt * TT
        P.dma("sp", [lambda e, c=c: e.dma_start(out=x[:, c, :], in_=x_src[c, :, t0:t0 + TT]) for c in range(KC)], writes=allx, sem="xin")
        if do_mix:
            ob = u2
            P.dma("sp", [lambda e, k=k: e.dma_start(out=ob[:, k, :], in_=o_src[k, :, t0:t0 + TT]) for k in range(FK)], writes=Tu2[:FK], sem="oin")
            for n in range(KC):
                sl = cnt["wo"] % 2
                cnt["wo"] += 1
                P.dma("pool", lambda e, n=n, sl=sl: e.dma_start(
                    out=wo[sl][:], in_=wout[:, n * 128:(n + 1) * 128].rearrange("(k p) n -> p k n", p=128)),
                    writes=[Two[sl]], sem="wo%d" % sl)
                for s_ in range(NS):
                    ss = slice(s_ * 512, (s_ + 1) * 512)
                    b = C.bank("dn", (4, 5))
                    mm_group(C, b, [(wo[sl][:, k, :], ob[:, k, ss]) for k in range(FK)], reads=[Two[sl]] + Tu2[:FK])
                    P.op("dve", lambda e, n=n, ss=ss, b=b: e.tensor_tensor(out=x[:, n, ss], in0=x[:, n, ss], in1=C.ps[:, b, :], op=ALU.add),
                         reads=[C.Tps[b], Tx[n][s_]], writes=[Tx[n][s_]])
        if do_ffn:
            rmsnorm_fm(C, x, Tx, w2, Tw2, hb, Thb, TT, tmpn)
            for fg in range(8):
                sl = cnt["wu"] % 2
                cnt["wu"] += 1
                P.dma("pool", lambda e, fg=fg, sl=sl: e.dma_start(
                    out=wu[sl][:], in_=wup[:, fg * 512:(fg + 1) * 512].rearrange("(k p) f -> p k f", p=128)),
                    writes=[Twu[sl]], sem="wu%d" % sl)
                for fi in range(4):
                    f = fg * 4 + fi
                    for s_ in range(NS):
                        ss = slice(s_ * 512, (s_ + 1) * 512)
                        b = C.bank("up", (0, 1, 2, 3))
                        mm_group(C, b, [(wu[sl][:, c, fi * 128:(fi + 1) * 128], hb[:, c, ss]) for c in range(KC)],
                                 reads=[Twu[sl]] + [Thb[c][s_] for c in range(KC)])
                        r = cnt["rl"] % 2
                        cnt["rl"] += 1
                        P.op("act", lambda e, b=b, r=r: e.activation(out=rl[r][:], in_=C.ps[:, b, :], func=AF.Relu),
                             reads=[C.Tps[b]], writes=[Trl[r]])
                        P.op("dve", lambda e, f=f, r=r, ss=ss: e.tensor_tensor(out=u2[:, f, ss], in0=rl[r][:], in1=rl[r][:], op=ALU.mult),
                             reads=[Trl[r]], writes=[Tu2[f]])
            for ng in range(4):
                sl = cnt["wd"] % 2
                cnt["wd"] += 1
                P.dma("pool", lambda e, ng=ng, sl=sl: e.dma_start(
                    out=wd[sl][:], in_=wdown[:, ng * 256:(ng + 1) * 256].rearrange("(k p) n -> p k n", p=128)),
                    writes=[Twd[sl]], sem="wd%d" % sl)
                for ni in range(2):
                    n = ng * 2 + ni
                    for s_ in range(NS):
                        ss = slice(s_ * 512, (s_ + 1) * 512)
                        b = C.bank("dn", (4, 5))
                        mm_group(C, b, [(wd[sl][:, f, ni * 128:(ni + 1) * 128], u2[:, f, ss]) for f in range(32)],
                                 reads=[Twd[sl]] + Tu2)
                        P.op("dve", lambda e, n=n, ss=ss, b=b: e.tensor_tensor(out=x[:, n, ss], in0=x[:, n, ss], in1=C.ps[:, b, :], op=ALU.add),
                             reads=[C.Tps[b], Tx[n][s_]], writes=[Tx[n][s_]])
        if x_dst is not None:
            out_toks.append(P.dma("sp", [lambda e, c=c: e.dma_start(out=x_dst[c, :, t0:t0 + TT], in_=x[:, c, :]) for c in range(KC)],
                                  reads=allx, sem="xout"))
        if do_next:
            rmsnorm_fm(C, x, Tx, w1, Tw1, hb, Thb, TT, tmpn)
            out_toks.append(P.dma("sp", [lambda e, c=c: e.dma_start(out=h_dst[c, :, t0:t0 + TT], in_=hb[:, c, :]) for c in range(KC)],
                                  reads=allh, sem="hout"))
    for tt in range(ntok // TT):
        tile_body(tt)
    return out_toks


RH, RDK, RDV, RC = 4, 256, 512, 128


def ret_consts():
    inv = (10000.0 ** (-np.arange(0, RDK, 2, dtype=np.float32) / np.float32(RDK))).astype(np.float32)
    ang = (np.arange(S, dtype=np.float32)[:, None] * inv[None, :]).astype(np.float32)
    cs = np.ascontiguousarray(np.stack([np.cos(ang).T, np.sin(ang).T]).astype(np.float32))
    lg = np.log(1.0 - 2.0 ** (-5.0 - np.arange(RH, dtype=np.float64)))
    pos = np.arange(RC, dtype=np.float64)
    per_head = []
    for h in range(RH):
        rel = pos[None, :] - pos[:, None]
        dT = np.where(rel >= 0, np.exp(np.maximum(rel, 0) * lg[h]), 0.0) * RDK ** -0.5
        gq = np.broadcast_to(np.exp((pos + 1.0) * lg[h])[None, :], (128, 128))
        kd = np.exp((RC - 1.0 - pos) * lg[h]) * RDK ** -0.5
        cd = np.full(128, np.exp(RC * lg[h]))
        per_head.append((dT.astype(np.float32), gq.astype(np.float32), kd.astype(np.float32), cd.astype(np.float32)))
    return cs, per_head


def emit_ret(C, h_src, win, hsel, cs_d, dmat_d, gq_d, kdcd_d, ident_d, o_dst, c0, ntt=S // 512):
    if True:
        P = C.P
        ps = C.ps
        oT = o_dst
        hT = h_src
        w = [C.sb("w%d" % h, [128, KC, 1536], BF16) for h in range(2)]
        Tw = [T("w%d" % h) for h in range(2)]
        for h in range(2):
            g = hsel[h]
            segs = [(0, g * 256, 256), (256, 1024 + g * 256, 256), (512, 2048 + g * 512, 512), (1024, 4096 + g * 512, 512)]
            P.dma("pool", [lambda e, h=h, d0=d0, s0=s0, n=n: e.dma_start(
                out=w[h][:, :, d0:d0 + n], in_=win[:, s0:s0 + n].rearrange("(k p) n -> p k n", p=128)) for d0, s0, n in segs],
                writes=[Tw[h]], sem="w%d" % h)
        dmat = C.sb("dmat_s", [128, 2, 128], F32)
        gq = C.sb("gq_s", [128, 2, 128], F32)
        kdcd = C.sb("kdcd_s", [128, 4], F32)
        ident = C.sb("ident_s", [128, 128], F32)
        Tc = T("consts")
        P.dma("sp", [lambda e, h=h: e.dma_start(out=dmat[:, h, :], in_=dmat_d[hsel[h]]) for h in range(2)]
              + [lambda e, h=h: e.dma_start(out=gq[:, h, :], in_=gq_d[hsel[h]]) for h in range(2)]
              + [lambda e, h=h: e.dma_start(out=kdcd[:, h:h + 1], in_=kdcd_d[:, hsel[h]:hsel[h] + 1], allow_slow_non_contiguous=True) for h in range(2)]
              + [lambda e, h=h: e.dma_start(out=kdcd[:, 2 + h:3 + h], in_=kdcd_d[:, 4 + hsel[h]:5 + hsel[h]], allow_slow_non_contiguous=True) for h in range(2)]
              + [lambda e: e.dma_start(out=ident[:], in_=ident_d)],
              writes=[Tc], sem="c")

        hb = [C.sb("hb%d" % i, [128, KC, 512], BF16) for i in range(2)]
        Thb = [T("hb%d" % i) for i in range(2)]
        csb = [C.sb("cs%d" % i, [128, 2, 512], F32) for i in range(2)]
        Tcs = [T("cs%d" % i) for i in range(2)]
        raw = C.sb("raw", [128, 2, 512], F32); Traw = T("raw")
        t1 = C.sb("t1", [128, 512], F32); Tt1 = T("t1")
        t2 = C.sb("t2", [128, 512], F32); Tt2 = T("t2")
        rot = C.sb("rot", [128, 2, 512], F32); Trot = T("rot")
        qb = C.sb("qb", [128, 2, 512], BF16); Tqb = T("qb")
        qd = C.sb("qd", [128, 2, 512], BF16); Tqd = T("qd")
        kb = C.sb("kb", [128, 2, 512], BF16); Tkb = T("kb")
        kdt = C.sb("kdt", [128, 4, 256], BF16); Tkdt = T("kdt")
        vt = C.sb("vt", [128, 4, 512], BF16); Tvt = T("vt")
        sg = C.sb("sg", [128, 4, 512], F32); Tsg = T("sg")
        of = C.sb("of", [128, 4, 512], F32); Tof = [[T("of%d_%d" % (v, 0))] for v in range(4)]
        at = C.sb("at", [128, 128], BF16); Tat = T("at")
        state = [C.sb("st%d" % h, [128, 2, 512], F32) for h in range(2)]
        stb = [C.sb("stb%d" % h, [128, 2, 512], BF16) for h in range(2)]
        Tst = [[T("st%d_%d" % (h, dc)) for dc in range(2)] for h in range(2)]
        Tstb = [T("stb%d" % h) for h in range(2)]
        sq = C.sb("sq", [128, 4, 512], BF16); Tsq = [T("sq%d" % c) for c in range(4)]
        rstd = C.sb("rstd", [128, 512], F32); Trstd = T("rstd")
        ob = [C.sb("ob%d" % i, [128, 8, 512], BF16) for i in range(2)]
        Tob = [T("ob%d" % i) for i in range(2)]
        out_toks = []

        for tt in range(ntt):
            ts = slice(tt * 512, (tt + 1) * 512)
            hbuf, Th = hb[tt % 2], Thb[tt % 2]
            cbuf, Tcb = csb[tt % 2], Tcs[tt % 2]
            P.dma("sp", [lambda e, k=k, ts=ts, hbuf=hbuf: e.dma_start(out=hbuf[:, k, :], in_=hT[k, :, ts]) for k in range(KC)],
                  writes=[Th], sem="h%d" % (tt % 2))
            P.dma("sp", [lambda e, i=i, ts=ts, cbuf=cbuf: e.dma_start(out=cbuf[:, i, :], in_=cs_d[i, :, ts]) for i in range(2)],
                  writes=[Tcb], sem="cs%d" % (tt % 2))
            obuf, Tobuf = ob[tt % 2], Tob[tt % 2]
            def head_body(h, tt=tt, ts=ts, hbuf=hbuf, Th=Th, cbuf=cbuf, Tcb=Tcb, obuf=obuf, Tobuf=Tobuf):
                def proj_fm(col0, banks):
                    b = C.bank("pj", banks)
                    mm_group(C, b, [(w[h][:, k, col0:col0 + 128], hbuf[:, k, :]) for k in range(KC)], reads=[Tw[h], Th])
                    return b

                def rotary(col0, dst_bf, Tdst, want_f32):
                    b0 = proj_fm(col0, (0, 1, 2, 3))
                    b1 = proj_fm(col0 + 128, (0, 1, 2, 3))
                    P.op("act", lambda e: e.copy(out=raw[:, 0, :], in_=ps[:, b0, :]), reads=[C.Tps[b0]], writes=[Traw])
                    P.op("act", lambda e: e.copy(out=raw[:, 1, :], in_=ps[:, b1, :]), reads=[C.Tps[b1]], writes=[Traw])
                    cos, sin = cbuf[:, 0, :], cbuf[:, 1, :]
                    P.op("dve", lambda e: e.tensor_tensor(out=t1[:], in0=raw[:, 0, :], in1=cos, op=ALU.mult), reads=[Traw, Tcb], writes=[Tt1])
                    P.op("pool", lambda e: e.tensor_tensor(out=t2[:], in0=raw[:, 1, :], in1=sin, op=ALU.mult), reads=[Traw, Tcb], writes=[Tt2])
                    P.op("dve", lambda e: e.tensor_tensor(out=rot[:, 0, :], in0=t1[:], in1=t2[:], op=ALU.subtract), reads=[Tt1, Tt2], writes=[Trot])
                    P.op("dve", lambda e: e.tensor_tensor(out=t1[:], in0=raw[:, 0, :], in1=sin, op=ALU.mult), reads=[Traw, Tcb], writes=[Tt1])
                    P.op("pool", lambda e: e.tensor_tensor(out=t2[:], in0=raw[:, 1, :], in1=cos, op=ALU.mult), reads=[Traw, Tcb], writes=[Tt2])
                    P.op("dve", lambda e: e.tensor_tensor(out=rot[:, 1, :], in0=t1[:], in1=t2[:], op=ALU.add), reads=[Tt1, Tt2], writes=[Trot])
                    P.op("act", lambda e: e.copy(out=dst_bf[:], in_=rot[:]), reads=[Trot], writes=[Tdst])

                rotary(0, qb, Tqb, False)
                for dc in range(2):
                    P.op("pool", lambda e, dc=dc: e.tensor_tensor(
                        out=qd[:, dc, :].rearrange("p (c i) -> p c i", i=128), in0=rot[:, dc, :].rearrange("p (c i) -> p c i", i=128),
                        in1=gq[:, h:h + 1, :].to_broadcast([128, 4, 128]), op=ALU.mult), reads=[Trot, Tc], writes=[Tqd])
                rotary(256, kb, Tkb, True)
                for ci in range(4):
                    for dc in range(2):
                        def tr(e, ci=ci, dc=dc):
                            return e.transpose(out=ps[:, 4, dc * 128:(dc + 1) * 128], in_=rot[:, dc, ci * 128:(ci + 1) * 128], identity=ident[:])
                        P.op("pe", tr, reads=[Trot, Tc], writes=[C.Tps[4]])
                    P.op("act", lambda e, ci=ci: e.activation(out=kdt[:, ci, :], in_=ps[:, 4, 0:256], func=AF.Copy, scale=kdcd[:, h:h + 1]),
                         reads=[C.Tps[4], Tc], writes=[Tkdt])
                for ci in range(4):
                    b = C.bank("pj", (0, 1, 2, 3))
                    mm_group(C, b, [(hbuf[:, k, ci * 128:(ci + 1) * 128], w[h][:, k, 512:1024]) for k in range(KC)], reads=[Tw[h], Th])
                    P.op("act", lambda e, ci=ci, b=b: e.copy(out=vt[:, ci, :], in_=ps[:, b, :]), reads=[C.Tps[b]], writes=[Tvt])
                for vc in range(4):
                    b = proj_fm(1024 + vc * 128, (0, 1, 2, 3))
                    P.op("act", lambda e, vc=vc, b=b: e.activation(out=sg[:, vc, :], in_=ps[:, b, :], func=AF.Silu), reads=[C.Tps[b]], writes=[Tsg])
                for ci in range(4):
                    cs_ = slice(ci * 128, (ci + 1) * 128)
                    first = (tt == 0 and ci == 0)
                    mm_group(C, 4, [(kb[:, dc, cs_], qb[:, dc, cs_]) for dc in range(2)], reads=[Tkb, Tqb], n=128)
                    P.op("dve", lambda e: e.tensor_tensor(out=at[:], in0=ps[:, 4, 0:128], in1=dmat[:, h, :], op=ALU.mult),
                         reads=[C.Tps[4], Tc], writes=[Tat])

                    for dc in range(2):
                        b = 6 + dc
                        mm_group(C, b, [(kdt[:, ci, dc * 128:(dc + 1) * 128], vt[:, ci, :])], reads=[Tkdt, Tvt])
                        if first:
                            P.op("dve", lambda e, dc=dc, b=b: e.tensor_copy(out=state[h][:, dc, :], in_=ps[:, b, :]), reads=[C.Tps[b]], writes=[Tst[h][dc]])
                        else:
                            P.op("dve", lambda e, dc=dc, b=b: e.scalar_tensor_tensor(
                                out=state[h][:, dc, :], in0=state[h][:, dc, :], scalar=kdcd[:, 2 + h:3 + h], in1=ps[:, b, :],
                                op0=ALU.mult, op1=ALU.add), reads=[C.Tps[b], Tst[h][dc], Tc], writes=[Tst[h][dc]])
                    def omm(e, ci=ci, cs_=cs_, first=first):
                        ins = None
                        for vc in range(4):
                            vs = slice(vc * 128, (vc + 1) * 128)
                            ins = e.matmul(ps[:, 5, vs], lhsT=vt[:, ci, vs], rhs=at[:], start=True, stop=first)
                            if not first:
                                for dc in range(2):
                                    ins = e.matmul(ps[:, 5, vs], lhsT=stb[h][:, dc, vs], rhs=qd[:, dc, cs_], start=False, stop=(dc == 1))
                        return ins
                    P.op("pe", omm, reads=[Tvt, Tat, Tstb[h], Tqd], writes=[C.Tps[5]])
                    P.op("act", lambda e, cs_=cs_: e.copy(out=of[:, :, cs_], in_=ps[:, 5, :].rearrange("p (v i) -> p v i", i=128)),
                         reads=[C.Tps[5]], writes=[Tof[0][0]])
                    P.op("pool", lambda e: e.tensor_copy(out=stb[h][:], in_=state[h][:]), reads=Tst[h], writes=[Tstb[h]])
                for vc in range(4):
                    P.op("act", lambda e, vc=vc: e.activation(out=sq[:, vc, :], in_=of[:, vc, :], func=AF.Square), reads=[Tof[0][0]], writes=[Tsq[vc]])
                b = C.bank("pj", (0, 1, 2, 3))
                mm_group(C, b, [(C.ones[:], sq[:, vc, :]) for vc in range(4)], reads=[C.Tones] + Tsq)
                P.op("act", lambda e, b=b: e.activation(out=rstd[:], in_=ps[:, b, :], func=AF.Sqrt, scale=1.0 / RDV, bias=C.eps[:]),
                     reads=[C.Tps[b], C.Tones], writes=[Trstd])
                P.op("dve", lambda e: e.reciprocal(out=rstd[:], in_=rstd[:]), reads=[Trstd], writes=[Trstd])
                for vc in range(4):
                    P.op("dve", lambda e, vc=vc: e.tensor_tensor(out=of[:, vc, :], in0=of[:, vc, :], in1=rstd[:], op=ALU.mult),
                         reads=[Tof[0][0], Trstd], writes=[Tof[0][0]])
                    P.op("pool", lambda e, vc=vc: e.tensor_tensor(out=obuf[:, h * 4 + vc, :], in0=of[:, vc, :], in1=sg[:, vc, :], op=ALU.mult),
                         reads=[Tof[0][0], Tsg], writes=[Tobuf])
            for h in range(2):
                head_body(h)
            out_toks.append(P.dma("sp", [lambda e, c=c, ts=ts, obuf=obuf: e.dma_start(out=oT[c0 + c, :, ts], in_=obuf[:, c, :]) for c in range(8)],
                                  reads=[Tobuf], sem="o%d" % (tt % 2)))
    return out_toks


TL = 1919
TU = 1792
MASKNEG = -30000.0


def rel_bucket_np(n):
    n = np.maximum(n, 0)
    nf = np.maximum(n, 1).astype(np.float32)
    large = 16 + (np.log(nf / np.float32(16)) / np.float32(math.log(1024 / 16)) * np.float32(16)).astype(np.int32)
    large = np.minimum(large, 31)
    return np.where(n < 16, n, large)


def attn_consts():
    dist = np.arange(TL) - 511
    oh = np.zeros((33, TL), np.float32)
    bk = rel_bucket_np(dist)
    for j in range(TL):
        if dist[j] < 0:
            oh[32, j] = 1.0
        else:
            oh[bk[j], j] = 1.0
    cneg = np.zeros((16, 16), np.float32)
    negown = np.full((16, 16), MASKNEG, np.float32)
    for b in range(16):
        cneg[b, b:] = -2e30
        negown[b, b] = 0.0
    e16 = np.zeros((16, 16, 128), np.float32)
    for n in range(16):
        e16[n, n, :] = 1.0
    cneg, negown = np.repeat(cneg, 2, axis=0), np.repeat(negown, 2, axis=0)
    return oh, np.broadcast_to(cneg[None], (128, 32, 16)).copy(), np.broadcast_to(negown[None], (128, 32, 16)).copy(), e16


def emit_attn(C, kind, lam_init, h_src, win, r, rb_full, qkn_d, oh_d, ident_d, cneg_d, negown_d, e16_d, lam_d, sub_d, Rd, o_dst, c0,
              nqt=S // 512):
    moba = (kind == "moba")
    if True:
        P = C.P
        ps = C.ps
        hT = h_src
        oT = o_dst
        win_segs = [(0, r * 512), (512, 1024 + r * 512), (1024, 2048 + r * 512)]
        rb_d = rb_full[:, r * 8:(r + 1) * 8]
        w = C.sb("w", [128, KC, 1536], BF16); Tw = T("w")
        P.dma("pool", [lambda e, d0=d0, s0=s0: e.dma_start(out=w[:, :, d0:d0 + 512], in_=win[:, s0:s0 + 512].rearrange("(k p) n -> p k n", p=128))
                       for d0, s0 in win_segs], writes=[Tw], sem="w")
        rbx = C.sb("rbx", [33, 8], F32)
        qkn = C.sb("qkn_s", [128, 2], F32)
        oh = C.sb("oh_s", [33, TL], F32)
        ident = C.sb("ident_s", [128, 128], F32)
        Tc = T("consts")
        fns = [lambda e: e.dma_start(out=rbx[0:32, :], in_=rb_d), lambda e: e.dma_start(out=qkn[:], in_=qkn_d),
               lambda e: e.dma_start(out=oh[:], in_=oh_d), lambda e: e.dma_start(out=ident[:], in_=ident_d)]
        if moba:
            cneg = C.sb("cneg_s", [128, 32, 16], F32)
            negown = C.sb("negown_s", [128, 32, 16], F32)
            fns += [lambda e: e.dma_start(out=cneg[:], in_=cneg_d), lambda e: e.dma_start(out=negown[:], in_=negown_d)]
        else:
            lam = C.sb("lam_s", [1, 256], F32)
            subw = C.sb("subw_s", [128, 1], F32)
            fns += [lambda e: e.dma_start(out=lam[:], in_=lam_d), lambda e: e.dma_start(out=subw[:], in_=sub_d)]
        P.dma("sp", fns, writes=[Tc], sem="c")
        if moba:
            e16 = C.sb("e16_s", [128, 16, 128], BF16)
            Te16 = T("e16")
            P.op("pool", lambda e: e.memset(e16[:], 0.0), writes=[Te16])
            P.dma("pool", lambda e: e.dma_start(out=e16[0:16], in_=e16_d), writes=[Te16], sem="e16")
        P.op("pool", lambda e: e.memset(rbx[32:33, :], MASKNEG), reads=[Tc], writes=[Tc])
        identr = C.sb("identr", [128, 128], F32R)
        P.op("act", lambda e: e.copy(out=identr[:], in_=ident[:]), reads=[Tc], writes=[Tc])
        P.op("dve", lambda e: e.tensor_scalar(out=qkn[:, 0:1], in0=qkn[:, 0:1], scalar1=0.125, scalar2=None, op0=ALU.mult), reads=[Tc], writes=[Tc])
        ones33 = C.sb("ones33", [33, 128], F32)
        onesf = C.sb("onesf", [1, 128], F32)
        bones = C.sb("bones", [128, 128], BF16)
        Tk = T("kconst")
        P.op("pool", lambda e: e.memset(ones33[:], 1.0), writes=[Tk])
        P.op("pool", lambda e: e.memset(onesf[:], 1.0), writes=[Tk])
        P.op("pool", lambda e: e.memset(bones[:], 0.0), writes=[Tk])
        P.op("pool", lambda e: e.memset(bones[0:64, 0:64], 1.0), writes=[Tk])
        P.op("pool", lambda e: e.memset(bones[64:128, 64:128], 1.0), writes=[Tk])
        hm = C.sb("hm", [128, 2], F32)
        P.op("pool", lambda e: e.memset(hm[:], 0.0), writes=[Tk])
        P.op("pool", lambda e: e.memset(hm[0:64, 0:1], 1.0), writes=[Tk])
        P.op("pool", lambda e: e.memset(hm[64:128, 1:2], 1.0), writes=[Tk])
        c31 = C.sb("c31", [128, 8], F32); Tc31 = T("c31")
        brep = C.sb("brep", [33, 128], F32); Tbrep = T("brep")
        rsb = C.sb("rsb", [128, TL], F32); Trsb = T("rsb")
        TRd = [T("Rd%d" % h) for h in range(8)]

        def build_strip(hh):
            P.op("dve", lambda e: e.tensor_scalar(out=brep[:], in0=ones33[:], scalar1=rbx[:, hh:hh + 1], scalar2=None, op0=ALU.mult),
                 reads=[Tc, Tk], writes=[Tbrep])
            for cb in range(4):
                c0, c1 = cb * 512, min(TL, (cb + 1) * 512)
                b = C.bank("pj", (0, 1, 2, 3))

                def mmf(e, c0=c0, c1=c1, b=b):
                    return e.matmul(ps[:, b, 0:c1 - c0], lhsT=brep[:], rhs=oh[:, c0:c1], start=True, stop=True)
                P.op("pe", mmf, reads=[Tbrep, Tc], writes=[C.Tps[b]])
                P.op("act", lambda e, c0=c0, c1=c1, b=b: e.copy(out=rsb[:, c0:c1], in_=ps[:, b, 0:c1 - c0]), reads=[C.Tps[b]], writes=[Trsb])
            P.op("dve", lambda e: e.tensor_copy(out=c31[:, hh:hh + 1], in_=rsb[:, TL - 1:TL]), reads=[Trsb], writes=[Tc31])
            P.dma("sp", lambda e: e.dma_start(out=Rd.ap()[hh], in_=rsb[:]), reads=[Trsb], writes=[TRd[hh]], sem="rd")
        for hh in range(8):
            build_strip(hh)

        if not moba:
            lp = C.sb("lp", [1, 128], F32); l2 = C.sb("l2", [1, 2], F32); nlam = C.sb("nlam", [128, 1], F32); Tl = T("lam")
            P.op("dve", lambda e: e.tensor_tensor(out=lp[:, 0:64], in0=lam[:, 0:64], in1=lam[:, 64:128], op=ALU.mult), reads=[Tc], writes=[Tl])
            P.op("dve", lambda e: e.tensor_tensor(out=lp[:, 64:128], in0=lam[:, 128:192], in1=lam[:, 192:256], op=ALU.mult), reads=[Tc, Tl], writes=[Tl])
            P.op("dve", lambda e: e.reduce_sum(out=l2[:], in_=lp[:].rearrange("p (a b) -> p a b", a=2), axis=AX.X), reads=[Tl], writes=[Tl])
            P.op("act", lambda e: e.activation(out=l2[:], in_=l2[:], func=AF.Exp), reads=[Tl], writes=[Tl])
            P.op("dve", lambda e: e.tensor_tensor(out=lp[:, 0:1], in0=l2[:, 1:2], in1=l2[:, 0:1], op=ALU.subtract), reads=[Tl], writes=[Tl])
            P.op("dve", lambda e: e.tensor_scalar(out=lp[:, 0:1], in0=lp[:, 0:1], scalar1=-float(lam_init), scalar2=None, op0=ALU.add), reads=[Tl], writes=[Tl])

            def mml(e):
                return e.matmul(ps[:, 3, 0:1], lhsT=onesf[:], rhs=lp[:, 0:1], start=True, stop=True)
            P.op("pe", mml, reads=[Tl, Tk], writes=[C.Tps[3]])
            P.op("act", lambda e: e.copy(out=nlam[:], in_=ps[:, 3, 0:1]), reads=[C.Tps[3]], writes=[Tl])
            P.op("dve", lambda e: e.tensor_scalar(out=subw[:], in0=subw[:], scalar1=float(1.0 - lam_init), scalar2=None, op0=ALU.mult), reads=[Tc], writes=[Tc])

        hb = [C.sb("hb%d" % i, [128, KC, 512], BF16) for i in range(2)]
        Thb = [T("hb%d" % i) for i in range(2)]
        if not moba:
            qT = C.sb("qT", [128, S], BF16)
        TqT = T("qT")
        kT = C.sb("kT", [128, S], BF16); TkT = T("kT")
        qz = [C.sb("qz%d" % j, [128, S], BF16) for j in range(2)]
        Tqz = [T("qz%d" % j) for j in range(2)]
        vt = C.sb("vt", [128, 32, 512], BF16); Tvt = T("vt")
        sq = C.sb("sq", [128, 512], BF16); Tsq = T("sq")
        rstd = C.sb("rstd", [128, 512], F32); Trstd = T("rstd")
        tsk = [C.sb("tsk%d" % j, [128, TU], F32) for j in range(2)]
        Ttsk = [T("tsk%d" % j) for j in range(2)]
        pt = [C.sb("pt%d" % i, [128, 512], BF16) for i in range(6)]
        Tpt = [T("pt%d" % i) for i in range(6)]
        tmp = [C.sb("tmp%d" % i, [128, 512], F32) for i in range(1)]
        Ttmp = [T("tmp%d" % i) for i in range(1)]
        ob = [C.sb("ob%d" % i, [128, 512], BF16) for i in range(2)]
        Tob = [T("ob%d" % i) for i in range(2)]
        if moba:
            qF = C.sb("qF", [128, S], F32); TqF = T("qF")
            kF = C.sb("kF", [128, S], F32); TkF = T("kF")
            km = C.sb("km", [128, 16], F32); Tkm = T("km")
            gm = C.sb("gm", [128, 32, 16], F32); Tgm = T("gm")
            top8 = C.sb("top8", [128, 32, 8], F32); Ttop = [T("top8_%d" % g) for g in range(32)]
            thr = C.sb("thr", [128, 32], F32); Tthr = T("thr")
            mneg = C.sb("mneg", [128, 32, 16], F32); Tmn = T("mneg")
            mT = [C.sb("mT%d" % j, [128, S], BF16) for j in range(2)]
            TmT = [T("mT%d" % j) for j in range(2)]
            for j in range(2):
                P.op("pool", lambda e, j=j: e.memset(mT[j][:], 0.0), writes=[TmT[j]])
        r0 = C.sb("r0", [128, 512], F32); Tr0 = T("r0")
        a0 = C.sb("a0", [128, 512], F32); Ta0 = T("a0")
        if not moba:
            a1 = C.sb("a1", [128, 512], F32); Ta1 = T("a1")
        out_toks = []
        cnt = {"h": 0, "pt": 0, "tmp": 0, "ob": 0}

        def qk_norm(b, wcol, dst_main, Tdst, ts, dstF=None, TdstF=None):
            P.op("act", lambda e: e.activation(out=sq[:], in_=ps[:, b, :], func=AF.Square), reads=[C.Tps[b]], writes=[Tsq])
            b2 = C.bank("nrm", (4, 5))
            mm_group(C, b2, [(bones[:], sq[:])], reads=[Tk, Tsq])
            P.op("act", lambda e: e.activation(out=rstd[:], in_=ps[:, b2, :], func=AF.Sqrt, scale=1.0 / 64, bias=C.eps[:]),
                 reads=[C.Tps[b2], C.Tones], writes=[Trstd])
            P.op("dve", lambda e: e.reciprocal(out=rstd[:], in_=rstd[:]), reads=[Trstd], writes=[Trstd])
            if dstF is not None:
                P.op("dve", lambda e: e.scalar_tensor_tensor(out=dstF[:, ts], in0=ps[:, b, :], scalar=qkn[:, wcol:wcol + 1], in1=rstd[:],
                                                             op0=ALU.mult, op1=ALU.mult), reads=[C.Tps[b], Tc, Trstd], writes=[TdstF])
                if dst_main is not None:
                    P.op("pool", lambda e: e.tensor_copy(out=dst_main[:, ts], in_=dstF[:, ts]), reads=[TdstF], writes=[Tdst])
            else:
                P.op("dve", lambda e: e.scalar_tensor_tensor(out=dst_main[:, ts], in0=ps[:, b, :], scalar=qkn[:, wcol:wcol + 1], in1=rstd[:],
                                                             op0=ALU.mult, op1=ALU.mult), reads=[C.Tps[b], Tc, Trstd], writes=[Tdst])

        def project(p, tt):
            ts = slice(tt * 512, (tt + 1) * 512)
            i = cnt["h"] % 2
            cnt["h"] += 1
            hbuf, Th = hb[i], Thb[i]
            P.dma("sp", [lambda e, k=k: e.dma_start(out=hbuf[:, k, :], in_=hT[k, :, ts]) for k in range(KC)], writes=[Th], sem="h%d" % i)
            for which, (dst, Td, dF, TdF) in enumerate([(None if moba else qT, TqT, qF if moba else None, TqF if moba else None),
                                                        (kT, TkT, kF if moba else None, TkF if moba else None)]):
                b = C.bank("pj", (0, 1, 2, 3))
                col0 = which * 512 + p * 128
                mm_group(C, b, [(w[:, k, col0:col0 + 128], hbuf[:, k, :]) for k in range(KC)], reads=[Tw, Th])
                qk_norm(b, which, dst, Td, ts, dF, TdF)
                if which == 0:
                    for j in range(2):
                        qsrc, Tqsrc = (qF, TqF) if moba else (qT, TqT)
                        P.op("pool", lambda e, j=j: e.tensor_scalar(out=qz[j][:, ts], in0=qsrc[:, ts], scalar1=hm[:, j:j + 1], scalar2=None, op0=ALU.mult),
                             reads=[Tqsrc, Tk], writes=[Tqz[j]])
            if p == 0:
                for ci in range(4):
                    b = C.bank("pj", (0, 1, 2, 3))
                    mm_group(C, b, [(hbuf[:, k, ci * 128:(ci + 1) * 128], w[:, k, 1024:1536]) for k in range(KC)], reads=[Tw, Th])
                    P.op("act", lambda e, ci=ci, b=b: e.copy(out=vt[:, tt * 4 + ci, :], in_=ps[:, b, :]), reads=[C.Tps[b]], writes=[Tvt])

        def gate_head(j):
            hp = slice(64 * j, 64 * j + 64)
            NG = 4 * nqt

            def mg(e):
                ins = None
                for g in range(NG):
                    ins = e.matmul(ps[:, 6, g * 16:(g + 1) * 16], lhsT=qF[hp, g * 128:(g + 1) * 128], rhs=km[hp, :], start=True, stop=True)
                return ins
            P.op("pe", mg, reads=[TqF, Tkm], writes=[C.Tps[6]])
            P.op("dve", lambda e: e.tensor_tensor(out=gm[:, 0:NG, :], in0=ps[:, 6, 0:NG * 16].rearrange("p (g n) -> p g n", n=16),
                                                  in1=cneg[:, 0:NG, :], op=ALU.add), reads=[C.Tps[6], Tc], writes=[Tgm])
            for g in range(NG):
                P.op("dve", lambda e, g=g: e.max(out=top8[:, g, :], in_=gm[:, g, :]), reads=[Tgm], writes=[Ttop[g]])
            P.op("dve", lambda e: e.tensor_scalar(out=thr[:, 0:NG], in0=top8[:, 0:NG, 2], scalar1=-1e30, scalar2=None, op0=ALU.max),
                 reads=Ttop[:NG], writes=[Tthr])
            P.op("dve", lambda e: e.tensor_tensor(out=mneg[:, 0:NG, :], in0=gm[:, 0:NG, :],
                                                  in1=thr[:, 0:NG].rearrange("p (g o) -> p g o", o=1).to_broadcast([128, NG, 16]), op=ALU.is_lt),
                 reads=[Tgm, Tthr], writes=[Tmn])
            P.op("pool", lambda e: e.tensor_tensor(out=mneg[:, 0:NG, :], in0=mneg[:, 0:NG, :], in1=negown[:, 0:NG, :], op=ALU.mult),
                 reads=[Tmn, Tc], writes=[Tmn])
            for r_ in range(NG // 4):
                def tr(e, r_=r_):
                    ins = None
                    for i in range(4):
                        ins = e.transpose(out=ps[0:16, 7, i * 128:(i + 1) * 128], in_=mneg[:, r_ * 4 + i, :], identity=ident[:])
                    return ins
                P.op("pe", tr, reads=[Tmn, Tc], writes=[C.Tps[7]])
                P.op("act", lambda e, r_=r_: e.copy(out=mT[j][0:16, r_ * 512:(r_ + 1) * 512], in_=ps[0:16, 7, :]), reads=[C.Tps[7]], writes=[TmT[j]])

        def stage1(p, j, qt, kt):
            hp = slice(64 * j, 64 * j + 64)
            qs = slice(qt * 512, (qt + 1) * 512)
            ks = slice(kt * 128, (kt + 1) * 128)
            delta = qt * 512 - kt * 128
            bs = C.bank("s", (0, 1, 2, 3))

            near = delta < 1024

            def ms(e):
                ins = e.matmul(ps[:, bs, :], lhsT=kT[:, ks], rhs=qz[j][:, qs], start=True, stop=not (moba or near))
                if moba:
                    ins = e.matmul(ps[:, bs, :], lhsT=e16[:, kt // 2, :], rhs=mT[j][:, qs], start=False, stop=not near)
                if near:
                    ins = e.matmul(ps[:, bs, :], lhsT=identr[:], rhs=tsk[j][:, delta + 384:delta + 384 + 512].bitcast(F32R),
                                   start=False, stop=True)
                return ins
            P.op("pe", ms, reads=[TkT, Tqz[j], Tc] + ([Te16, TmT[j]] if moba else []) + ([Ttsk[j]] if near else []), writes=[C.Tps[bs]])
            ip = cnt["pt"] % len(pt)
            cnt["pt"] += 1
            if near:
                P.op("act", lambda e: e.activation(out=pt[ip][:], in_=ps[:, bs, :], func=AF.Exp), reads=[C.Tps[bs]], writes=[Tpt[ip]])
            else:
                P.op("act", lambda e: e.activation(out=pt[ip][:], in_=ps[:, bs, :], func=AF.Exp, bias=c31[:, 2 * p + j:2 * p + j + 1]),
                     reads=[C.Tps[bs], Tc31], writes=[Tpt[ip]])
            return ip

        def stage2(p, j, kt, ip, bo, bl, first, last):
            hp = slice(64 * j, 64 * j + 64)

            def mpv(e):
                e.matmul(ps[:, bo, :], lhsT=vt[:, kt, p * 128:(p + 1) * 128], rhs=pt[ip][:], start=first, stop=last)
                return e.matmul(ps[:, bl, :], lhsT=C.ones[:], rhs=pt[ip][:], start=first, stop=last)
            P.op("pe", mpv, reads=[Tvt, Tpt[ip], C.Tones], writes=[C.Tps[bo], C.Tps[bl]])

        def finish_head(j, bo, bl):
            if moba:
                hp = slice(64 * j, 64 * j + 64)
                P.op("dve", lambda e: e.reciprocal(out=r0[hp, :], in_=ps[hp, bl, :]), reads=[C.Tps[bl]], writes=[Tr0])
                P.op("dve", lambda e: e.tensor_tensor(out=a0[hp, :], in0=ps[hp, bo, :], in1=r0[hp, :], op=ALU.mult), reads=[C.Tps[bo], Tr0], writes=[Ta0])
                return
            a, Ta = (a0, Ta0) if j == 0 else (a1, Ta1)
            P.op("dve", lambda e: e.reciprocal(out=r0[:], in_=ps[:, bl, :]), reads=[C.Tps[bl]], writes=[Tr0])
            P.op("dve", lambda e: e.tensor_tensor(out=a[:], in0=ps[:, bo, :], in1=r0[:], op=ALU.mult), reads=[C.Tps[bo], Tr0], writes=[Ta])

        def finish_qt(p, qt, bo, bl):
            qs = slice(qt * 512, (qt + 1) * 512)
            io = cnt["ob"] % 2
            cnt["ob"] += 1
            if moba:
                P.op("pool", lambda e: e.tensor_copy(out=ob[io][:], in_=a0[:]), reads=[Ta0], writes=[Tob[io]])
            else:
                P.op("dve", lambda e: e.scalar_tensor_tensor(out=a0[:], in0=a1[:], scalar=nlam[:], in1=a0[:], op0=ALU.mult, op1=ALU.add),
                     reads=[Ta0, Ta1, Tl], writes=[Ta0])
                P.op("act", lambda e: e.activation(out=sq[:], in_=a0[:], func=AF.Square), reads=[Ta0], writes=[Tsq])
                bn = C.bank("s", (0, 1, 2, 3))
                mm_group(C, bn, [(C.ones[:], sq[:])], reads=[C.Tones, Tsq])
                P.op("act", lambda e: e.activation(out=rstd[:], in_=ps[:, bn, :], func=AF.Sqrt, scale=1.0 / 128, bias=C.eps[:]),
                     reads=[C.Tps[bn], C.Tones], writes=[Trstd])
                P.op("dve", lambda e: e.reciprocal(out=rstd[:], in_=rstd[:]), reads=[Trstd], writes=[Trstd])
                P.op("dve", lambda e: e.scalar_tensor_tensor(out=ob[io][:], in0=a0[:], scalar=subw[:], in1=rstd[:], op0=ALU.mult, op1=ALU.mult),
                     reads=[Ta0, Tc, Trstd], writes=[Tob[io]])
            out_toks.append(P.dma("sp", lambda e: e.dma_start(out=oT[c0 + p, :, qs], in_=ob[io][:]), reads=[Tob[io]], sem="o%d" % io))

        def attention(p):
            PD = 3
            tiles = []
            for qt in range(nqt):
                nk = 4 * qt + 4
                if True:
                    for j in range(2):
                        bo, bl = 4 + 2 * j, 5 + 2 * j
                        for kt in range(nk):
                            lastt = (kt == nk - 1)
                            tiles.append((j, qt, kt, bo, bl, kt == 0, lastt, (j, bo, bl) if lastt else None, (qt, bo, bl) if (lastt and j == 1) else None))
            pend = []

            def retire():
                (j, qt, kt, bo, bl, first, last, fh, fq), ip = pend.pop(0)
                stage2(p, j, kt, ip, bo, bl, first, last)
                if fh is not None:
                    finish_head(*fh)
                if fq is not None:
                    finish_qt(p, *fq)
            for tl in tiles:
                ip = stage1(p, tl[0], tl[1], tl[2])
                pend.append((tl, ip))
                if len(pend) > PD:
                    retire()
            while pend:
                retire()

        def load_tsk(p, j):
            hh = 2 * p + j
            src = bass.AP(Rd, hh * 128 * TL + 127, [[TL - 1, 128], [1, TU]])
            P.dma("sp", lambda e: e.dma_start(out=tsk[j][:], in_=src), reads=[TRd[hh]], writes=[Ttsk[j]], sem="tsk%d" % j)
            P.op("act", lambda e: e.copy(out=tsk[j][:].bitcast(F32R), in_=tsk[j][:]), reads=[Ttsk[j]], writes=[Ttsk[j]])

        for p in range(4):
            for tt in range(8):
                project(p, tt)
            for j in range(2):
                load_tsk(p, j)
            if moba:
                P.op("dve", lambda e: e.reduce_sum(out=km[:], in_=kF[:].rearrange("p (n t) -> p n t", t=256), axis=AX.X), reads=[TkF], writes=[Tkm])
                for j in range(2):
                    gate_head(j)
            attention(p)
    return out_toks


def build_fused():
    nc = bass.Bass("TRN2", target_bir_lowering=False)

    def inp(name, shape, dt=F32):
        return nc.dram_tensor(name, shape, dt, kind="ExternalInput").ap()
    xT = inp("xT", [KC, 128, S])
    n1 = inp("n1", [4, 128, KC]); n2 = inp("n2", [4, 128, KC])
    wup = inp("wup", [4, D, DFF]); wdown = inp("wdown", [4, DFF, D])
    retin = inp("retin", [2, D, 6144]); retout = inp("retout", [2, 2048, D])
    mobain = inp("mobain", [D, 3072]); mobaout = inp("mobaout", [D, D])
    diffin = inp("diffin", [D, 3072]); diffout = inp("diffout", [D, D])
    rb = inp("rb", [32, 16]); qknm = inp("qknm", [128, 2]); qknd = inp("qknd", [128, 2])
    lam = inp("lam", [1, 256]); subw = inp("subw", [128, 1])
    cs_d = inp("cs", [2, 128, S]); dmat_d = inp("dmat", [4, 128, 128]); gq_d = inp("gq", [4, 128, 128]); kdcd_d = inp("kdcd", [128, 8])
    ident_d = inp("ident", [128, 128]); oh_d = inp("oh", [33, TL])
    cneg_d = inp("cneg", [128, 32, 16]); negown_d = inp("negown", [128, 32, 16]); e16_d = inp("e16", [16, 16, 128])
    xs = nc.dram_tensor("xs", [KC, 128, S], F32, kind="Internal").ap()
    hs = nc.dram_tensor("hs", [KC, 128, S], BF16, kind="Internal").ap()
    osc = nc.dram_tensor("osc", [16, 128, S], BF16, kind="Internal").ap()
    Rd = nc.dram_tensor("Rscr", [8, 128, TL], F32, kind="Internal")
    xo = nc.dram_tensor("xo", [KC, 128, S], F32, kind="ExternalOutput").ap()

    with ExitStack() as st:
        C = Ctx(nc, st)
        P = C.P

        def phase(fn):
            P.barrier()
            with ExitStack() as ph:
                C.ph = ph
                C.bank_rr = {}
                fn()
            C.ph = None
        phase(lambda: emit_phase_a(C, S, xT, None, n1=n1[0], h_dst=hs))
        for i in range(4):
            kind, j = i % 3, i // 3
            for r in range(2):
                if kind == 0:
                    phase(lambda: emit_ret(C, hs, retin[j], (2 * r, 2 * r + 1), cs_d, dmat_d, gq_d, kdcd_d, ident_d, osc, 8 * r))
                elif kind == 1:
                    phase(lambda: emit_attn(C, "moba", 0.0, hs, mobain, r, rb, qknm, oh_d, ident_d, cneg_d, negown_d, e16_d, None, None, Rd, osc, 4 * r))
                else:
                    lam_init = 0.8 - 0.6 * math.exp(-0.3 * i)
                    phase(lambda: emit_attn(C, "diff", lam_init, hs, diffin, r, rb, qknd, oh_d, ident_d, None, None, None, lam, subw, Rd, osc, 4 * r))
            wout, fo = ((retout[j], 2048) if kind == 0 else ((mobaout, 1024) if kind == 1 else (diffout, 1024)))
            last = (i == 3)
            phase(lambda: emit_phase_a(C, S, xT if i == 0 else xs, xo if last else xs, o_src=osc, wout=wout, fo=fo, wup=wup[i], wdown=wdown[i],
                                       n2=n2[i], n1=None if last else n1[i + 1], h_dst=None if last else hs))
        P.barrier()
        P.emit(st)
    return nc


_NC_CACHE = {}


def _fm(a):
    return np.ascontiguousarray(a.T.reshape(a.shape[1] // 128, 128, a.shape[0]))


def _unfm(a):
    return np.ascontiguousarray(a.reshape(-1, a.shape[2]).T)


def _nw(w):
    return np.ascontiguousarray(w.reshape(w.shape[0], KC, 128).transpose(0, 2, 1))


def kernel(x, rel_bias, norm1, norm2, w_up, w_down, ret_w_in, ret_w_out,
           moba_w_in, moba_q_norm, moba_k_norm, moba_w_out,
           diff_w_in, diff_q_norm, diff_k_norm, diff_lambda, diff_subln, diff_w_out):
    f32 = np.float32
    A = lambda a: np.ascontiguousarray(np.asarray(a, f32))
    x = A(x)
    if "fused" not in _NC_CACHE:
        _NC_CACHE["fused"] = build_fused()
    nc = _NC_CACHE["fused"]
    cs, ph = ret_consts()
    oh, cneg, negown, e16 = attn_consts()
    shared = {
        "n1": _nw(A(norm1)), "n2": _nw(A(norm2)), "wup": A(w_up), "wdown": A(w_down),
        "retin": A(ret_w_in), "retout": A(ret_w_out), "mobain": A(moba_w_in[0]), "mobaout": A(moba_w_out[0]),
        "diffin": A(diff_w_in[0]), "diffout": A(diff_w_out[0]), "rb": A(rel_bias),
        "qknm": A(np.stack([np.tile(A(moba_q_norm[0]), 2), np.tile(A(moba_k_norm[0]), 2)], axis=1)),
        "qknd": A(np.stack([np.tile(A(diff_q_norm[0]), 2), np.tile(A(diff_k_norm[0]), 2)], axis=1)),
        "lam": A(diff_lambda[0]).reshape(1, 256), "subw": A(diff_subln[0]).reshape(128, 1),
        "cs": cs, "dmat": A(np.stack([ph[h][0] for h in range(4)])), "gq": A(np.stack([ph[h][1] for h in range(4)])),
        "kdcd": A(np.stack([ph[h][2] for h in range(4)] + [ph[h][3] for h in range(4)], axis=1)),
        "ident": np.eye(128, dtype=f32), "oh": oh, "cneg": cneg, "negown": negown, "e16": e16,
    }
    ims = []
    for c in range(8):
        d = dict(shared)
        d["xT"] = _fm(x[c % 4])
        ims.append(d)
    res = run_bass_kernel_spmd(nc, ims, core_ids=list(range(8))).results
    out = np.empty((4, S, D), f32)
    for b in range(4):
        out[b] = _unfm(res[b]["xo"])
    return out
```
